# Optimizing a Trainium2 kernel written in Bass

```python
import jax
import jax.numpy as jnp
from jax import lax
import numpy as np

D_MODEL = 2048
BATCH = 4
SEQ = 4096
DEPTH = 2

GRID_W = 64
CTX_LEN = 256
HEAD_DIM = 128
MIX_DIM = D_MODEL
RET_HEADS = MIX_DIM // 2 // HEAD_DIM
RET_DIM = RET_HEADS * HEAD_DIM
RET_CHUNK = 128
GMLP_DIM = MIX_DIM - RET_DIM
GMLP_GROUPS = 8
GMLP_CHUNK = 128
NA_HEADS = MIX_DIM // 2 // HEAD_DIM
NA_DIM = NA_HEADS * HEAD_DIM
WIN_ROWS = 8
WIN_COLS = 16
GLA_HEADS = 8
GLA_DV = (MIX_DIM - NA_DIM) // GLA_HEADS
GLA_DK = GLA_DV // 2
GLA_QK_DIM = GLA_HEADS * GLA_DK
GLA_V_DIM = GLA_HEADS * GLA_DV
GLA_GATE_RANK = 16
GLA_TAU = 16.0
GLA_CHUNK = 64
N_EXPERTS = 16
D_EXPERT = 2048
EC_CAPACITY = 2
ROPE_BASE = 10000.0
RMS_EPS = 1e-6
N_EVEN = (DEPTH + 1) // 2
N_ODD = DEPTH // 2

EVEN_WIDTHS = (RET_DIM, RET_DIM, GMLP_DIM, GMLP_DIM, RET_DIM, RET_DIM)
EVEN_Q_COLS = 2 * RET_DIM + 2 * GMLP_DIM
EVEN_COLS = EVEN_Q_COLS + 2 * RET_DIM
ODD_Q_WIDTHS = (NA_DIM, GLA_QK_DIM, GLA_V_DIM)
ODD_KV_WIDTHS = (NA_DIM, NA_DIM, GLA_QK_DIM, GLA_V_DIM, 2 * GLA_GATE_RANK)
ODD_Q_COLS = NA_DIM + GLA_QK_DIM + GLA_V_DIM
ODD_COLS = ODD_Q_COLS + 2 * NA_DIM + GLA_QK_DIM + GLA_V_DIM + 2 * GLA_GATE_RANK

kernel_name = 'hybrid_diffusion_retention_gmlp_natten_gla_ecmoe'


def rmsnorm(x, gain):
    xf = x.astype(jnp.float32)
    xf = xf * lax.rsqrt(jnp.mean(xf * xf, axis=-1, keepdims=True) + RMS_EPS)
    return (xf * gain.astype(jnp.float32)).astype(x.dtype)


def split_cols(p, widths):
    return jnp.split(p, np.cumsum(widths)[:-1].tolist(), axis=-1)


def to_heads(z, n_heads):
    b, t, _ = z.shape
    return z.reshape(b, t, n_heads, -1).transpose(0, 2, 1, 3)


def merge_heads(z):
    b, h, t, d = z.shape
    return z.transpose(0, 2, 1, 3).reshape(b, t, h * d)


def head_rmsnorm(o, gain, dtype):
    of = o.astype(jnp.float32)
    of = of * lax.rsqrt(jnp.mean(of * of, axis=-1, keepdims=True) + RMS_EPS)
    return (merge_heads(of) * gain.astype(jnp.float32)).astype(dtype)


def axial_rotary(z, rows, cols):
    half = z.shape[-1] // 2
    nf = half // 2
    freq = ROPE_BASE ** (-jnp.arange(nf, dtype=jnp.float32) / nf)

    def rotate(u, pos):
        ang = pos.astype(jnp.float32)[:, None] * freq
        cos, sin = jnp.cos(ang).astype(u.dtype), jnp.sin(ang).astype(u.dtype)
        u1, u2 = u[..., :nf], u[..., nf:]
        return jnp.concatenate([u1 * cos - u2 * sin, u1 * sin + u2 * cos], axis=-1)

    return jnp.concatenate([rotate(z[..., :half], rows), rotate(z[..., half:], cols)], axis=-1)


def chunked_decay_attention(q, k, v, log_a, s0, chunk):
    B, H, T, dk = q.shape
    dv = v.shape[-1]
    n = T // chunk
    f32 = jnp.float32

    def blocks(z):
        return z.astype(f32).reshape(B, H, n, chunk, z.shape[-1])

    qc, kc, vc = blocks(q), blocks(k), blocks(v)
    b = jnp.cumsum(blocks(jnp.broadcast_to(log_a, q.shape)), axis=3)
    b_end = b[:, :, :, -1:, :]
    q_in = qc * jnp.exp(b)
    k_in = kc * jnp.exp(-b)
    k_out = kc * jnp.exp(b_end - b)
    tri = jnp.tril(jnp.ones((chunk, chunk), dtype=bool))
    att = jnp.where(tri, jnp.einsum('bhntd,bhnsd->bhnts', q_in, k_in), 0.0)
    o_intra = jnp.einsum('bhnts,bhnse->bhnte', att, vc)
    kv = jnp.einsum('bhnsd,bhnse->bhnde', k_out, vc)
    dec = jnp.exp(b_end[:, :, :, 0, :])

    def step(S, inp):
        kv_i, dec_i = inp
        return dec_i[..., None] * S + kv_i, S

    s_fin, s_in = lax.scan(step, s0.astype(f32), (jnp.moveaxis(kv, 2, 0), jnp.moveaxis(dec, 2, 0)))
    o_inter = jnp.einsum('bhntd,nbhde->bhnte', q_in, s_in)
    return (o_intra + o_inter).reshape(B, H, T, dv), s_fin


def decayed_final_state(k, v, log_a):
    la = jnp.broadcast_to(log_a, k.shape).astype(jnp.float32)
    cum = jnp.cumsum(la, axis=2)
    w = jnp.exp(cum[:, :, -1:, :] - cum)
    return jnp.einsum('bhtd,bhte->bhde', k.astype(jnp.float32) * w, v.astype(jnp.float32))


def bidir_decay_attention(q, k, v, la, q_c, k_c, v_c, la_c, chunk):
    B, H, _, dk = k.shape
    dv = v.shape[-1]
    s_zero = jnp.zeros((B, H, dk, dv), jnp.float32)
    o_lat = 0.0
    o_ctx = None if q_c is None else 0.0
    for d in range(2):
        fl = (lambda z: z) if d == 0 else (lambda z: jnp.flip(z, axis=2))
        if q_c is None:
            s_c = decayed_final_state(fl(k_c), fl(v_c), fl(la_c[d]))
        else:
            oc, s_c = chunked_decay_attention(fl(q_c), fl(k_c), fl(v_c), fl(la_c[d]), s_zero, chunk)
            o_ctx = o_ctx + fl(oc)
        ol, _ = chunked_decay_attention(fl(q), fl(k), fl(v), fl(la[d]), s_c, chunk)
        o_lat = o_lat + fl(ol)
    return o_ctx, o_lat


def chunk_gmlp(u, v, norm_gain, ws, bs):
    B, T, _ = u.shape
    u = jax.nn.gelu(u)
    v = rmsnorm(jax.nn.gelu(v), norm_gain)
    vc = v.reshape(B, T // GMLP_CHUNK, GMLP_CHUNK, GMLP_GROUPS, -1)
    mixed = jnp.einsum('gps,bnsgc->bnpgc', ws, vc) + bs.T[None, None, :, :, None]
    return u * mixed.reshape(B, T, -1).astype(u.dtype)


def context_attention(q, k, v):
    s = jnp.einsum('bhqd,bhkd->bhqk', q, k).astype(jnp.float32) * (q.shape[-1] ** -0.5)
    return jnp.einsum('bhqk,bhkd->bhqd', jax.nn.softmax(s, axis=-1).astype(v.dtype), v)


def neighbourhood_attention(q, k, v, k_ctx, v_ctx, rpb):
    B, H, T, dh = q.shape
    rows = T // GRID_W
    wr, wc = min(WIN_ROWS, rows), WIN_COLS
    grid = lambda z: z.reshape(B, H, rows, GRID_W, dh)
    qg, kg, vg = grid(q * (dh ** -0.5)), grid(k), grid(v)
    col = jnp.arange(GRID_W)
    cstart = jnp.clip(col - wc // 2, 0, GRID_W - wc)
    cidx = cstart[:, None] + jnp.arange(wc)[None, :]
    rpb_c = rpb[:, :, cidx - col[:, None] + (WIN_COLS - 1)]

    def row_block(args):
        r, q_r = args
        rstart = jnp.clip(r - wr // 2, 0, rows - wr)
        k_win = lax.dynamic_slice_in_dim(kg, rstart, wr, axis=2)[:, :, :, cidx, :]
        v_win = lax.dynamic_slice_in_dim(vg, rstart, wr, axis=2)[:, :, :, cidx, :]
        dr = rstart + jnp.arange(wr) - r + (WIN_ROWS - 1)
        bias = jnp.take(rpb_c, dr, axis=1).transpose(0, 2, 1, 3)
        s_win = jnp.einsum('bhqd,bhiqjd->bhqij', q_r, k_win).astype(jnp.float32) + bias[None].astype(jnp.float32)
        s_ctx = jnp.einsum('bhqd,bhld->bhql', q_r, k_ctx).astype(jnp.float32)
        s = jnp.concatenate([s_win.reshape(B, H, GRID_W, wr * wc), s_ctx], axis=-1)
        p = jax.nn.softmax(s, axis=-1).astype(v.dtype)
        p_win = p[..., :wr * wc].reshape(B, H, GRID_W, wr, wc)
        return (jnp.einsum('bhqij,bhiqjd->bhqd', p_win, v_win)
                + jnp.einsum('bhql,bhld->bhqd', p[..., wr * wc:], v_ctx))

    out = lax.map(row_block, (jnp.arange(rows), jnp.moveaxis(qg, 2, 0)))
    return jnp.moveaxis(out, 0, 2).reshape(B, H, T, dh)


def gla_log_decay(lr, w_up, b_up, d):
    z = lr[..., d * GLA_GATE_RANK:(d + 1) * GLA_GATE_RANK] @ w_up[d] + b_up[d]
    return to_heads(jax.nn.log_sigmoid(z.astype(jnp.float32)) / GLA_TAU, GLA_HEADS)


def even_mixer(h, hc, w_in, w_out, gamma_logit, ret_norm, gmlp_norm, gmlp_ws, gmlp_bs, last):
    T = h.shape[1]
    t = jnp.arange(T)
    rows, cols = t // GRID_W, t % GRID_W
    scale = HEAD_DIM ** -0.5
    rq, rg, u, vg, rk, rv = split_cols(h @ w_in, EVEN_WIDTHS)
    log_g = jax.nn.log_sigmoid(gamma_logit.astype(jnp.float32))
    la = [log_g[d][None, :, None, None] for d in range(2)]
    q_l = axial_rotary(to_heads(rq, RET_HEADS) * scale, rows, cols)
    k_l = axial_rotary(to_heads(rk, RET_HEADS), rows, cols)
    v_l = to_heads(rv, RET_HEADS)
    if last:
        ck, cv = split_cols(hc @ w_in[:, EVEN_Q_COLS:], EVEN_WIDTHS[4:])
        cq = None
    else:
        cq, cg, cu, cvg, ck, cv = split_cols(hc @ w_in, EVEN_WIDTHS)
        cq = to_heads(cq, RET_HEADS) * scale
    o_c, o_l = bidir_decay_attention(q_l, k_l, v_l, la, cq, to_heads(ck, RET_HEADS), to_heads(cv, RET_HEADS), la, RET_CHUNK)
    ret_lat = head_rmsnorm(o_l, ret_norm, h.dtype) * jax.nn.silu(rg)
    y = jnp.concatenate([ret_lat, chunk_gmlp(u, vg, gmlp_norm, gmlp_ws, gmlp_bs)], axis=-1) @ w_out
    if last:
        return None, y
    ret_ctx = head_rmsnorm(o_c, ret_norm, hc.dtype) * jax.nn.silu(cg)
    yc = jnp.concatenate([ret_ctx, chunk_gmlp(cu, cvg, gmlp_norm, gmlp_ws, gmlp_bs)], axis=-1) @ w_out
    return yc, y


def odd_mixer(h, hc, w_in, w_out, rpb, w_up, b_up, gla_norm, last):
    p = h @ w_in
    nq, gq, gr = split_cols(p[..., :ODD_Q_COLS], ODD_Q_WIDTHS)
    nk, nv, gk, gv, glr = split_cols(p[..., ODD_Q_COLS:], ODD_KV_WIDTHS)
    if last:
        pc_kv = hc @ w_in[:, ODD_Q_COLS:]
    else:
        pc = hc @ w_in
        cnq, cgq, cgr = split_cols(pc[..., :ODD_Q_COLS], ODD_Q_WIDTHS)
        pc_kv = pc[..., ODD_Q_COLS:]
    cnk, cnv, cgk, cgv, clr = split_cols(pc_kv, ODD_KV_WIDTHS)
    nk_c, nv_c = to_heads(cnk, NA_HEADS), to_heads(cnv, NA_HEADS)
    o_na = neighbourhood_attention(to_heads(nq, NA_HEADS), to_heads(nk, NA_HEADS), to_heads(nv, NA_HEADS), nk_c, nv_c, rpb)
    la_l = [gla_log_decay(glr, w_up, b_up, d) for d in range(2)]
    la_c = [gla_log_decay(clr, w_up, b_up, d) for d in range(2)]
    qscale = GLA_DK ** -0.5
    cq = None if last else to_heads(cgq, GLA_HEADS) * qscale
    o_c, o_l = bidir_decay_attention(to_heads(gq, GLA_HEADS) * qscale, to_heads(gk, GLA_HEADS), to_heads(gv, GLA_HEADS), la_l,
                                     cq, to_heads(cgk, GLA_HEADS), to_heads(cgv, GLA_HEADS), la_c, GLA_CHUNK)
    gla_lat = head_rmsnorm(o_l, gla_norm, h.dtype) * jax.nn.silu(gr)
    y = jnp.concatenate([merge_heads(o_na), gla_lat], axis=-1) @ w_out
    if last:
        return None, y
    o_na_c = context_attention(to_heads(cnq, NA_HEADS), nk_c, nv_c)
    gla_ctx = head_rmsnorm(o_c, gla_norm, hc.dtype) * jax.nn.silu(cgr)
    yc = jnp.concatenate([merge_heads(o_na_c), gla_ctx], axis=-1) @ w_out
    return yc, y


def expert_choice_ffn(h, router_w, w_gate, w_up, w_down):
    B, T, _ = h.shape
    cap = EC_CAPACITY * T // N_EXPERTS
    aff = jax.nn.softmax((h @ router_w).astype(jnp.float32), axis=-1)
    g, idx = lax.top_k(aff.transpose(0, 2, 1), cap)
    b_idx = jnp.arange(B)[:, None, None]
    xs = h[b_idx, idx]
    a = jnp.einsum('becd,edf->becf', xs, w_gate)
    up = jnp.einsum('becd,edf->becf', xs, w_up)
    y = jnp.einsum('becf,efd->becd', jax.nn.silu(a) * up, w_down) * g[..., None].astype(h.dtype)
    return jnp.zeros_like(h).at[b_idx, idx].add(y.astype(h.dtype))


def setup_inputs(seed: int = 0) -> dict:
    key = jax.random.key(seed)
    ks = iter(jax.random.split(key, 32))
    f32 = jnp.float32
    nrm = lambda shape, s: jax.random.normal(next(ks), shape, f32) * s
    D = D_MODEL
    base_logit = jnp.log(2.0 ** (5.0 + jnp.arange(RET_HEADS, dtype=f32)) - 1.0)
    return {
        'x': nrm((BATCH, SEQ, D), 1.0),
        'c': nrm((BATCH, D), 1.0),
        'ctx': nrm((BATCH, CTX_LEN, D), 1.0),
        'c_ctx': nrm((D,), 1.0),
        'ada_w': nrm((DEPTH, D, 6 * D), 0.5 * D ** -0.5),
        'ada_b': nrm((DEPTH, 6 * D), 0.01),
        'norm_mix': 1.0 + nrm((DEPTH, D), 0.05),
        'norm_ffn': 1.0 + nrm((DEPTH, D), 0.05),
        'norm_final': 1.0 + nrm((D,), 0.05),
        'ev_w_in': nrm((N_EVEN, D, EVEN_COLS), D ** -0.5),
        'ev_w_out': nrm((N_EVEN, MIX_DIM, D), MIX_DIM ** -0.5),
        'ret_gamma_logit': base_logit + nrm((N_EVEN, 2, RET_HEADS), 0.1),
        'ret_norm': 1.0 + nrm((N_EVEN, RET_DIM), 0.05),
        'gmlp_norm': 1.0 + nrm((N_EVEN, GMLP_DIM), 0.05),
        'gmlp_ws': nrm((N_EVEN, GMLP_GROUPS, GMLP_CHUNK, GMLP_CHUNK), GMLP_CHUNK ** -0.5),
        'gmlp_bs': 1.0 + nrm((N_EVEN, GMLP_GROUPS, GMLP_CHUNK), 0.1),
        'od_w_in': nrm((N_ODD, D, ODD_COLS), D ** -0.5),
        'od_w_out': nrm((N_ODD, MIX_DIM, D), MIX_DIM ** -0.5),
        'na_rpb': nrm((N_ODD, NA_HEADS, 2 * WIN_ROWS - 1, 2 * WIN_COLS - 1), 0.1),
        'gla_w_up': nrm((N_ODD, 2, GLA_GATE_RANK, GLA_QK_DIM), GLA_GATE_RANK ** -0.5),
        'gla_b_up': nrm((N_ODD, 2, GLA_QK_DIM), 0.1),
        'gla_norm': 1.0 + nrm((N_ODD, GLA_V_DIM), 0.05),
        'router_w': nrm((DEPTH, D, N_EXPERTS), D ** -0.5),
        'moe_w_gate': nrm((DEPTH, N_EXPERTS, D, D_EXPERT), D ** -0.5),
        'moe_w_up': nrm((DEPTH, N_EXPERTS, D, D_EXPERT), D ** -0.5),
        'moe_w_down': nrm((DEPTH, N_EXPERTS, D_EXPERT, D), D_EXPERT ** -0.5),
    }


def reference(x, c, ctx, c_ctx, ada_w, ada_b, norm_mix, norm_ffn, norm_final,
              ev_w_in, ev_w_out, ret_gamma_logit, ret_norm, gmlp_norm, gmlp_ws, gmlp_bs,
              od_w_in, od_w_out, na_rpb, gla_w_up, gla_b_up, gla_norm,
              router_w, moe_w_gate, moe_w_up, moe_w_down):
    for l in range(DEPTH):
        last = l == DEPTH - 1
        li = l // 2
        mod = jax.nn.silu(c) @ ada_w[l] + ada_b[l]
        sh1, sc1, g1, sh2, sc2, g2 = [m[:, None, :] for m in jnp.split(mod, 6, axis=-1)]
        n_ctx = 2 if last else 6
        mc = jnp.split(jax.nn.silu(c_ctx) @ ada_w[l][:, :n_ctx * D_MODEL] + ada_b[l][:n_ctx * D_MODEL], n_ctx)
        h = rmsnorm(x, norm_mix[l]) * (1.0 + sc1) + sh1
        hc = rmsnorm(ctx, norm_mix[l]) * (1.0 + mc[1]) + mc[0]
        if l % 2 == 0:
            yc, y = even_mixer(h, hc, ev_w_in[li], ev_w_out[li], ret_gamma_logit[li], ret_norm[li],
                               gmlp_norm[li], gmlp_ws[li], gmlp_bs[li], last)
        else:
            yc, y = odd_mixer(h, hc, od_w_in[li], od_w_out[li], na_rpb[li], gla_w_up[li], gla_b_up[li],
                              gla_norm[li], last)
        x = x + g1 * y
        h2 = rmsnorm(x, norm_ffn[l]) * (1.0 + sc2) + sh2
        x = x + g2 * expert_choice_ffn(h2, router_w[l], moe_w_gate[l], moe_w_up[l], moe_w_down[l])
        if not last:
            ctx = ctx + mc[2] * yc
            hc2 = rmsnorm(ctx, norm_ffn[l]) * (1.0 + mc[4]) + mc[3]
            ctx = ctx + mc[5] * expert_choice_ffn(hc2, router_w[l], moe_w_gate[l], moe_w_up[l], moe_w_down[l])
    return rmsnorm(x, norm_final)
```

```python
import math
import numpy as np
from contextlib import ExitStack
import concourse.bass as bass
import concourse.mybir as mybir
from concourse.bass_utils import run_bass_kernel_spmd

F32 = mybir.dt.float32
BF16 = mybir.dt.bfloat16
I32 = mybir.dt.int32
U32 = mybir.dt.uint32
ALU = mybir.AluOpType
AF = mybir.ActivationFunctionType
ENGS = ("pe", "act", "dve", "pool", "sp")
NCORES = 4
BIG = 60000.0
NEG = -30000.0


class Prog:
    def __init__(self):
        self.nc = bass.Bass("TRN2", target_bir_lowering=False)
        self.es = ExitStack()
        self.ops = []
        self.n_dma_sems = 8
        self.bufs = {}
        self.arena = None

    def dram(self, name, shape, dt, kind="Internal"):
        return self.nc.dram_tensor(name, list(shape), dt, kind=kind)

    ARENA_BYTES = 176 * 1024

    def sb(self, name, shape, dt):
        if name in self.bufs:
            return self.bufs[name]
        if self.arena is None:
            self.arena = self.es.enter_context(self.nc.sbuf_tensor("arena", [128, self.ARENA_BYTES // 4], F32))
            self.bump = 0
            self.scopes = []
        esz = mybir.dt.size(dt)
        n = 1
        for d in shape[1:]:
            n *= d
        nbytes = (n * esz + 31) // 32 * 32
        assert self.bump + nbytes <= self.ARENA_BYTES, (name, self.bump, nbytes)
        v = self.arena[0:shape[0], self.bump // 4:(self.bump + nbytes) // 4]
        if dt != F32:
            v = v.bitcast(dt)
        v = v[:, 0:n]
        if len(shape) == 3:
            v = v.rearrange("p (a b) -> p a b", a=shape[1])
        elif len(shape) == 4:
            v = v.rearrange("p (a b c) -> p a b c", a=shape[1], b=shape[2])
        self.bump += nbytes
        self.peak = max(getattr(self, "peak", 0), self.bump)
        self.bufs[name] = v
        if self.scopes:
            self.scopes[-1][1].append(name)
        return v

    def push(self):
        if self.arena is None:
            self.sb("_dummy", [128, 8], F32)
        self.scopes.append((self.bump, []))

    def pop(self):
        bump, names = self.scopes.pop()
        for n in names:
            del self.bufs[n]
        self.bump = bump
        self.ops.append(("*", None, (), (), "bar"))

    def ps(self, name, shape, dt=F32):
        if name not in self.bufs:
            self.bufs[name] = self.es.enter_context(self.nc.psum_tensor(name, list(shape), dt))
        return self.bufs[name]

    def op(self, eng, fn, reads=(), writes=(), dma=False):
        self.ops.append((eng, fn, tuple(reads), tuple(writes), dma))

    def dma(self, q, out, in_, reads=(), writes=(), **kw):
        self.op(q, lambda e: e.dma_start(out=out, in_=in_, **kw), reads, writes, dma=True)

    def emit(self):
        nc, es = self.nc, self.es
        sem_c = {e: es.enter_context(nc.semaphore("s_" + e)) for e in ENGS}
        sem_d = {e: [es.enter_context(nc.semaphore(f"d_{e}{i}")) for i in range(self.n_dma_sems)]
                 for e in ("sp", "act", "pool")}
        sem_cc = es.enter_context(nc.semaphore("s_cc"))
        cnt_cc = [0]
        cnt_c = {e: 0 for e in ENGS}
        cnt_d = {e: [0] * self.n_dma_sems for e in sem_d}
        rr = {e: 0 for e in sem_d}
        last_w, readers = {}, {}
        known = {e: {} for e in ENGS}
        streams = {e: [] for e in ENGS}
        for (eng, fn, reads, writes, is_dma) in self.ops:
            if is_dma == "bar":
                alltok = [(sem_c[e], cnt_c[e]) for e in ENGS if cnt_c[e]]
                alltok += [(sem_d[e][k], cnt_d[e][k] * 16) for e in sem_d for k in range(self.n_dma_sems) if cnt_d[e][k]]
                if cnt_cc[0]:
                    alltok.append((sem_cc, cnt_cc[0]))
                for e in ENGS:
                    need = [(s_, v_) for (s_, v_) in alltok if known[e].get(id(s_), 0) < v_]
                    for (s_, v_) in need:
                        known[e][id(s_)] = v_
                    if need:
                        streams[e].append((need, None, None, 0))
                last_w, readers = {}, {}
                continue
            toks = []
            for r in reads:
                if r in last_w:
                    toks.append(last_w[r])
            for w in writes:
                if w in last_w:
                    toks.append(last_w[w])
                toks.extend(readers.get(w, ()))
            if is_dma == "cc":
                sem = sem_cc
                cnt_cc[0] += 1
                tok = (sem, cnt_cc[0])
                inc = 1
            elif is_dma:
                k = rr[eng]
                rr[eng] = (k + 1) % self.n_dma_sems
                sem = sem_d[eng][k]
                if cnt_d[eng][k] > 0:
                    toks.append((sem, cnt_d[eng][k] * 16))
                cnt_d[eng][k] += 1
                tok = (sem, cnt_d[eng][k] * 16)
                inc = 16
            else:
                sem = sem_c[eng]
                cnt_c[eng] += 1
                tok = (sem, cnt_c[eng])
                inc = 1
            need = {}
            for (s, v) in toks:
                if known[eng].get(id(s), 0) >= v:
                    continue
                if need.get(id(s), (None, 0))[1] < v:
                    need[id(s)] = (s, v)
            for (s, v) in need.values():
                known[eng][id(s)] = v
            streams[eng].append((list(need.values()), fn, sem, inc))
            for r in reads:
                readers.setdefault(r, []).append(tok)
            for w in writes:
                last_w[w] = tok
                readers[w] = []
        fin = []
        for e in ENGS:
            if cnt_c[e]:
                fin.append((sem_c[e], cnt_c[e]))
        for e in sem_d:
            for k in range(self.n_dma_sems):
                if cnt_d[e][k]:
                    fin.append((sem_d[e][k], cnt_d[e][k] * 16))
        if cnt_cc[0]:
            fin.append((sem_cc, cnt_cc[0]))
        self.n_instr = {e: len(streams[e]) for e in ENGS}
        self.n_instr['peak_sbuf'] = getattr(self, 'peak', 0)
        with nc.Block() as block:
            def mk(e):
                def body(engine):
                    for (waits, fn, sem, inc) in streams[e]:
                        for (s, v) in waits:
                            engine.wait_ge(s, v)
                        if fn is not None:
                            fn(engine).then_inc(sem, inc)
                    if e == "sp":
                        for (s, v) in fin:
                            engine.wait_ge(s, v)
                return body
            block.tensor(mk("pe"))
            block.scalar(mk("act"))
            block.vector(mk("dve"))
            block.gpsimd(mk("pool"))
            block.sync(mk("sp"))
        self.es.close()
        return nc


def make_cfg(T=4096, L=256, D=2048, F=2048, E=16, depth=2, debug=()):
    c = dict(T=T, L=L, D=D, F=F, E=E, depth=depth, debug=tuple(debug))
    c["NT"], c["NCT"] = T // 128, L // 128
    c["NTT"] = c["NT"] + c["NCT"]
    c["M"] = T + L
    c["HD"] = 128
    c["RH"] = D // 2 // 128
    c["RD"] = c["RH"] * 128
    c["GD"] = D - c["RD"]
    c["GG"] = 8
    c["GW"] = c["GD"] // 8
    c["NAH"] = D // 2 // 128
    c["NAD"] = c["NAH"] * 128
    c["GH"] = 8
    c["GDV"] = (D - c["NAD"]) // 8
    c["GDK"] = c["GDV"] // 2
    c["GQK"] = 8 * c["GDK"]
    c["GV"] = 8 * c["GDV"]
    c["EVC"] = 4 * c["RD"] + 2 * c["GD"]
    c["ODQ"] = c["NAD"] + c["GQK"] + c["GV"]
    c["ODC"] = c["ODQ"] + 2 * c["NAD"] + c["GQK"] + c["GV"] + 32
    c["capL"] = 2 * T // E
    c["capC"] = 2 * L // E
    c["rows"] = T // 64
    return c


def big_layout(c):
    D, F, E = c["D"], c["F"], c["E"]
    items = []
    for l in range(c["depth"]):
        items.append((f"ada{l}", D, 6 * D))
        items.append((f"win{l}", D, c["EVC"] if l % 2 == 0 else c["ODC"]))
        items.append((f"wout{l}", D, D))
        for e in range(E):
            items.append((f"wg{l}_{e}", D, F))
            items.append((f"wu{l}_{e}", D, F))
            items.append((f"wd{l}_{e}", F, D))
    off, table = 0, {}
    for (n, K, N) in items:
        table[n] = (off, K, N)
        off += K * N
    CH = 8 * 2048 * 16
    tot = (off + CH - 1) // CH * CH
    return table, tot


class K:
    def __init__(self, c):
        self.c = c
        self.P = Prog()
        self.nc = self.P.nc
        self.dbg = {}

    def D_(self, name, shape, dt, kind=None):
        if kind is None:
            kind = "ExternalOutput" if name in self.c["debug"] else "Internal"
        t = self.P.dram(name, shape, dt, kind)
        if kind == "ExternalOutput":
            self.dbg[name] = t
        return t.ap()

    def bcreg(self, eng, val):
        if not hasattr(self, "_bcregs"):
            self._bcregs = {}
        if val not in self._bcregs:
            self._bcregs[val] = eng.to_reg(val)
        return self._bcregs[val]

    def nkt(self):
        return min(5, self.c["NT"])

    def accb(self, i):
        return self.pp[i // 2][:, (i % 2) * 512:(i % 2) * 512 + 512]

    def tp(self, i):
        return self.pp[2][:].bitcast(BF16)[:, i * 1024:(i + 1) * 1024]

    def linear(self, tag, tiles, Kd, N, producer, W, epilogue, G=4, wkey=()):
        P = self.P
        KC = Kd // 128
        ident = self.ident
        NB = (N + 511) // 512
        ngrp = (len(tiles) + G - 1) // G
        for g in range(ngrp):
            grp = tiles[g * G:(g + 1) * G]
            xT = P.sb("lin_xT", [128, 16, 5 * 128], BF16)
            for j, (mt, rows) in enumerate(grp):
                i = g * G + j
                xb = P.sb(f"lin_xbf{i % 2}", [128, 2048], BF16)
                xkey = ("xbf", i % 2)
                producer(i, mt, rows, xb, xkey)
                for kc in range(KC):
                    tpi = (i * KC + kc) % 2
                    tp = self.tp(tpi)
                    P.op("pe", lambda e, tp=tp, xb=xb, kc=kc, rows=rows: e.transpose(
                        out=tp[:, 0:rows], in_=xb[0:rows, kc * 128:(kc + 1) * 128], identity=ident[0:rows, 0:rows]),
                        reads=[xkey, "ident"], writes=[("tp", tpi)])
                    ce = "act" if kc % 2 == 0 else "dve"
                    if ce == "act":
                        P.op("act", lambda e, tp=tp, kc=kc, j=j, rows=rows: e.copy(
                            out=xT[:, kc, j * 128:j * 128 + rows], in_=tp[:, 0:rows]),
                            reads=[("tp", tpi)], writes=[("xT", kc, j)])
                    else:
                        P.op("dve", lambda e, tp=tp, kc=kc, j=j, rows=rows: e.tensor_copy(
                            out=xT[:, kc, j * 128:j * 128 + rows], in_=tp[:, 0:rows]),
                            reads=[("tp", tpi)], writes=[("xT", kc, j)])
            for nb in range(NB):
                ncols = min(512, N - nb * 512)
                wi = self.wcnt % 2
                self.wcnt += 1
                wb = P.sb(f"lin_wb{wi}", [128, 16, 512], BF16)
                for hf in range((KC + 7) // 8):
                    k0, k1 = hf * 8, min(KC, hf * 8 + 8)
                    si = self.scnt % 2
                    self.scnt += 1
                    wst = P.sb(f"lin_wst{si}", [128, 8, 512], F32)
                    src = W[k0 * 128:k1 * 128, nb * 512:nb * 512 + ncols].rearrange("(c p) n -> p c n", p=128)
                    P.dma("sp", wst[:, 0:k1 - k0, 0:ncols], src, reads=list(wkey), writes=[("wst", si)])
                    ce = ("pool", "act")[self.scnt % 2] if self.c.get("cast2") else "pool"
                    if ce == "pool":
                        P.op("pool", lambda e, wb=wb, wst=wst, k0=k0, k1=k1, ncols=ncols: e.tensor_copy(
                            out=wb[:, k0:k1, 0:ncols], in_=wst[:, 0:k1 - k0, 0:ncols]),
                            reads=[("wst", si)], writes=[("wb", wi, hf)])
                    else:
                        P.op("act", lambda e, wb=wb, wst=wst, k0=k0, k1=k1, ncols=ncols: e.copy(
                            out=wb[:, k0:k1, 0:ncols], in_=wst[:, 0:k1 - k0, 0:ncols]),
                            reads=[("wst", si)], writes=[("wb", wi, hf)])
                for j, (mt, rows) in enumerate(grp):
                    i = g * G + j
                    ai = self.acnt % 4
                    self.acnt += 1
                    acc = self.accb(ai)
                    for kc in range(KC):
                        P.op("pe", lambda e, acc=acc, kc=kc, j=j, rows=rows, wb=wb, ncols=ncols: e.matmul(
                            out=acc[0:rows, 0:ncols], lhsT=xT[:, kc, j * 128:j * 128 + rows], rhs=wb[:, kc, 0:ncols],
                            start=(kc == 0), stop=(kc == KC - 1)),
                            reads=[("xT", kc, j), ("wb", wi, kc // 8)], writes=[("acc", ai)])
                    epilogue(i, mt, rows, nb, ncols, acc, ("acc", ai))

    def setup_consts(self):
        P = self.P
        self.wcnt = self.scnt = self.acnt = 0
        identf = P.sb("identf", [128, 128], F32)
        ident = P.sb("ident", [128, 128], BF16)
        P.op("pool", lambda e: e.memset(identf[:], 0.0), writes=["identf"])
        P.op("pool", lambda e: e.affine_select(out=identf[:], in_=identf[:], pattern=[[-1, 128]],
                                               compare_op=ALU.not_equal, fill=1.0, base=0, channel_multiplier=1),
             reads=["identf"], writes=["identf"])
        P.op("dve", lambda e: e.tensor_copy(out=ident[:], in_=identf[:]), reads=["identf"], writes=["ident"])
        self.ident, self.identf = ident, identf
        self.pp = [P.ps(f"pp{i}", [128, 1024], F32) for i in range(4)]
        self.consts_attn()

    def build(self):
        c, P, nc = self.c, self.P, self.nc
        D, M, T, L = c["D"], c["M"], c["T"], c["L"]
        table, tot = big_layout(c)
        self.table = table
        EI = lambda n, s, dt=F32: P.dram(n, s, dt, "ExternalInput").ap()
        self.x_in = EI("x", [T, D])
        self.ctx_in = EI("ctx", [L, D])
        self.cc_in = EI("cc", [2, D])
        self.small = {}
        for l in range(c["depth"]):
            self.small[f"ada_b{l}"] = EI(f"ada_b{l}", [1, 6 * D])
            self.small[f"norm_mix{l}"] = EI(f"norm_mix{l}", [1, D])
            self.small[f"norm_ffn{l}"] = EI(f"norm_ffn{l}", [1, D])
            self.small[f"router{l}"] = EI(f"router{l}", [D, c["E"]])
        self.small["norm_final"] = EI("norm_final", [1, D])
        for l in range(c["depth"]):
            if l % 2 == 0:
                self.small[f"gamma{l}"] = EI(f"gamma{l}", [1, 2 * c["RH"]])
                self.small[f"ret_norm{l}"] = EI(f"ret_norm{l}", [1, c["RD"]])
                self.small[f"gmlp_norm{l}"] = EI(f"gmlp_norm{l}", [1, c["GD"]])
                self.small[f"gmlp_wsT{l}"] = EI(f"gmlp_wsT{l}", [8, 128, 128])
                self.small[f"gmlp_bsT{l}"] = EI(f"gmlp_bsT{l}", [128, 8])
            else:
                self.small[f"gla_wup{l}"] = EI(f"gla_wup{l}", [2, 16, c["GQK"]])
                self.small[f"gla_bup{l}"] = EI(f"gla_bup{l}", [1, 2, c["GQK"]])
                self.small[f"gla_norm{l}"] = EI(f"gla_norm{l}", [1, c["GV"]])
                self.small[f"na_bias{l}"] = EI(f"na_bias{l}", [6, c["NAH"], 128, self.nkt() * 128])
        self.small["rot_cs"] = EI("rot_cs", [T, 128])
        self.small["rot_sn"] = EI("rot_sn", [T, 128])
        self.out = P.dram("out", [T, D], F32, "ExternalOutput").ap()
        self.setup_consts()
        E, F = c["E"], c["F"]
        self.W = {}
        for l in range(c["depth"]):
            self.W[f"ada{l}"] = EI(f"ada{l}", [D, 6 * D])
            self.W[f"win{l}"] = EI(f"win{l}", [D, c["EVC"] if l % 2 == 0 else c["ODC"]])
            self.W[f"wout{l}"] = EI(f"wout{l}", [D, D])
            wg = EI(f"wg{l}", [E * D, F]); wu = EI(f"wu{l}", [E * D, F]); wd = EI(f"wd{l}", [E * F, D])
            for e in range(E):
                self.W[f"wg{l}_{e}"] = wg[e * D:(e + 1) * D, :]
                self.W[f"wu{l}_{e}"] = wu[e * D:(e + 1) * D, :]
                self.W[f"wd{l}_{e}"] = wd[e * F:(e + 1) * F, :]
        self.Wm = lambda name: self.W[name]
        self.XC = self.D_("XC", [M, D], F32)
        self.X1 = self.D_("X1", [M, D], F32)
        for mt in range(c["NTT"]):
            src = self.x_in[mt * 128:(mt + 1) * 128, :] if mt < c["NT"] else \
                self.ctx_in[(mt - c["NT"]) * 128:(mt - c["NT"] + 1) * 128, :]
            P.dma("sp", self.XC[mt * 128:(mt + 1) * 128, :], src, writes=[("XC", mt)])
        self.phase_mod()
        for l in range(c["depth"]):
            self.layer(l)
            if f"stop_l{l}" in c["debug"]:
                break
        self.phase_final()
        return P.emit()

    def phase_mod(self):
        c, P = self.c, self.P
        D = c["D"]
        self.MOD = []
        P.push()
        ccs = P.sb("S5", [128, 2048], F32)
        P.op("pool", lambda e: e.memset(ccs[:], 0.0), writes=["S5"])
        P.dma("sp", ccs[0:2, 0:D], self.cc_in[:, :], reads=["S5"], writes=["S5"])
        for l in range(c["depth"]):
            MODl = self.D_(f"MOD{l}", [2, 6 * D], F32)
            self.MOD.append(MODl)
            bias = None

            def prod(i, mt, rows, xb, xkey):
                P.op("act", lambda e: e.activation(out=xb[:, 0:D], in_=ccs[:, 0:D], func=AF.Silu),
                     reads=["S5"], writes=[xkey])

            def epi(i, mt, rows, nb, ncols, acc, akey, l=l, MODl=MODl, bias=bias):
                st = P.sb("mod_st", [2, 512], F32)
                bst = P.sb("mod_bst", [2, 512], F32)
                bsrc = self.small[f"ada_b{l}"][0:1, nb * 512:nb * 512 + ncols]
                P.dma("sp", bst[0:1, 0:ncols], bsrc, writes=["mod_bst0"])
                P.dma("sp", bst[1:2, 0:ncols], bsrc, writes=["mod_bst1"])
                P.op("dve", lambda e: e.tensor_tensor(out=st[0:2, 0:ncols], in0=acc[0:2, 0:ncols],
                                                      in1=bst[0:2, 0:ncols], op=ALU.add),
                     reads=[akey, "mod_bst0", "mod_bst1"], writes=["mod_st"])
                P.dma("sp", MODl[:, nb * 512:nb * 512 + ncols], st[0:2, 0:ncols], reads=["mod_st"],
                      writes=[("MOD", l, nb)])
            self.linear(f"mod{l}", [(0, 128)], D, 6 * D, prod, self.Wm(f"ada{l}"), epi)
        P.pop()

    def load_modvec(self, name, l, row, seg, gain=None, plus1=False):
        c, P = self.c, self.P
        D = c["D"]
        t = P.sb(name, [128, 2048], F32)
        src = self.MOD[l][row:row + 1, seg * D:(seg + 1) * D]
        rk = [("MOD", l, nb) for nb in range(seg * D // 512, (seg + 1) * D // 512)]
        P.dma("sp", t[:, 0:D], src.partition_broadcast(128), reads=rk, writes=[name])
        if gain is not None:
            g = P.sb("S4", [128, 2048], F32)
            P.dma("sp", g[:, 0:D], self.small[gain].partition_broadcast(128), writes=["S4"])
            P.op("dve", lambda e: e.scalar_tensor_tensor(out=t[:, 0:D], in0=t[:, 0:D], scalar=1.0 if plus1 else 0.0, in1=g[:, 0:D],
                                                         op0=ALU.add, op1=ALU.mult),
                 reads=[name, "S4"], writes=[name])
        return t

    def norm_tile(self, src_ap, src_keys, A, Akey, Bv, Bkey, out_bf, out_key, xname="S5"):
        c, P = self.c, self.P
        D = c["D"]
        xt = P.sb(xname, [128, 2048], F32)
        junk = P.sb("nt_junk", [128, 2048], BF16)
        ss = P.sb("nt_ss", [128, 1], F32)
        rs = P.sb("nt_rs", [128, 1], F32)
        P.dma("sp", xt[:, 0:D], src_ap, reads=src_keys, writes=[xname])
        P.op("act", lambda e: e.activation(out=junk[:, 0:D], in_=xt[:, 0:D], func=AF.Square, accum_out=ss[:]),
             reads=[xname], writes=["nt_junk", "nt_ss"])
        P.op("dve", lambda e: e.tensor_scalar(out=rs[:], in0=ss[:], scalar1=1.0 / D, scalar2=1e-6,
                                              op0=ALU.mult, op1=ALU.add), reads=["nt_ss"], writes=["nt_rs"])
        P.op("act", lambda e: e.activation(out=rs[:], in_=rs[:], func=AF.Sqrt), reads=["nt_rs"], writes=["nt_rs"])
        P.op("dve", lambda e: e.reciprocal(out=rs[:], in_=rs[:]), reads=["nt_rs"], writes=["nt_rs"])
        P.op("dve", lambda e: e.scalar_tensor_tensor(out=xt[:, 0:D], in0=xt[:, 0:D], scalar=rs[:, 0:1], in1=A[:, 0:D],
                                                     op0=ALU.mult, op1=ALU.mult),
             reads=[xname, "nt_rs", Akey], writes=[xname])
        P.op("pool", lambda e: e.tensor_tensor(out=out_bf[:, 0:D], in0=xt[:, 0:D], in1=Bv[:, 0:D], op=ALU.add),
             reads=[xname, Bkey], writes=[out_key])

    def layer(self, l):
        c, P = self.c, self.P
        D, M = c["D"], c["M"]
        last = (l == c["depth"] - 1)
        even = (l % 2 == 0)
        NCOL = c["EVC"] if even else c["ODC"]
        PB = self.D_(f"PB{l}", [M, NCOL], BF16)
        self.PB = PB
        P.push()
        A1 = self.load_modvec("S0", l, 0, 1, gain=f"norm_mix{l}", plus1=True)
        B1 = self.load_modvec("S1", l, 0, 0)
        A1c = self.load_modvec("S2", l, 1, 1, gain=f"norm_mix{l}", plus1=True)
        B1c = self.load_modvec("S3", l, 1, 0)
        tiles = [(mt, 128) for mt in range(c["NTT"])]

        def prod(i, mt, rows, xb, xkey):
            isc = mt >= c["NT"]
            self.norm_tile(self.XC[mt * 128:(mt + 1) * 128, :], [("XC", mt)],
                           A1c if isc else A1, "S2" if isc else "S0",
                           B1c if isc else B1, "S3" if isc else "S1", xb, xkey)

        def epi(i, mt, rows, nb, ncols, acc, akey):
            oi = self.ocnt % 2
            self.ocnt += 1
            st = P.sb(f"p1_st{oi}", [128, 512], BF16)
            P.op("act", lambda e: e.copy(out=st[:, 0:ncols], in_=acc[:, 0:ncols]), reads=[akey],
                 writes=[("p1_st", oi)])
            P.dma("sp", PB[mt * 128:(mt + 1) * 128, nb * 512:nb * 512 + ncols], st[:, 0:ncols],
                  reads=[("p1_st", oi)], writes=[("PB", mt, nb)])
        self.ocnt = 0
        self.linear(f"win{l}", tiles, D, NCOL, prod, self.Wm(f"win{l}"), epi)
        P.pop()
        if "stop_p1" in c["debug"] or f"stop_p1_{l}" in c["debug"]:
            return
        self.MIX = self.D_(f"MIX{l}", [M, D], BF16)
        for fn in ((lambda: self.gmlp(l, last)), (lambda: self.decay_attn(l, "ret", last))) if even else \
                ((lambda: self.nattn(l)), (lambda: self.decay_attn(l, "gla", last))):
            if (not even) and (("nattn" in fn.__code__.co_names and "skip_na" in c["debug"]) or ("decay_attn" in fn.__code__.co_names and "skip_gla" in c["debug"])):
                continue
            P.push()
            fn()
            P.pop()
        if "stop_p2" in c["debug"] or f"stop_p2_{l}" in c["debug"]:
            return
        ntl = c["NT"] if last else c["NTT"]
        tiles = [(mt, 128) for mt in range(ntl)]
        P.push()
        G1 = self.load_modvec("S0", l, 0, 2)
        G1c = self.load_modvec("S1", l, 1, 2) if not last else None
        X1 = self.X1
        mixkeys = lambda mt: [("MIX", mt, k_, s_) for (k_, s_) in ((("gmlp", 0), ("ret", 0)) if even else (("na", 0), ("gla", 0)))]

        def prod3(i, mt, rows, xb, xkey):
            P.dma("sp", xb[:, 0:D], self.MIX[mt * 128:(mt + 1) * 128, :], reads=mixkeys(mt), writes=[xkey])

        def epi3(i, mt, rows, nb, ncols, acc, akey):
            oi = self.ocnt % 2
            self.ocnt += 1
            xt = P.sb(f"p3_x{oi}", [128, 512], F32)
            g = G1c if mt >= c["NT"] else G1
            gk = "S1" if mt >= c["NT"] else "S0"
            P.dma("sp", xt[:, 0:ncols], self.XC[mt * 128:(mt + 1) * 128, nb * 512:nb * 512 + ncols], reads=[("XC", mt)], writes=[("p3_x", oi)])
            st = P.sb(f"p3_st{oi}", [128, 512], F32)
            P.op("dve", lambda e: e.tensor_tensor(out=st[:, 0:ncols], in0=acc[:, 0:ncols], in1=g[:, nb * 512:nb * 512 + ncols], op=ALU.mult),
                 reads=[akey, gk], writes=[("p3_st", oi)])
            P.op("pool", lambda e: e.tensor_tensor(out=st[:, 0:ncols], in0=st[:, 0:ncols], in1=xt[:, 0:ncols], op=ALU.add),
                 reads=[("p3_st", oi), ("p3_x", oi)], writes=[("p3_st", oi)])
            P.dma("sp", X1[mt * 128:(mt + 1) * 128, nb * 512:nb * 512 + ncols], st[:, 0:ncols], reads=[("p3_st", oi)], writes=[("X1", mt, nb)])
        self.linear(f"wout{l}", tiles, D, D, prod3, self.Wm(f"wout{l}"), epi3)
        P.pop()
        if "stop_p3" in c["debug"] or f"stop_p3_{l}" in c["debug"]:
            return
        self.moe(l, last, tiles)

    def moe(self, l, last, tiles):
        c, P = self.c, self.P
        D, M, E, F, NT = c["D"], c["M"], c["E"], c["F"], c["NT"]
        self.consts_attn()
        capL, capC = c["capL"], (0 if last else c["capC"])
        SLOTS = capL + capC
        H2 = self.D_(f"H2_{l}", [M, D], BF16)
        AFF = self.D_(f"AFF{l}", [M, E], F32)
        P.push()
        affT = P.sb("moe_affT", [16, M], F32)
        idxT = P.sb("moe_idxT", [128, 34, 16], I32)
        wT = P.sb("moe_wT", [128, 34, 16], F32)
        P.push()
        A2 = self.load_modvec("S0", l, 0, 4, gain=f"norm_ffn{l}", plus1=True)
        B2 = self.load_modvec("S1", l, 0, 3)
        if not last:
            A2c = self.load_modvec("S2", l, 1, 4, gain=f"norm_ffn{l}", plus1=True)
            B2c = self.load_modvec("S3", l, 1, 3)
        x1keys = lambda mt: [("X1", mt, nb) for nb in range(D // 512)]

        def prod(i, mt, rows, xb, xkey):
            isc = mt >= NT
            self.norm_tile(self.X1[mt * 128:(mt + 1) * 128, :], x1keys(mt), A2c if isc else A2, "S2" if isc else "S0",
                           B2c if isc else B2, "S3" if isc else "S1", xb, xkey)
            P.dma("sp", H2[mt * 128:(mt + 1) * 128, :], xb[:, 0:D], reads=[xkey], writes=[("H2", mt)])

        def epi(i, mt, rows, nb, ncols, acc, akey):
            lg = P.sb("moe_lg", [128, 16], F32)
            mx = P.sb("moe_mx", [128, 1], F32)
            sm = P.sb("moe_sm", [128, 1], F32)
            P.op("dve", lambda e: e.tensor_reduce(out=mx[:], in_=acc[:, 0:E], axis=mybir.AxisListType.X, op=ALU.max), reads=[akey], writes=["moe_mx"])
            P.op("dve", lambda e: e.tensor_scalar(out=mx[:], in0=mx[:], scalar1=-1.0, scalar2=None, op0=ALU.mult), reads=["moe_mx"], writes=["moe_mx"])
            P.op("act", lambda e: e.activation(out=lg[:, 0:E], in_=acc[:, 0:E], func=AF.Exp, bias=mx[:, 0:1], accum_out=sm[:]),
                 reads=[akey, "moe_mx"], writes=["moe_lg", "moe_sm"])
            P.op("dve", lambda e: e.reciprocal(out=sm[:], in_=sm[:]), reads=["moe_sm"], writes=["moe_sm"])
            P.op("dve", lambda e: e.tensor_scalar(out=lg[:, 0:E], in0=lg[:, 0:E], scalar1=sm[:, 0:1], scalar2=None, op0=ALU.mult),
                 reads=["moe_lg", "moe_sm"], writes=["moe_lg"])
            P.dma("sp", AFF[mt * 128:(mt + 1) * 128, :], lg[:, 0:E], reads=["moe_lg"], writes=[("AFF", mt)])
            tpf = self.pp[2]
            P.op("pe", lambda e: e.transpose(out=tpf[0:E, 0:128], in_=lg[:, 0:E], identity=self.identf[:, :]), reads=["moe_lg", "identf"], writes=[("tp", 0)])
            P.op("act", lambda e: e.copy(out=affT[0:E, mt * 128:(mt + 1) * 128], in_=tpf[0:E, 0:128]), reads=[("tp", 0)], writes=[("affT", mt)])
        self.linear(f"router{l}", tiles, D, E, prod, self.small[f"router{l}"], epi)
        P.pop()
        P.push()
        ones16 = P.sb("mo_ones", [16, 2048], F32)
        P.op("pool", lambda e: e.memset(ones16[:], 1.0), writes=["mo_ones"])
        work = P.sb("moe_work", [16, M], F32)
        m8 = P.sb("moe_m8", [16, 8], F32)
        idxf = P.sb("moe_idxf", [16, M], F32)
        segs = [(0, c["T"], capL, 0)] + ([] if last else [(c["T"], c["L"], capC, capL)])
        ntl = len(tiles)
        for (o0, n, cap, slot0) in segs:
            rk = [("affT", mt) for mt in range(o0 // 128, (o0 + n) // 128)]
            P.op("dve", lambda e, o0=o0, n=n: e.tensor_copy(out=work[0:E, o0:o0 + n], in_=affT[0:E, o0:o0 + n]), reads=rk, writes=["moe_work"])
            for it in range(cap // 8):
                P.op("dve", lambda e, o0=o0, n=n: e.max(out=m8[0:E, :], in_=work[0:E, o0:o0 + n]), reads=["moe_work"], writes=["moe_m8"])
                if it < cap // 8 - 1:
                    P.op("dve", lambda e, o0=o0, n=n: e.match_replace(out=work[0:E, o0:o0 + n], in_to_replace=m8[0:E, :], in_values=work[0:E, o0:o0 + n],
                                                                      imm_value=-1.0), reads=["moe_work", "moe_m8"], writes=["moe_work"])
            sel = work
            P.op("dve", lambda e, o0=o0, n=n: e.tensor_scalar(out=sel[0:E, o0:o0 + n], in0=affT[0:E, o0:o0 + n], scalar1=m8[0:E, 7:8], scalar2=None,
                                                              op0=ALU.is_ge), reads=rk + ["moe_m8", "moe_work"], writes=["moe_work"])
            for q0 in range(0, n, 2048):
                qn = min(2048, n - q0)
                init = 0.0 if q0 == 0 else idxf[0:E, o0 + q0 - 1:o0 + q0]
                P.op("dve", lambda e, a=o0 + q0, qn=qn, init=init: e.tensor_tensor_scan(out=idxf[0:E, a:a + qn], data0=ones16[0:E, 0:qn], data1=sel[0:E, a:a + qn],
                                                                                        initial=init, op0=ALU.mult, op1=ALU.add), reads=["moe_work", "mo_ones", "moe_idxf"], writes=["moe_idxf"])
            P.op("dve", lambda e, o0=o0, n=n, cap=cap: e.scalar_tensor_tensor(out=sel[0:E, o0:o0 + n], in0=idxf[0:E, o0:o0 + n], scalar=float(cap) + 0.5,
                                                                              in1=sel[0:E, o0:o0 + n], op0=ALU.is_le, op1=ALU.mult),
                 reads=["moe_work", "moe_idxf"], writes=["moe_work"])
            P.op("dve", lambda e, o0=o0, n=n, slot0=slot0: e.tensor_scalar(out=idxf[0:E, o0:o0 + n], in0=idxf[0:E, o0:o0 + n], scalar1=float(slot0 - 1) - BIG,
                                                                           scalar2=None, op0=ALU.add), reads=["moe_idxf"], writes=["moe_idxf"])
            P.op("dve", lambda e, o0=o0, n=n: e.tensor_tensor(out=idxf[0:E, o0:o0 + n], in0=idxf[0:E, o0:o0 + n], in1=sel[0:E, o0:o0 + n], op=ALU.mult),
                 reads=["moe_idxf", "moe_work"], writes=["moe_idxf"])
            P.op("dve", lambda e, o0=o0, n=n: e.tensor_scalar(out=idxf[0:E, o0:o0 + n], in0=idxf[0:E, o0:o0 + n], scalar1=BIG, scalar2=None, op0=ALU.add),
                 reads=["moe_idxf"], writes=["moe_idxf"])
        tpf = self.pp[2]
        for (mt, rows) in tiles:
            P.op("pe", lambda e, mt=mt: e.transpose(out=tpf[:, 0:E], in_=idxf[0:E, mt * 128:(mt + 1) * 128], identity=self.identf[0:E, 0:E]),
                 reads=["moe_idxf", "identf"], writes=[("tp", 0)])
            P.op("pe", lambda e, mt=mt: e.transpose(out=tpf[:, 512:512 + E], in_=affT[0:E, mt * 128:(mt + 1) * 128], identity=self.identf[0:E, 0:E]),
                 reads=[("affT", mt), "identf"], writes=[("tp", 1)])
            P.op("dve", lambda e, mt=mt: e.tensor_copy(out=idxT[:, mt, :], in_=tpf[:, 0:E]), reads=[("tp", 0)], writes=[("idxT", mt)])
            P.op("dve", lambda e, mt=mt: e.tensor_scalar(out=wT[:, mt, :], in0=tpf[:, 0:E], scalar1=BIG / 2, scalar2=None, op0=ALU.is_lt),
                 reads=[("tp", 0)], writes=[("wT", mt)])
            P.op("dve", lambda e, mt=mt: e.tensor_tensor(out=wT[:, mt, :], in0=wT[:, mt, :], in1=tpf[:, 512:512 + E], op=ALU.mult),
                 reads=[("wT", mt), ("tp", 1)], writes=[("wT", mt)])
        P.pop()
        P.push()
        XSe = [self.D_(f"XS{l}_{ex}", [SLOTS, D], BF16) for ex in range(E)]
        YSe = [self.D_(f"YS{l}_{ex}", [SLOTS, D], F32) for ex in range(E)]
        for (mt, rows) in tiles:
            hb = P.sb(f"lin_xbf{mt % 2}", [128, 2048], BF16)
            hk = ("xbf", mt % 2)
            P.dma("sp", hb[:, 0:D], H2[mt * 128:(mt + 1) * 128, :], reads=[("H2", mt)], writes=[hk])
            for ex in range(E):
                P.op("pool", lambda e, mt=mt, ex=ex, hb=hb: e.indirect_dma_start(
                    out=XSe[ex][:, :], out_offset=bass.IndirectOffsetOnAxis(ap=idxT[:, mt, ex:ex + 1], axis=0),
                    in_=hb[:, 0:D], in_offset=None, bounds_check=self.bcreg(e, SLOTS - 1), oob_is_err=False),
                    reads=[hk, ("idxT", mt)], writes=[("XS", ex, mt)], dma=True)
        P.pop()
        P.push()
        stiles = [(j, min(128, SLOTS - j * 128)) for j in range((SLOTS + 127) // 128)]
        HS = self.D_(f"HS{l}", [SLOTS, F], BF16)
        GA = self.D_(f"GA{l}", [SLOTS, F], BF16)
        for ex in range(E):
            xsk = [("XS", ex, mt) for (mt, _) in tiles]

            def prodx(i, j, rows, xb, xkey, ex=ex):
                P.dma("sp", xb[0:rows, 0:D], XSe[ex][j * 128:j * 128 + rows, :], reads=xsk, writes=[xkey])

            def epig(i, j, rows, nb, ncols, acc, akey, ex=ex):
                oi = self.ocnt % 2
                self.ocnt += 1
                st = P.sb(f"p1_st{oi}", [128, 512], BF16)
                P.op("act", lambda e: e.activation(out=st[0:rows, 0:ncols], in_=acc[0:rows, 0:ncols], func=AF.Silu), reads=[akey], writes=[("p1_st", oi)])
                P.dma("sp", GA[j * 128:j * 128 + rows, nb * 512:nb * 512 + ncols], st[0:rows, 0:ncols], reads=[("p1_st", oi)], writes=[("GA", j, nb)])

            def epiu(i, j, rows, nb, ncols, acc, akey, ex=ex):
                oi = self.ocnt % 2
                self.ocnt += 1
                ga = P.sb(f"p1_st{oi}", [128, 512], BF16)
                st = P.sb(f"mo_hs{oi}", [128, 512], BF16)
                P.dma("sp", ga[0:rows, 0:ncols], GA[j * 128:j * 128 + rows, nb * 512:nb * 512 + ncols], reads=[("GA", j, nb)], writes=[("p1_st", oi)])
                P.op("dve", lambda e: e.tensor_tensor(out=st[0:rows, 0:ncols], in0=acc[0:rows, 0:ncols], in1=ga[0:rows, 0:ncols], op=ALU.mult),
                     reads=[akey, ("p1_st", oi)], writes=[("mo_hs", oi)])
                P.dma("sp", HS[j * 128:j * 128 + rows, nb * 512:nb * 512 + ncols], st[0:rows, 0:ncols], reads=[("mo_hs", oi)], writes=[("HS", j, nb)])

            def prodh(i, j, rows, xb, xkey):
                P.dma("sp", xb[0:rows, 0:F], HS[j * 128:j * 128 + rows, :], reads=[("HS", j, nb) for nb in range((F + 511) // 512)], writes=[xkey])

            def epid(i, j, rows, nb, ncols, acc, akey, ex=ex):
                oi = self.ocnt % 2
                self.ocnt += 1
                st = P.sb(f"p3_st{oi}", [128, 512], F32)
                P.op("act", lambda e: e.copy(out=st[0:rows, 0:ncols], in_=acc[0:rows, 0:ncols]), reads=[akey], writes=[("p3_st", oi)])
                P.dma("sp", YSe[ex][j * 128:j * 128 + rows, nb * 512:nb * 512 + ncols], st[0:rows, 0:ncols],
                      reads=[("p3_st", oi)], writes=[("YS", ex, j, nb)])
            self.linear(f"g{l}_{ex}", stiles, D, F, prodx, self.Wm(f"wg{l}_{ex}"), epig, G=len(stiles))
            self.linear(f"u{l}_{ex}", stiles, D, F, prodx, self.Wm(f"wu{l}_{ex}"), epiu, G=len(stiles))
            self.linear(f"d{l}_{ex}", stiles, F, D, prodh, self.Wm(f"wd{l}_{ex}"), epid, G=len(stiles))
        P.pop()
        P.push()
        G2 = self.load_modvec("S0", l, 0, 5)
        G2c = self.load_modvec("S1", l, 1, 5) if not last else None
        for (mt, rows) in tiles:
            isc = mt >= NT
            accs = P.sb("S2", [128, 2048], F32)
            x1t = P.sb("S3", [128, 2048], F32)
            P.dma("sp", x1t[:, 0:D], self.X1[mt * 128:(mt + 1) * 128, :], reads=x1keys(mt), writes=["S3"])
            P.op("pool", lambda e: e.memset(accs[:, 0:D], 0.0), writes=["S2"])
            for ex in range(E):
                bi = ex % 2
                buf = P.sb(f"S{4 + bi}", [128, 2048], F32)
                if mt == tiles[0][0] and ex < 2:
                    P.op("pool", lambda e, buf=buf: e.memset(buf[:, 0:D], 0.0), writes=[f"S{4 + bi}"])
                yk = [("YS", ex, j, nb) for (j, _) in stiles for nb in range(D // 512)]
                P.op("pool", lambda e, mt=mt, ex=ex, buf=buf: e.indirect_dma_start(
                    out=buf[:, 0:D], out_offset=None, in_=YSe[ex][:, :],
                    in_offset=bass.IndirectOffsetOnAxis(ap=idxT[:, mt, ex:ex + 1], axis=0), bounds_check=self.bcreg(e, SLOTS - 1), oob_is_err=False),
                    reads=yk + [("idxT", mt)], writes=[f"S{4 + bi}"], dma=True)
                P.op("dve", lambda e, mt=mt, ex=ex, buf=buf: e.scalar_tensor_tensor(out=accs[:, 0:D], in0=buf[:, 0:D], scalar=wT[:, mt, ex:ex + 1], in1=accs[:, 0:D],
                                                                                    op0=ALU.mult, op1=ALU.add), reads=[f"S{4 + bi}", ("wT", mt), "S2"], writes=["S2"])
            g = G2c if isc else G2
            P.op("pool", lambda e, g=g: e.tensor_tensor(out=accs[:, 0:D], in0=accs[:, 0:D], in1=g[:, 0:D], op=ALU.mult), reads=["S2", "S1" if isc else "S0"], writes=["S2"])
            P.op("dve", lambda e: e.tensor_tensor(out=accs[:, 0:D], in0=accs[:, 0:D], in1=x1t[:, 0:D], op=ALU.add), reads=["S2", "S3"], writes=["S2"])
            P.dma("sp", self.XC[mt * 128:(mt + 1) * 128, :], accs[:, 0:D], reads=["S2"], writes=[("XC", mt)])
        P.pop()
        P.pop()

    def consts_attn(self):
        P = self.P
        if hasattr(self, "maskF"):
            return
        ones = P.sb("c_ones", [128, 128], F32)
        self.maskF = P.sb("c_maskF", [128, 128], F32)
        self.maskB = P.sb("c_maskB", [128, 128], F32)
        P.op("pool", lambda e: e.memset(ones[:], 1.0), writes=["c_ones"])
        P.op("pool", lambda e: e.affine_select(out=self.maskF[:], in_=ones[:], pattern=[[1, 128]], compare_op=ALU.is_ge,
                                               fill=0.0, base=0, channel_multiplier=-1), reads=["c_ones"], writes=["c_maskF", "c_maskB"])
        P.op("pool", lambda e: e.affine_select(out=self.maskB[:], in_=ones[:], pattern=[[-1, 128]], compare_op=ALU.is_ge,
                                               fill=0.0, base=0, channel_multiplier=1), reads=["c_ones"], writes=["c_maskF", "c_maskB"])
        self.ones = ones
        pi = P.sb("c_pi", [128, 1], I32)
        pf = P.sb("c_pf", [128, 8], F32)
        P.op("pool", lambda e: e.iota(out=pi[:], pattern=[[0, 1]], base=0, channel_multiplier=1), writes=["c_pi"])
        P.op("dve", lambda e: e.tensor_copy(out=pf[:, 0:1], in_=pi[:]), reads=["c_pi"], writes=["c_pf"])
        for j, (m, a) in enumerate([(1.0, 1.0), (-1.0, -1.0), (1.0, -127.0), (-1.0, 128.0), (1.0, -128.0), (-1.0, 0.0)]):
            P.op("dve", lambda e, j=j, m=m, a=a: e.tensor_scalar(out=pf[:, j + 1:j + 2], in0=pf[:, 0:1], scalar1=m, scalar2=a,
                                                               op0=ALU.mult, op1=ALU.add), reads=["c_pf"], writes=["c_pf"])
        self.pf = pf

    def decay_attn(self, l, kind, last):
        c, P = self.c, self.P
        PB = self.PB
        NT, NTT = c["NT"], c["NTT"]
        C = 128
        if kind == "ret":
            H, dk, dv = c["RH"], 128, 128
            qo, go, ko, vo = 0, c["RD"], 2 * c["RD"] + 2 * c["GD"], 3 * c["RD"] + 2 * c["GD"]
            qscale = 128 ** -0.5
            mixo = 0
            gnorm_name = f"ret_norm{l}"
            NCOLS = c["EVC"]
        else:
            H, dk, dv = 8, c["GDK"], c["GDV"]
            qo = c["NAD"]
            go = c["NAD"] + c["GQK"]
            ko = c["ODQ"] + 2 * c["NAD"]
            vo = ko + c["GQK"]
            lro = vo + c["GV"]
            qscale = dk ** -0.5
            mixo = c["NAD"]
            gnorm_name = f"gla_norm{l}"
            NCOLS = c["ODC"]
        hp = 128 // dk
        npk = H // hp
        HK, HV = H * dk, H * dv
        lnq = math.log(qscale)
        OF = self.D_(f"OF{l}", [c["M"], HV], F32)
        lat_units = list(range(NT))
        ctx_units = list(range(NT, NTT))
        gn = P.sb("at_gn", [128, 1024], F32)
        P.dma("sp", gn[:, 0:HV], self.small[gnorm_name].partition_broadcast(128), writes=["at_gn"])
        S32 = P.sb("at_S32", [128, 1024], F32)
        Sbf = P.sb("at_Sbf", [128, 1024], BF16)
        qt = P.sb("at_q", [128, 1024], BF16)
        kt = P.sb("at_k", [128, 1024], BF16)
        vt = P.sb("at_v", [128, 1024], BF16)
        gt = P.sb("at_g", [128, 1024], BF16)
        qin = P.sb("at_qin", [128, 1024], BF16)
        kin = P.sb("at_kin", [128, 1024], BF16)
        kout = P.sb("at_kout", [128, 1024], BF16)
        qT = P.sb("at_qT", [128, 8, 128], BF16)
        kT = P.sb("at_kT", [128, 8, 128], BF16)
        attT = P.sb("at_attT", [128, 8, 128], BF16)
        osb = P.sb("S4", [128, 2048], F32)
        ofl = P.sb("S5", [128, 2048], F32)
        pf = self.pf
        if hp > 1:
            rowm = P.sb("at_rowm", [128, 4], F32)
            P.op("pool", lambda e: e.memset(rowm[:, :], 0.0), writes=["at_rowm"])
            for j in range(hp):
                P.op("pool", lambda e, j=j: e.memset(rowm[j * dk:(j + 1) * dk, j:j + 1], 1.0), reads=["at_rowm"], writes=["at_rowm"])
        if kind == "ret":
            gl = P.sb("at_gl", [128, 16], F32)
            P.dma("sp", gl[:, 0:2 * H], self.small[f"gamma{l}"].partition_broadcast(128), writes=["at_gl"])
            P.op("act", lambda e: e.activation(out=gl[:, 0:2 * H], in_=gl[:, 0:2 * H], func=AF.Exp, scale=-1.0),
                 reads=["at_gl"], writes=["at_gl"])
            P.op("act", lambda e: e.activation(out=gl[:, 0:2 * H], in_=gl[:, 0:2 * H], func=AF.Ln, bias=1.0),
                 reads=["at_gl"], writes=["at_gl"])
            tb = P.sb("at_tb", [128, 2, 4, 8], F32)
            for d in range(2):
                sp_d = gl[:, d * H:(d + 1) * H]
                cols = [(2, lnq), (1, 0.0), (3, 0.0)] if d == 0 else [(5, lnq), (4, 0.0), (6, 0.0)]
                for j, (pc, bias) in enumerate(cols):
                    P.op("act", lambda e, d=d, j=j, pc=pc, bias=bias, sp_d=sp_d: e.activation(
                        out=tb[:, d, j, 0:H], in_=sp_d, func=AF.Exp, scale=pf[:, pc:pc + 1], bias=bias),
                        reads=["at_gl", "c_pf"], writes=["at_tb"])
                P.op("act", lambda e, d=d, sp_d=sp_d: e.activation(out=tb[:, d, 3, 0:H], in_=sp_d, func=AF.Exp, scale=-128.0),
                     reads=["at_gl"], writes=["at_tb"])
            rcs = P.sb("at_rcs", [128, 128], F32)
            rsn = P.sb("at_rsn", [128, 128], F32)
        else:
            lrT = P.sb("at_lrT", [128, 128], BF16)
            wup = P.sb("at_wup", [128, 2, 512], F32)
            wuph = P.sb("at_wuph", [128, 2, 512], BF16)
            wupl = P.sb("at_wupl", [128, 2, 512], BF16)
            P.op("pool", lambda e: e.memset(lrT[:, :], 1.0), writes=["at_lrT"])
            P.op("pool", lambda e: e.memset(wup[:, :, :], 0.0), writes=["at_wup"])
            P.dma("sp", wup[0:16, :, 0:HK], self.small[f"gla_wup{l}"].rearrange("d r n -> r d n"), reads=["at_wup"], writes=["at_wup"])
            P.dma("sp", wup[16:17, :, 0:HK], self.small[f"gla_bup{l}"], reads=["at_wup"], writes=["at_wup"])
            P.op("dve", lambda e: e.tensor_copy(out=wuph[:, :, :], in_=wup[:, :, :]), reads=["at_wup"], writes=["at_wuph"])
            P.op("dve", lambda e: e.tensor_tensor(out=wupl[:, :, :], in0=wup[:, :, :], in1=wuph[:, :, :], op=ALU.subtract),
                 reads=["at_wup", "at_wuph"], writes=["at_wupl"])
            sph = P.sb("at_sph", [128, 512], BF16)
            spl = P.sb("at_spl", [128, 512], BF16)
            sp = P.sb("at_sp", [128, 512], F32)
            bsb = P.sb("at_bsb", [128, 512], F32)
            EQ = P.sb("at_EQ", [128, 512], F32)
            EKI = P.sb("at_EKI", [128, 512], F32)
            EKO = P.sb("at_EKO", [128, 512], F32)
            dec = P.sb("at_dec", [128, 4, 2], F32)
            LmF = P.sb("at_LmF", [128, 128], BF16)
            LmB = P.sb("at_LmB", [128, 128], BF16)
            Em = P.sb("at_Em", [128, 128], BF16)
            nsc = P.sb("at_nsc", [128, 2], BF16)
            P.op("dve", lambda e: e.tensor_scalar(out=LmF[:], in0=self.maskF[:, :], scalar1=-1.0 / 16, scalar2=None,
                                                  op0=ALU.mult), reads=["c_maskF"], writes=["at_LmF"])
            P.op("dve", lambda e: e.tensor_scalar(out=LmB[:], in0=self.maskB[:, :], scalar1=-1.0 / 16, scalar2=None,
                                                  op0=ALU.mult), reads=["c_maskB"], writes=["at_LmB"])
            P.op("pool", lambda e: e.memset(Em[:], -1.0 / 16), writes=["at_Em"])
            P.op("pool", lambda e: e.memset(nsc[:], -1.0 / 16), writes=["at_nsc"])

        if kind == "gla":
            SPD = self.D_(f"SPD{l}", [2 * c["M"], HK], F32)
            for mt in range(NTT):
                r0 = mt * 128
                pk = [("PB", mt, nb) for nb in range((NCOLS + 511) // 512)]
                P.dma("sp", gt[:, 0:32], PB[r0:r0 + C, lro:lro + 32], reads=pk, writes=["at_g"])
                for d in range(2):
                    tpb = self.tp(0)
                    P.op("pe", lambda e, d=d, tpb=tpb: e.transpose(out=tpb[0:16, 0:C], in_=gt[:, d * 16:(d + 1) * 16], identity=self.ident[:, :]),
                         reads=["at_g", "ident"], writes=[("tp", 0)])
                    P.op("act", lambda e, tpb=tpb: e.copy(out=lrT[0:16, :], in_=tpb[0:16, 0:C]), reads=[("tp", 0)], writes=["at_lrT"])
                    zps = self.accb(2)
                    P.op("pe", lambda e, d=d: e.matmul(out=zps[:, 0:HK], lhsT=lrT[:, :], rhs=wuph[:, d, 0:HK], start=True, stop=False),
                         reads=["at_lrT", "at_wuph"], writes=[("acc", 2)])
                    P.op("pe", lambda e, d=d: e.matmul(out=zps[:, 0:HK], lhsT=lrT[:, :], rhs=wupl[:, d, 0:HK], start=False, stop=True),
                         reads=["at_lrT", "at_wupl", ("acc", 2)], writes=[("acc", 2)])
                    P.op("act", lambda e: e.activation(out=sp[:, 0:HK], in_=zps[:, 0:HK], func=AF.Exp, scale=-1.0),
                         reads=[("acc", 2)], writes=["at_sp"])
                    P.op("act", lambda e: e.activation(out=sp[:, 0:HK], in_=sp[:, 0:HK], func=AF.Ln, bias=1.0),
                         reads=["at_sp"], writes=["at_sp"])
                    P.dma("sp", SPD[d * c["M"] + r0:d * c["M"] + r0 + C, :], sp[:, 0:HK], reads=["at_sp"], writes=[("SPD", d, mt)])
            P.ops.append(("*", None, (), (), "bar"))
            if c.get("gla_cut") == 1:
                return

        for d in range(2):
            order = (ctx_units + lat_units) if d == 0 else (ctx_units[::-1] + lat_units[::-1])
            mask = self.maskF if d == 0 else self.maskB
            mkey = "c_maskF" if d == 0 else "c_maskB"
            P.op("pool", lambda e: e.memset(S32[:], 0.0), writes=["at_S32"])
            P.op("pool", lambda e: e.memset(Sbf[:], 0.0), writes=["at_Sbf"])
            for mt in order:
                isc = mt >= NT
                r0 = mt * 128
                need_out = (not isc) or (not last)
                pk = [("PB", mt, nb) for nb in range((NCOLS + 511) // 512)]
                P.dma("sp", kt[:, 0:HK], PB[r0:r0 + C, ko:ko + HK], reads=pk, writes=["at_k"])
                P.dma("sp", vt[:, 0:HV], PB[r0:r0 + C, vo:vo + HV], reads=pk, writes=["at_v"])
                if need_out:
                    P.dma("sp", qt[:, 0:HK], PB[r0:r0 + C, qo:qo + HK], reads=pk, writes=["at_q"])
                qsrc, ksrc, qk_, kk_ = qt, kt, "at_q", "at_k"
                if kind == "ret":
                    if not isc:
                        P.dma("sp", rcs[:], self.small["rot_cs"][r0:r0 + 128, :], writes=["at_rcs"])
                        P.dma("sp", rsn[:], self.small["rot_sn"][r0:r0 + 128, :], writes=["at_rsn"])
                        for (src, skey, dstname) in ((qt, "at_q", "S0"), (kt, "at_k", "S1")):
                            dst = P.sb(dstname, [128, 2048], F32)
                            t1 = dst[:, 0:HK].rearrange("p (h x) -> p h x", h=H)
                            t2 = dst[:, 1024:1024 + HK].rearrange("p (h a b x) -> p h a b x", h=H, a=2, b=2)
                            sv = src[:, 0:HK].rearrange("p (h x) -> p h x", h=H)
                            sv5 = src[:, 0:HK].rearrange("p (h a b x) -> p h a b x", h=H, a=2, b=2)
                            csb = rcs[:].unsqueeze(1).broadcast_to([128, H, 128])
                            sn5 = rsn[:].rearrange("p (a b x) -> p a b x", a=2, b=2)
                            P.op("dve", lambda e, t1=t1, sv=sv, csb=csb: e.tensor_tensor(out=t1, in0=sv, in1=csb, op=ALU.mult),
                                 reads=[skey, "at_rcs"], writes=[dstname])
                            for b_ in range(2):
                                snb = sn5[:, :, b_, :].unsqueeze(1).broadcast_to([128, H, 2, 32])
                                P.op("pool", lambda e, t2=t2, sv5=sv5, snb=snb, b_=b_: e.tensor_tensor(
                                    out=t2[:, :, :, b_, :], in0=sv5[:, :, :, 1 - b_, :], in1=snb, op=ALU.mult),
                                    reads=[skey, "at_rsn"], writes=[dstname + "b"])
                            P.op("dve", lambda e, dst=dst: e.tensor_tensor(out=dst[:, 0:HK], in0=dst[:, 0:HK],
                                                                           in1=dst[:, 1024:1024 + HK], op=ALU.add),
                                 reads=[dstname, dstname + "b"], writes=[dstname])
                        qsrc, ksrc, qk_, kk_ = P.sb("S0", [128, 2048], F32), P.sb("S1", [128, 2048], F32), "S0", "S1"
                    bq = lambda j, d=d: tb[:, d, j, 0:H].unsqueeze(2).broadcast_to([128, H, dk])
                    tq, tki, tko = bq(0), bq(1), bq(2)
                    v3 = lambda t: t[:, 0:HK].rearrange("p (h x) -> p h x", h=H)
                    tkeys = ["at_tb"]
                    decap = tb[:, d, 3, 0:H]
                    dkeys = ["at_tb"]
                else:
                    P.dma("sp", sp[:, 0:HK], SPD[d * c["M"] + r0:d * c["M"] + r0 + C, :], reads=[("SPD", d, mt)], writes=["at_sp"])
                    if c.get("gla_cut") == 21:
                        continue
                    Lm = LmF if d == 0 else LmB
                    bps, eps_ = self.accb(2), self.accb(3)
                    P.op("dve", lambda e: e.tensor_copy(out=sph[:, 0:HK], in_=sp[:, 0:HK]), reads=["at_sp"], writes=["at_sph"])
                    P.op("dve", lambda e: e.tensor_tensor(out=spl[:, 0:HK], in0=sp[:, 0:HK], in1=sph[:, 0:HK], op=ALU.subtract),
                         reads=["at_sp", "at_sph"], writes=["at_spl"])
                    for (dst_, akey_, lhs_, lk_) in ((bps, ("acc", 2), Lm, ["at_LmF", "at_LmB"]), (eps_, ("acc", 3), Em, ["at_Em"])):
                        P.op("pe", lambda e, dst_=dst_, lhs_=lhs_: e.matmul(out=dst_[:, 0:HK], lhsT=lhs_[:, :], rhs=sph[:, 0:HK], start=True, stop=False),
                             reads=["at_sph"] + lk_, writes=[akey_])
                        P.op("pe", lambda e, dst_=dst_, lhs_=lhs_: e.matmul(out=dst_[:, 0:HK], lhsT=lhs_[:, :], rhs=spl[:, 0:HK], start=False, stop=True),
                             reads=["at_spl", akey_] + lk_, writes=[akey_])
                    if c.get("gla_cut") == 22:
                        continue
                    P.op("act", lambda e: e.activation(out=EQ[:, 0:HK], in_=bps[:, 0:HK], func=AF.Exp, bias=lnq),
                         reads=[("acc", 2)], writes=["at_EQ"])
                    P.op("act", lambda e: e.activation(out=EKI[:, 0:HK], in_=bps[:, 0:HK], func=AF.Exp, scale=-1.0),
                         reads=[("acc", 2)], writes=["at_EKI"])
                    if c.get("gla_cut") == 23:
                        continue
                    P.op("act", lambda e: e.activation(out=bsb[:, 0:HK], in_=eps_[:, 0:HK], func=AF.Exp), reads=[("acc", 3)], writes=["at_bsb"])
                    P.op("dve", lambda e: e.tensor_tensor(out=EKO[:, 0:HK], in0=bsb[:, 0:HK], in1=EKI[:, 0:HK], op=ALU.mult),
                         reads=["at_bsb", "at_EKI"], writes=["at_EKO"])
                    if c.get("gla_cut") == 2:
                        continue
                    dps = self.pp[2]
                    for p_ in range(npk):
                        P.op("pe", lambda e, p_=p_: e.matmul(out=dps[:, 512 + 2 * p_:512 + 2 * p_ + 2], lhsT=sph[:, p_ * 128:(p_ + 1) * 128], rhs=nsc[:, 0:2],
                                                             start=True, stop=False), reads=["at_sph", "at_nsc"], writes=[("tp", 1)])
                        P.op("pe", lambda e, p_=p_: e.matmul(out=dps[:, 512 + 2 * p_:512 + 2 * p_ + 2], lhsT=spl[:, p_ * 128:(p_ + 1) * 128], rhs=nsc[:, 0:2],
                                                             start=False, stop=True), reads=["at_spl", "at_nsc", ("tp", 1)], writes=[("tp", 1)])
                    P.op("act", lambda e: e.activation(out=dec[:, 0:npk, :], in_=dps[:, 512:512 + 2 * npk].rearrange("p (h x) -> p h x", h=npk), func=AF.Exp),
                         reads=[("tp", 1)], writes=["at_dec"])
                    if c.get("gla_cut") == 3:
                        continue
                    v3 = lambda t: t[:, 0:HK]
                    tq, tki, tko = EQ[:, 0:HK], EKI[:, 0:HK], EKO[:, 0:HK]
                    tkeys = ["at_EQ", "at_EKI", "at_EKO"]
                    decap = dec[:, 0:npk, 0]
                    dkeys = ["at_dec"]
                if need_out:
                    P.op("dve", lambda e, qsrc=qsrc, tq=tq, v3=v3: e.tensor_tensor(out=v3(qin), in0=v3(qsrc), in1=tq, op=ALU.mult),
                         reads=[qk_] + tkeys, writes=["at_qin"])
                    P.op("pool", lambda e, ksrc=ksrc, tki=tki, v3=v3: e.tensor_tensor(out=v3(kin), in0=v3(ksrc), in1=tki, op=ALU.mult),
                         reads=[kk_] + tkeys, writes=["at_kin"])
                P.op("dve", lambda e, ksrc=ksrc, tko=tko, v3=v3: e.tensor_tensor(out=v3(kout), in0=v3(ksrc), in1=tko, op=ALU.mult),
                     reads=[kk_] + tkeys, writes=["at_kout"])
                if c.get("gla_cut") == 4 and kind == "gla":
                    continue
                if need_out:
                    for (src, skey, dstT, dkey, tpi) in ((qin, "at_qin", qT, "at_qT", 0), (kin, "at_kin", kT, "at_kT", 1)):
                        tp = self.tp(tpi)
                        for p_ in range(npk):
                            P.op("pe", lambda e, tp=tp, src=src, p_=p_: e.transpose(out=tp[:, p_ * 128:(p_ + 1) * 128],
                                                                                   in_=src[:, p_ * 128:(p_ + 1) * 128], identity=self.ident[:, :]),
                                 reads=[skey, "ident"], writes=[("tp", tpi)])
                        tpv = tp[:, 0:npk * 128].rearrange("p (h x) -> p h x", h=npk)
                        if hp == 1:
                            P.op("act", lambda e, tpv=tpv, dstT=dstT: e.copy(out=dstT[:, 0:H, :], in_=tpv), reads=[("tp", tpi)], writes=[dkey])
                        else:
                            for j in range(hp):
                                P.op("act", lambda e, tpv=tpv, dstT=dstT, j=j: e.activation(out=dstT[:, j:H:hp, :], in_=tpv, func=AF.Copy, scale=rowm[:, j:j + 1]),
                                     reads=[("tp", tpi), "at_rowm"], writes=[dkey + str(j)])
                    if c.get("gla_cut") == 5 and kind == "gla":
                        continue
                    tkq = ["at_qT"] + [f"at_qT{j}" for j in range(hp)]
                    tkk = ["at_kT"] + [f"at_kT{j}" for j in range(hp)]
                    nbh = 4
                    for bk in range((H + nbh - 1) // nbh):
                        acc = self.accb(bk)
                        hs = list(range(bk * nbh, min(H, (bk + 1) * nbh)))
                        for h in hs:
                            P.op("pe", lambda e, acc=acc, h=h, bk=bk: e.matmul(out=acc[:, (h - bk * nbh) * C:(h - bk * nbh + 1) * C],
                                                                               lhsT=kT[:, h, :], rhs=qT[:, h, :], start=True, stop=True),
                                 reads=tkq + tkk, writes=[("acc", bk)])
                        P.op("dve", lambda e, acc=acc, hs=hs, mask=mask: e.tensor_tensor(
                            out=attT[:, hs[0]:hs[-1] + 1, :], in0=acc[:, 0:len(hs) * C].rearrange("p (h x) -> p h x", h=len(hs)),
                            in1=mask[:, :].unsqueeze(1).broadcast_to([C, len(hs), C]), op=ALU.mult),
                            reads=[("acc", bk), mkey], writes=["at_attT"])
                    if c.get("gla_cut") == 6 and kind == "gla":
                        continue
                    ops_ = self.pp[3]
                    for h in range(H):
                        P.op("pe", lambda e, h=h: e.matmul(out=ops_[:, h * dv:(h + 1) * dv], lhsT=attT[:, h, :],
                                                           rhs=vt[:, h * dv:(h + 1) * dv], start=True, stop=False),
                             reads=["at_attT", "at_v"], writes=["pp3"])
                        P.op("pe", lambda e, h=h: e.matmul(out=ops_[:, h * dv:(h + 1) * dv], lhsT=qT[:, h, :],
                                                           rhs=Sbf[:, (h // hp) * dv:(h // hp + 1) * dv], start=False, stop=True),
                             reads=tkq + ["at_Sbf", "pp3"], writes=["pp3"])
                if c.get("gla_cut") == 7 and kind == "gla":
                    continue
                kvp = self.pp[1]
                for h in range(H):
                    P.op("pe", lambda e, h=h: e.matmul(out=kvp[:, h * dv:(h + 1) * dv], lhsT=kout[:, (h // hp) * 128:(h // hp + 1) * 128],
                                                       rhs=vt[:, h * dv:(h + 1) * dv], start=True, stop=True),
                         reads=["at_kout", "at_v"], writes=[("acc", 2), ("acc", 3)])
                for j in range(hp):
                    rr = slice(j * dk, (j + 1) * dk)
                    s3 = S32[rr, 0:npk * dv].rearrange("p (h x) -> p h x", h=npk)
                    k3 = kvp[rr, 0:HV].rearrange("p (a b x) -> p a b x", a=npk, b=hp)[:, :, j, :]
                    P.op("dve", lambda e, s3=s3, decap=decap, rr=rr: e.tensor_tensor(out=s3, in0=s3, in1=decap[rr, :].unsqueeze(2).broadcast_to([dk, npk, dv]),
                                                                                    op=ALU.mult), reads=["at_S32"] + dkeys, writes=["at_S32"])
                    P.op("dve", lambda e, s3=s3, k3=k3: e.tensor_tensor(out=s3, in0=k3, in1=s3, op=ALU.add),
                         reads=["at_S32", ("acc", 2), ("acc", 3)], writes=["at_S32"])
                P.op("act", lambda e: e.copy(out=Sbf[:, 0:npk * dv], in_=S32[:, 0:npk * dv]), reads=["at_S32", "pp3"], writes=["at_Sbf"])
                if not need_out:
                    continue
                okey = ("OF", mt)
                if d == 0:
                    P.op("act", lambda e: e.copy(out=osb[:, 0:HV], in_=self.pp[3][:, 0:HV]), reads=["pp3"], writes=["S4"])
                    P.dma("sp", OF[r0:r0 + C, :], osb[:, 0:HV], reads=["S4"], writes=[okey])
                    continue
                P.dma("sp", ofl[:, 0:HV], OF[r0:r0 + C, :], reads=[okey], writes=["S5"])
                P.dma("sp", gt[:, 0:HV], PB[r0:r0 + C, go:go + HV], reads=pk, writes=["at_g"])
                P.op("dve", lambda e: e.tensor_tensor(out=osb[:, 0:HV], in0=self.pp[3][:, 0:HV], in1=ofl[:, 0:HV], op=ALU.add),
                     reads=["pp3", "S5"], writes=["S4"])
                if f"OFB{l}" in c["debug"]:
                    if not hasattr(self, "OFB"):
                        self.OFB = self.D_(f"OFB{l}", [c["M"], HV], F32)
                    P.dma("sp", self.OFB[r0:r0 + C, :], osb[:, 0:HV], reads=["S4"], writes=[("OFB", mt)])
                sq = ofl
                ss = P.sb("at_ss", [128, 8], F32)
                P.op("pool", lambda e: e.tensor_tensor(out=sq[:, 0:HV], in0=osb[:, 0:HV], in1=osb[:, 0:HV], op=ALU.mult),
                     reads=["S4"], writes=["S5"])
                P.op("dve", lambda e: e.tensor_reduce(out=ss[:, 0:H], in_=sq[:, 0:HV].rearrange("p (h x) -> p h x", h=H),
                                                      axis=mybir.AxisListType.X, op=ALU.add), reads=["S5"], writes=["at_ss"])
                P.op("dve", lambda e: e.tensor_scalar(out=ss[:, 0:H], in0=ss[:, 0:H], scalar1=1.0 / dv, scalar2=1e-6,
                                                      op0=ALU.mult, op1=ALU.add), reads=["at_ss"], writes=["at_ss"])
                P.op("act", lambda e: e.activation(out=ss[:, 0:H], in_=ss[:, 0:H], func=AF.Sqrt), reads=["at_ss"], writes=["at_ss"])
                P.op("dve", lambda e: e.reciprocal(out=ss[:, 0:H], in_=ss[:, 0:H]), reads=["at_ss"], writes=["at_ss"])
                o3 = osb[:, 0:HV].rearrange("p (h x) -> p h x", h=H)
                P.op("dve", lambda e, o3=o3: e.tensor_tensor(out=o3, in0=o3, in1=ss[:, 0:H].unsqueeze(2).broadcast_to([C, H, dv]), op=ALU.mult),
                     reads=["S4", "at_ss"], writes=["S4"])
                P.op("pool", lambda e: e.tensor_tensor(out=osb[:, 0:HV], in0=osb[:, 0:HV], in1=gn[:, 0:HV], op=ALU.mult),
                     reads=["S4", "at_gn"], writes=["S4"])
                P.op("act", lambda e: e.activation(out=sq[:, 0:HV], in_=gt[:, 0:HV], func=AF.Silu), reads=["at_g"], writes=["S5"])
                ob = P.sb("at_ob", [128, 1024], BF16)
                P.op("dve", lambda e, ob=ob: e.tensor_tensor(out=ob[:, 0:HV], in0=osb[:, 0:HV], in1=sq[:, 0:HV], op=ALU.mult),
                     reads=["S4", "S5"], writes=["at_ob"])
                P.dma("sp", self.MIX[r0:r0 + C, mixo:mixo + HV], ob[:, 0:HV], reads=["at_ob"], writes=[("MIX", mt, kind, 0)])

    def nattn(self, l):
        c, P = self.c, self.P
        PB = self.PB
        NT, NTT, T, L = c["NT"], c["NTT"], c["T"], c["L"]
        NAH, NAD = c["NAH"], c["NAD"]
        nkt = self.nkt()
        Wk = nkt * 128
        qo, ko = 0, c["ODQ"]
        vo = ko + NAD
        pkeys = lambda mt: [("PB", mt, nb) for nb in range((c["ODC"] + 511) // 512)]
        QT = self.D_(f"QT{l}", [NAH, 128, T], BF16)
        KT = self.D_(f"KT{l}", [NAH, 128, c["M"]], BF16)
        cls_of, _ = na_geometry(c)
        for mt in range(NTT):
            for (which, off, dst, scale) in (("q", qo, QT, 128 ** -0.5), ("k", ko, KT, 1.0)):
                if which == "q" and mt >= NT:
                    continue
                src = P.sb(f"na_src{which}", [128, 1024], BF16)
                stg = P.sb(f"na_stg{which}", [128, 8, 128], BF16)
                tpi = 0 if which == "q" else 1
                tp = self.tp(tpi)
                P.dma("sp", src[:, 0:NAD], PB[mt * 128:(mt + 1) * 128, off:off + NAD], reads=pkeys(mt), writes=[f"na_src{which}"])
                for h in range(NAH):
                    P.op("pe", lambda e, tp=tp, src=src, h=h: e.transpose(out=tp[:, h * 128:(h + 1) * 128], in_=src[:, h * 128:(h + 1) * 128], identity=self.ident[:, :]),
                         reads=[f"na_src{which}", "ident"], writes=[("tp", tpi)])
                P.op("act", lambda e, tp=tp, stg=stg, scale=scale: e.activation(out=stg[:, 0:NAH, :], in_=tp[:, 0:NAH * 128].rearrange("p (h x) -> p h x", h=NAH),
                                                                              func=AF.Copy, scale=scale), reads=[("tp", tpi)], writes=[f"na_stg{which}"])
                P.dma("sp", dst[:, :, mt * 128:(mt + 1) * 128].rearrange("h d t -> d h t"), stg[:, 0:NAH, :], reads=[f"na_stg{which}"], writes=[(which + "T", mt)])
        vctx = P.sb("na_vctx", [128, c["NCT"], 1024], BF16)
        kctx = P.sb("na_kctx", [128, 8, L], BF16)
        for j in range(c["NCT"]):
            P.dma("sp", vctx[:, j, 0:NAD], PB[(NT + j) * 128:(NT + j + 1) * 128, vo:vo + NAD], reads=pkeys(NT + j), writes=[("na_vctx", j)])
        P.dma("sp", kctx[:, 0:NAH, :], KT[:, :, T:T + L].rearrange("h d t -> d h t"), reads=[("kT", NT + j) for j in range(c["NCT"])], writes=["na_kctx"])
        vwin = P.sb("na_vwin", [128, 5, 1024], BF16)
        kTh = P.sb("na_kTh", [128, 640], BF16)
        qTh = P.sb("na_qTh", [128, 128], BF16)
        bias = P.sb("na_bias", [128, 640], F32)
        sc = P.sb("na_sc", [128, 896], F32)
        pb = P.sb("na_pb", [128, 896], BF16)
        pT = P.sb("na_pT", [128, 7, 128], BF16)
        osb = P.sb("na_osb", [128, 1024], BF16)
        mx = P.sb("na_mx", [128, 1], F32)
        sm = P.sb("na_sm", [128, 1], F32)
        nj = nkt + c["NCT"]
        for mt in range(NT):
            kt0 = min(max(mt - 2, 0), NT - nkt)
            for j in range(nkt):
                P.dma("sp", vwin[:, j, 0:NAD], PB[(kt0 + j) * 128:(kt0 + j + 1) * 128, vo:vo + NAD], reads=pkeys(kt0 + j), writes=[("na_vwin", j)])
            for h in range(NAH):
                P.dma("sp", qTh[:, :], QT[h, :, mt * 128:(mt + 1) * 128], reads=[("qT", mt)], writes=["na_qTh"])
                P.dma("sp", kTh[:, 0:Wk], KT[h, :, kt0 * 128:kt0 * 128 + Wk], reads=[("kT", kt0 + j) for j in range(nkt)], writes=["na_kTh"])
                P.dma("sp", bias[:, 0:Wk], self.small[f"na_bias{l}"][cls_of[mt], h, :, :], writes=["na_bias"])
                sps = self.pp[0]
                for c0 in range(0, Wk, 512):
                    cn = min(512, Wk - c0)
                    P.op("pe", lambda e, c0=c0, cn=cn: e.matmul(out=sps[:, c0:c0 + cn], lhsT=qTh[:, :], rhs=kTh[:, c0:c0 + cn], start=True, stop=True),
                         reads=["na_qTh", "na_kTh"], writes=[("acc", c0 // 512)])
                P.op("pe", lambda e, h=h: e.matmul(out=sps[:, Wk:Wk + L], lhsT=qTh[:, :], rhs=kctx[:, h, :], start=True, stop=True),
                     reads=["na_qTh", "na_kctx", ("acc", 1)], writes=[("acc", 1)])
                P.op("dve", lambda e: e.tensor_tensor(out=sc[:, 0:Wk], in0=sps[:, 0:Wk], in1=bias[:, 0:Wk], op=ALU.add),
                     reads=[("acc", 0), ("acc", 1), "na_bias"], writes=["na_sc"])
                P.op("act", lambda e: e.copy(out=sc[:, Wk:Wk + L], in_=sps[:, Wk:Wk + L]), reads=[("acc", 1)], writes=["na_scc"])
                P.op("dve", lambda e: e.tensor_reduce(out=mx[:], in_=sc[:, 0:Wk + L], axis=mybir.AxisListType.X, op=ALU.max), reads=["na_sc", "na_scc"], writes=["na_mx"])
                P.op("dve", lambda e: e.tensor_scalar(out=mx[:], in0=mx[:], scalar1=-1.0, scalar2=None, op0=ALU.mult), reads=["na_mx"], writes=["na_mx"])
                P.op("act", lambda e: e.activation(out=pb[:, 0:Wk + L], in_=sc[:, 0:Wk + L], func=AF.Exp, bias=mx[:, 0:1], accum_out=sm[:]),
                     reads=["na_sc", "na_scc", "na_mx"], writes=["na_pb", "na_sm"])
                P.op("dve", lambda e: e.reciprocal(out=sm[:], in_=sm[:]), reads=["na_sm"], writes=["na_sm"])
                tp = self.tp(0)
                for j in range(nj):
                    P.op("pe", lambda e, j=j: e.transpose(out=tp[:, j * 128:(j + 1) * 128], in_=pb[:, j * 128:(j + 1) * 128], identity=self.ident[:, :]),
                         reads=["na_pb", "ident"], writes=[("tp", 0)])
                P.op("act", lambda e: e.copy(out=pT[:, 0:nj, :], in_=tp[:, 0:nj * 128].rearrange("p (j x) -> p j x", j=nj)), reads=[("tp", 0)], writes=["na_pT"])
                ops_ = self.accb(2)
                for j in range(nj):
                    rhs = vwin[:, j, h * 128:(h + 1) * 128] if j < nkt else vctx[:, j - nkt, h * 128:(h + 1) * 128]
                    rk = ("na_vwin", j) if j < nkt else ("na_vctx", j - nkt)
                    P.op("pe", lambda e, j=j, rhs=rhs: e.matmul(out=ops_[:, 0:128], lhsT=pT[:, j, :], rhs=rhs, start=(j == 0), stop=(j == nj - 1)),
                         reads=["na_pT", rk], writes=[("acc", 2)])
                P.op("dve", lambda e, h=h: e.tensor_scalar(out=osb[:, h * 128:(h + 1) * 128], in0=ops_[:, 0:128], scalar1=sm[:, 0:1], scalar2=None, op0=ALU.mult),
                     reads=[("acc", 2), "na_sm"], writes=[("na_osb", h)])
            P.dma("sp", self.MIX[mt * 128:(mt + 1) * 128, 0:NAD], osb[:, 0:NAD], reads=[("na_osb", h) for h in range(NAH)], writes=[("MIX", mt, "na", 0)])

    def gmlp(self, l, last):
        c, P = self.c, self.P
        PB = self.PB
        GD, GW, RD = c["GD"], c["GW"], c["RD"]
        uo, vo = 2 * RD, 2 * RD + GD
        wsT = P.sb("gm_wsT", [128, 8, 128], BF16)
        wsf = P.sb("S0", [128, 2048], F32)
        P.dma("sp", wsf[:, 0:1024].rearrange("p (g x) -> p g x", g=8), self.small[f"gmlp_wsT{l}"].rearrange("g s p -> s g p"), writes=["S0"])
        P.op("dve", lambda e: e.tensor_copy(out=wsT[:], in_=wsf[:, 0:1024].rearrange("p (g x) -> p g x", g=8)), reads=["S0"], writes=["gm_wsT"])
        bsT = P.sb("gm_bsT", [128, 8], F32)
        P.dma("sp", bsT[:], self.small[f"gmlp_bsT{l}"], writes=["gm_bsT"])
        gng = P.sb("S1", [128, 2048], F32)
        P.dma("sp", gng[:, 0:GD], self.small[f"gmlp_norm{l}"].partition_broadcast(128), writes=["S1"])
        ntl = c["NT"] if last else c["NTT"]
        K2 = 2 * math.sqrt(2.0 / math.pi)
        for mt in range(ntl):
            pk = [("PB", mt, nb) for nb in range(c["EVC"] // 512)]
            r0 = mt * 128
            raw = P.sb("at_q", [128, 1024], BF16)
            raw2 = P.sb("at_k", [128, 1024], BF16)
            P.dma("sp", raw[:, 0:GD], PB[r0:r0 + 128, uo:uo + GD], reads=pk, writes=["at_q"])
            P.dma("sp", raw2[:, 0:GD], PB[r0:r0 + 128, vo:vo + GD], reads=pk, writes=["at_k"])
            gl = {}
            for (src, skey, dname) in ((raw, "at_q", "S2"), (raw2, "at_k", "S3")):
                dst = P.sb(dname, [128, 2048], F32)
                a, b = dst[:, 0:GD], dst[:, 1024:1024 + GD]
                eng = "dve" if dname == "S2" else "pool"
                P.op(eng, lambda e, a=a, src=src: e.tensor_tensor(out=a, in0=src[:, 0:GD], in1=src[:, 0:GD], op=ALU.mult), reads=[skey], writes=[dname])
                P.op(eng, lambda e, a=a: e.tensor_scalar(out=a, in0=a, scalar1=0.044715, scalar2=1.0, op0=ALU.mult, op1=ALU.add), reads=[dname], writes=[dname])
                P.op(eng, lambda e, a=a, src=src: e.tensor_tensor(out=a, in0=a, in1=src[:, 0:GD], op=ALU.mult), reads=[dname, skey], writes=[dname])
                P.op("act", lambda e, a=a, b=b: e.activation(out=b, in_=a, func=AF.Sigmoid, scale=K2), reads=[dname], writes=[dname + "b"])
                P.op(eng, lambda e, a=a, b=b, src=src: e.tensor_tensor(out=a, in0=b, in1=src[:, 0:GD], op=ALU.mult), reads=[dname + "b", skey], writes=[dname])
                gl[dname] = a
            ug, vg = gl["S2"], gl["S3"]
            ss = P.sb("nt_ss", [128, 1], F32)
            rs = P.sb("nt_rs", [128, 1], F32)
            junk = P.sb("nt_junk", [128, 2048], BF16)
            P.op("act", lambda e: e.activation(out=junk[:, 0:GD], in_=vg, func=AF.Square, accum_out=ss[:]), reads=["S3"], writes=["nt_junk", "nt_ss"])
            P.op("dve", lambda e: e.tensor_scalar(out=rs[:], in0=ss[:], scalar1=1.0 / GD, scalar2=1e-6, op0=ALU.mult, op1=ALU.add), reads=["nt_ss"], writes=["nt_rs"])
            P.op("act", lambda e: e.activation(out=rs[:], in_=rs[:], func=AF.Sqrt), reads=["nt_rs"], writes=["nt_rs"])
            P.op("dve", lambda e: e.reciprocal(out=rs[:], in_=rs[:]), reads=["nt_rs"], writes=["nt_rs"])
            vn = P.sb("at_v", [128, 1024], BF16)
            P.op("dve", lambda e: e.scalar_tensor_tensor(out=vn[:, 0:GD], in0=vg, scalar=rs[:, 0:1], in1=gng[:, 0:GD], op0=ALU.mult, op1=ALU.mult),
                 reads=["S3", "nt_rs", "S1"], writes=["at_v"])
            ob = P.sb("at_ob", [128, 1024], BF16)
            for g in range(8):
                bank = g * GW // 512
                acc = self.accb(bank)
                col = g * GW - bank * 512
                P.op("pe", lambda e, acc=acc, g=g, col=col: e.matmul(out=acc[:, col:col + GW], lhsT=wsT[:, g, :], rhs=vn[:, g * GW:(g + 1) * GW], start=True, stop=True),
                     reads=["gm_wsT", "at_v"], writes=[("acc", bank)])
                P.op("dve", lambda e, acc=acc, g=g, col=col: e.scalar_tensor_tensor(out=ob[:, g * GW:(g + 1) * GW], in0=acc[:, col:col + GW], scalar=bsT[:, g:g + 1],
                                                                                    in1=ug[:, g * GW:(g + 1) * GW], op0=ALU.add, op1=ALU.mult),
                     reads=[("acc", bank), "gm_bsT", "S2"], writes=["at_ob"])
            P.dma("sp", self.MIX[r0:r0 + 128, RD:RD + GD], ob[:, 0:GD], reads=["at_ob"], writes=[("MIX", mt, "gmlp", 0)])

    def phase_final(self):
        c, P = self.c, self.P
        D = c["D"]
        P.push()
        g = P.sb("S0", [128, 2048], F32)
        P.dma("sp", g[:, 0:D], self.small["norm_final"].partition_broadcast(128), writes=["S0"])
        for mt in range(c["NT"]):
            i = mt % 2
            ob = P.sb(f"S{1 + i}", [128, 2048], F32)
            xt = P.sb("S5", [128, 2048], F32)
            junk = P.sb("nt_junk", [128, 2048], BF16)
            ss = P.sb("nt_ss", [128, 1], F32)
            rs = P.sb("nt_rs", [128, 1], F32)
            P.dma("sp", xt[:, 0:D], self.XC[mt * 128:(mt + 1) * 128, :], reads=[("XC", mt)], writes=["S5"])
            P.op("act", lambda e: e.activation(out=junk[:, 0:D], in_=xt[:, 0:D], func=AF.Square, accum_out=ss[:]),
                 reads=["S5"], writes=["nt_junk", "nt_ss"])
            P.op("dve", lambda e: e.tensor_scalar(out=rs[:], in0=ss[:], scalar1=1.0 / D, scalar2=1e-6,
                                                  op0=ALU.mult, op1=ALU.add), reads=["nt_ss"], writes=["nt_rs"])
            P.op("act", lambda e: e.activation(out=rs[:], in_=rs[:], func=AF.Sqrt), reads=["nt_rs"], writes=["nt_rs"])
            P.op("dve", lambda e: e.reciprocal(out=rs[:], in_=rs[:]), reads=["nt_rs"], writes=["nt_rs"])
            P.op("dve", lambda e, ob=ob: e.scalar_tensor_tensor(out=ob[:, 0:D], in0=xt[:, 0:D], scalar=rs[:, 0:1], in1=g[:, 0:D],
                                                                op0=ALU.mult, op1=ALU.mult),
                 reads=["S5", "nt_rs", "S0"], writes=[f"S{1 + i}"])
            P.dma("sp", self.out[mt * 128:(mt + 1) * 128, :], ob[:, 0:D], reads=[f"S{1 + i}"], writes=[("out", mt)])
        P.pop()


def na_geometry(c):
    NT, rows = c["NT"], c["rows"]
    nkt = min(5, NT)
    Wk = nkt * 128
    wr = min(8, rows)
    uniq, cls_of, maps = {}, [], []
    for mt in range(NT):
        kt0 = min(max(mt - 2, 0), NT - nkt)
        m = np.full((128, Wk), -1, np.int64)
        for p in range(128):
            t = mt * 128 + p
            r, col = t // 64, t % 64
            rstart = min(max(r - wr // 2, 0), rows - wr)
            cstart = min(max(col - 8, 0), 64 - 16)
            for r2 in range(rstart, rstart + wr):
                kk0 = r2 * 64 - kt0 * 128
                dr = r2 - r + 7
                for c2 in range(cstart, cstart + 16):
                    kk = kk0 + c2
                    assert 0 <= kk < Wk
                    m[p, kk] = dr * 31 + (c2 - col + 15)
        key = m.tobytes()
        if key not in uniq:
            uniq[key] = len(maps)
            maps.append(m)
        cls_of.append(uniq[key])
    return cls_of, maps


def host_inputs(c, inputs, ncores):
    f32 = lambda a: np.ascontiguousarray(a, dtype=np.float32)
    E, D, F = c["E"], c["D"], c["F"]
    shared = {"norm_final": f32(inputs["norm_final"])[None, :]}
    for l in range(c["depth"]):
        li = l // 2
        shared[f"ada{l}"] = f32(inputs["ada_w"][l])
        shared[f"win{l}"] = f32(inputs["ev_w_in"][li] if l % 2 == 0 else inputs["od_w_in"][li])
        shared[f"wout{l}"] = f32(inputs["ev_w_out"][li] if l % 2 == 0 else inputs["od_w_out"][li])
        shared[f"wg{l}"] = f32(inputs["moe_w_gate"][l]).reshape(E * D, F)
        shared[f"wu{l}"] = f32(inputs["moe_w_up"][l]).reshape(E * D, F)
        shared[f"wd{l}"] = f32(inputs["moe_w_down"][l]).reshape(E * F, D)
        shared[f"ada_b{l}"] = f32(inputs["ada_b"][l])[None, :]
        shared[f"norm_mix{l}"] = f32(inputs["norm_mix"][l])[None, :]
        shared[f"norm_ffn{l}"] = f32(inputs["norm_ffn"][l])[None, :]
        shared[f"router{l}"] = f32(inputs["router_w"][l])
        if l % 2 == 0:
            shared[f"gamma{l}"] = f32(inputs["ret_gamma_logit"][li]).reshape(1, -1)
            shared[f"ret_norm{l}"] = f32(inputs["ret_norm"][li])[None, :]
            shared[f"gmlp_norm{l}"] = f32(inputs["gmlp_norm"][li])[None, :]
            shared[f"gmlp_wsT{l}"] = f32(np.transpose(inputs["gmlp_ws"][li], (0, 2, 1)))
            shared[f"gmlp_bsT{l}"] = f32(np.transpose(inputs["gmlp_bs"][li], (1, 0)))
        else:
            shared[f"gla_wup{l}"] = f32(inputs["gla_w_up"][li])
            shared[f"gla_bup{l}"] = f32(inputs["gla_b_up"][li])[None]
            shared[f"gla_norm{l}"] = f32(inputs["gla_norm"][li])[None, :]
            cls_of, maps = na_geometry(c)
            rpb = f32(inputs["na_rpb"][li]).reshape(c["NAH"], -1)
            tabs = np.empty((6, c["NAH"], 128, maps[0].shape[1]), np.float32)
            tabs[:] = NEG
            for ci, m in enumerate(maps):
                valid = m >= 0
                for h in range(c["NAH"]):
                    tabs[ci, h][valid] = rpb[h][m[valid]]
            shared[f"na_bias{l}"] = tabs
    T = c["T"]
    t = np.arange(T)
    freq = (10000.0 ** (-np.arange(32, dtype=np.float32) / 32)).astype(np.float32)
    angr = ((t // 64).astype(np.float32)[:, None] * freq).astype(np.float32)
    angc = ((t % 64).astype(np.float32)[:, None] * freq).astype(np.float32)
    shared["rot_cs"] = np.concatenate([np.cos(angr), np.cos(angr), np.cos(angc), np.cos(angc)], 1).astype(np.float32)
    shared["rot_sn"] = np.concatenate([-np.sin(angr), np.sin(angr), -np.sin(angc), np.sin(angc)], 1).astype(np.float32)
    maps = []
    for core in range(ncores):
        s = core % 4
        m = dict(shared)
        m.update({"x": f32(inputs["x"][s]), "ctx": f32(inputs["ctx"][s]),
                  "cc": np.stack([inputs["c"][s], inputs["c_ctx"]]).astype(np.float32)})
        maps.append(m)
    return maps


def run(c, inputs):
    kb = K(c)
    nc = kb.build()
    maps = host_inputs(c, {k: np.asarray(v) for k, v in inputs.items()}, NCORES)
    res = run_bass_kernel_spmd(nc, maps, core_ids=list(range(NCORES)))
    return kb, res


def kernel(**inputs):
    c = make_cfg()
    kb, res = run(c, inputs)
    return np.stack([res.results[s]["out"] for s in range(4)]).astype(np.float32)
```

```python
import math
import numpy as np
from contextlib import ExitStack
import concourse.bass as bass
import concourse.mybir as mybir
from concourse.bass_utils import run_bass_kernel_spmd

F32 = mybir.dt.float32
BF16 = mybir.dt.bfloat16
I32 = mybir.dt.int32
U32 = mybir.dt.uint32
ALU = mybir.AluOpType
AF = mybir.ActivationFunctionType
ENGS = ("pe", "act", "dve", "pool", "sp")
NCORES = 4
BIG = 60000.0
NEG = -30000.0


class Prog:
    def __init__(self):
        self.nc = bass.Bass("TRN2", target_bir_lowering=False)
        self.es = ExitStack()
        self.ops = []
        self.n_dma_sems = 12
        self.bufs = {}
        self.arena = None

    def dram(self, name, shape, dt, kind="Internal"):
        return self.nc.dram_tensor(name, list(shape), dt, kind=kind)

    ARENA_BYTES = 176 * 1024

    def sb(self, name, shape, dt):
        if name in self.bufs:
            return self.bufs[name]
        if self.arena is None:
            self.arena = self.es.enter_context(self.nc.sbuf_tensor("arena", [128, self.ARENA_BYTES // 4], F32))
            self.bump = 0
            self.scopes = []
        esz = mybir.dt.size(dt)
        n = 1
        for d in shape[1:]:
            n *= d
        nbytes = (n * esz + 31) // 32 * 32
        assert self.bump + nbytes <= self.ARENA_BYTES, (name, self.bump, nbytes)
        v = self.arena[0:shape[0], self.bump // 4:(self.bump + nbytes) // 4]
        if dt != F32:
            v = v.bitcast(dt)
        v = v[:, 0:n]
        if len(shape) == 3:
            v = v.rearrange("p (a b) -> p a b", a=shape[1])
        elif len(shape) == 4:
            v = v.rearrange("p (a b c) -> p a b c", a=shape[1], b=shape[2])
        self.bump += nbytes
        self.peak = max(getattr(self, "peak", 0), self.bump)
        self.bufs[name] = v
        if self.scopes:
            self.scopes[-1][1].append(name)
        return v

    def push(self):
        if self.arena is None:
            self.sb("_dummy", [128, 8], F32)
        self.scopes.append((self.bump, []))

    def pop(self):
        bump, names = self.scopes.pop()
        for n in names:
            del self.bufs[n]
        self.bump = bump
        self.ops.append(("*", None, (), (), "bar"))

    def ps(self, name, shape, dt=F32):
        if name not in self.bufs:
            self.bufs[name] = self.es.enter_context(self.nc.psum_tensor(name, list(shape), dt))
        return self.bufs[name]

    def op(self, eng, fn, reads=(), writes=(), dma=False):
        self.ops.append((eng, fn, tuple(reads), tuple(writes), dma))

    def dma(self, q, out, in_, reads=(), writes=(), **kw):
        self.op(q, lambda e: e.dma_start(out=out, in_=in_, **kw), reads, writes, dma=True)

    def emit(self):
        nc, es = self.nc, self.es
        sem_c = {e: es.enter_context(nc.semaphore("s_" + e)) for e in ENGS}
        sem_d = {e: [es.enter_context(nc.semaphore(f"d_{e}{i}")) for i in range(self.n_dma_sems)]
                 for e in ("sp", "act", "pool")}
        sem_cc = es.enter_context(nc.semaphore("s_cc"))
        cnt_cc = [0]
        cnt_c = {e: 0 for e in ENGS}
        cnt_d = {e: [0] * self.n_dma_sems for e in sem_d}
        rr = {e: 0 for e in sem_d}
        last_w, readers = {}, {}
        known = {e: {} for e in ENGS}
        streams = {e: [] for e in ENGS}
        for (eng, fn, reads, writes, is_dma) in self.ops:
            if is_dma == "bar":
                alltok = [(sem_c[e], cnt_c[e]) for e in ENGS if cnt_c[e]]
                alltok += [(sem_d[e][k], cnt_d[e][k] * 16) for e in sem_d for k in range(self.n_dma_sems) if cnt_d[e][k]]
                if cnt_cc[0]:
                    alltok.append((sem_cc, cnt_cc[0]))
                for e in ENGS:
                    need = [(s_, v_) for (s_, v_) in alltok if known[e].get(id(s_), 0) < v_]
                    for (s_, v_) in need:
                        known[e][id(s_)] = v_
                    if need:
                        streams[e].append((need, None, None, 0))
                last_w, readers = {}, {}
                continue
            toks = []
            for r in reads:
                if r in last_w:
                    toks.append(last_w[r])
            for w in writes:
                if w in last_w:
                    toks.append(last_w[w])
                toks.extend(readers.get(w, ()))
            if is_dma == "cc":
                sem = sem_cc
                cnt_cc[0] += 1
                tok = (sem, cnt_cc[0])
                inc = 1
            elif is_dma:
                k = rr[eng]
                rr[eng] = (k + 1) % self.n_dma_sems
                sem = sem_d[eng][k]
                if cnt_d[eng][k] > 0:
                    toks.append((sem, cnt_d[eng][k] * 16))
                cnt_d[eng][k] += 1
                tok = (sem, cnt_d[eng][k] * 16)
                inc = 16
            else:
                sem = sem_c[eng]
                cnt_c[eng] += 1
                tok = (sem, cnt_c[eng])
                inc = 1
            need = {}
            for (s, v) in toks:
                if known[eng].get(id(s), 0) >= v:
                    continue
                if need.get(id(s), (None, 0))[1] < v:
                    need[id(s)] = (s, v)
            for (s, v) in need.values():
                known[eng][id(s)] = v
            streams[eng].append((list(need.values()), fn, sem, inc))
            for r in reads:
                readers.setdefault(r, []).append(tok)
            for w in writes:
                last_w[w] = tok
                readers[w] = []
        fin = []
        for e in ENGS:
            if cnt_c[e]:
                fin.append((sem_c[e], cnt_c[e]))
        for e in sem_d:
            for k in range(self.n_dma_sems):
                if cnt_d[e][k]:
                    fin.append((sem_d[e][k], cnt_d[e][k] * 16))
        if cnt_cc[0]:
            fin.append((sem_cc, cnt_cc[0]))
        self.n_instr = {e: len(streams[e]) for e in ENGS}
        self.n_instr['peak_sbuf'] = getattr(self, 'peak', 0)
        with nc.Block() as block:
            def mk(e):
                def body(engine):
                    for (waits, fn, sem, inc) in streams[e]:
                        for (s, v) in waits:
                            engine.wait_ge(s, v)
                        if fn is not None:
                            fn(engine).then_inc(sem, inc)
                    if e == "sp":
                        for (s, v) in fin:
                            engine.wait_ge(s, v)
                return body
            block.tensor(mk("pe"))
            block.scalar(mk("act"))
            block.vector(mk("dve"))
            block.gpsimd(mk("pool"))
            block.sync(mk("sp"))
        self.es.close()
        return nc


def make_cfg(T=4096, L=256, D=2048, F=2048, E=16, depth=2, debug=()):
    c = dict(T=T, L=L, D=D, F=F, E=E, depth=depth, debug=tuple(debug))
    c["NT"], c["NCT"] = T // 128, L // 128
    c["NTT"] = c["NT"] + c["NCT"]
    c["M"] = T + L
    c["HD"] = 128
    c["RH"] = D // 2 // 128
    c["RD"] = c["RH"] * 128
    c["GD"] = D - c["RD"]
    c["GG"] = 8
    c["GW"] = c["GD"] // 8
    c["NAH"] = D // 2 // 128
    c["NAD"] = c["NAH"] * 128
    c["GH"] = 8
    c["GDV"] = (D - c["NAD"]) // 8
    c["GDK"] = c["GDV"] // 2
    c["GQK"] = 8 * c["GDK"]
    c["GV"] = 8 * c["GDV"]
    c["EVC"] = 4 * c["RD"] + 2 * c["GD"]
    c["ODQ"] = c["NAD"] + c["GQK"] + c["GV"]
    c["ODC"] = c["ODQ"] + 2 * c["NAD"] + c["GQK"] + c["GV"] + 32
    c["capL"] = 2 * T // E
    c["capC"] = 2 * L // E
    c["rows"] = T // 64
    return c


def big_layout(c):
    D, F, E = c["D"], c["F"], c["E"]
    items = []
    for l in range(c["depth"]):
        items.append((f"ada{l}", D, 6 * D))
        items.append((f"win{l}", D, c["EVC"] if l % 2 == 0 else c["ODC"]))
        items.append((f"wout{l}", D, D))
        for e in range(E):
            items.append((f"wg{l}_{e}", D, F))
            items.append((f"wu{l}_{e}", D, F))
            items.append((f"wd{l}_{e}", F, D))
    off, table = 0, {}
    for (n, K, N) in items:
        table[n] = (off, K, N)
        off += K * N
    CH = 8 * 2048 * 16
    tot = (off + CH - 1) // CH * CH
    return table, tot


class K:
    def __init__(self, c):
        self.c = c
        self.P = Prog()
        self.nc = self.P.nc
        self.dbg = {}

    def D_(self, name, shape, dt, kind=None):
        if kind is None:
            kind = "ExternalOutput" if name in self.c["debug"] else "Internal"
        t = self.P.dram(name, shape, dt, kind)
        if kind == "ExternalOutput":
            self.dbg[name] = t
        return t.ap()

    def bcreg(self, eng, val):
        if not hasattr(self, "_bcregs"):
            self._bcregs = {}
        if val not in self._bcregs:
            self._bcregs[val] = eng.to_reg(val)
        return self._bcregs[val]

    def nkt(self):
        return min(5, self.c["NT"])

    def accb(self, i):
        return self.pp[i // 2][:, (i % 2) * 512:(i % 2) * 512 + 512]

    def tp(self, i):
        return self.pp[2][:].bitcast(BF16)[:, i * 1024:(i + 1) * 1024]

    def linear(self, tag, tiles, Kd, N, producer, W, epilogue, G=4, wkey=()):
        P = self.P
        KC = Kd // 128
        ident = self.ident
        NB = (N + 511) // 512
        ngrp = (len(tiles) + G - 1) // G
        for g in range(ngrp):
            grp = tiles[g * G:(g + 1) * G]
            xT = P.sb("lin_xT", [128, 16, max(G, 5) * 128], BF16)
            for j, (mt, rows) in enumerate(grp):
                i = g * G + j
                xb = P.sb(f"lin_xbf{i % 2}", [128, 2048], BF16)
                xkey = ("xbf", i % 2)
                producer(i, mt, rows, xb, xkey)
                for kc in range(KC):
                    tpi = (i * KC + kc) % 2
                    tp = self.tp(tpi)
                    P.op("pe", lambda e, tp=tp, xb=xb, kc=kc, rows=rows: e.transpose(
                        out=tp[:, 0:rows], in_=xb[0:rows, kc * 128:(kc + 1) * 128], identity=ident[0:rows, 0:rows]),
                        reads=[xkey, "ident"], writes=[("tp", tpi)])
                    ce = "act" if kc % 2 == 0 else "dve"
                    if ce == "act":
                        P.op("act", lambda e, tp=tp, kc=kc, j=j, rows=rows: e.copy(
                            out=xT[:, kc, j * 128:j * 128 + rows], in_=tp[:, 0:rows]),
                            reads=[("tp", tpi)], writes=[("xT", kc, j)])
                    else:
                        P.op("dve", lambda e, tp=tp, kc=kc, j=j, rows=rows: e.tensor_copy(
                            out=xT[:, kc, j * 128:j * 128 + rows], in_=tp[:, 0:rows]),
                            reads=[("tp", tpi)], writes=[("xT", kc, j)])
            for nb in range(NB):
                ncols = min(512, N - nb * 512)
                wi = self.wcnt % 2
                self.wcnt += 1
                wb = P.sb(f"lin_wb{wi}", [128, 16, 512], BF16)
                for hf in range((KC + 7) // 8):
                    k0, k1 = hf * 8, min(KC, hf * 8 + 8)
                    si = self.scnt % 2
                    self.scnt += 1
                    wst = P.sb(f"lin_wst{si}", [128, 8, 512], F32)
                    src = W[k0 * 128:k1 * 128, nb * 512:nb * 512 + ncols].rearrange("(c p) n -> p c n", p=128)
                    P.dma("sp", wst[:, 0:k1 - k0, 0:ncols], src, reads=list(wkey), writes=[("wst", si)])
                    ce = ("pool", "act")[self.scnt % 2]
                    if ce == "pool":
                        P.op("pool", lambda e, wb=wb, wst=wst, k0=k0, k1=k1, ncols=ncols: e.tensor_copy(
                            out=wb[:, k0:k1, 0:ncols], in_=wst[:, 0:k1 - k0, 0:ncols]),
                            reads=[("wst", si)], writes=[("wb", wi, hf)])
                    else:
                        P.op("act", lambda e, wb=wb, wst=wst, k0=k0, k1=k1, ncols=ncols: e.copy(
                            out=wb[:, k0:k1, 0:ncols], in_=wst[:, 0:k1 - k0, 0:ncols]),
                            reads=[("wst", si)], writes=[("wb", wi, hf)])
                for j, (mt, rows) in enumerate(grp):
                    i = g * G + j
                    ai = self.acnt % 4
                    self.acnt += 1
                    acc = self.accb(ai)
                    for kc in range(KC):
                        P.op("pe", lambda e, acc=acc, kc=kc, j=j, rows=rows, wb=wb, ncols=ncols: e.matmul(
                            out=acc[0:rows, 0:ncols], lhsT=xT[:, kc, j * 128:j * 128 + rows], rhs=wb[:, kc, 0:ncols],
                            start=(kc == 0), stop=(kc == KC - 1)),
                            reads=[("xT", kc, j), ("wb", wi, kc // 8)], writes=[("acc", ai)])
                    epilogue(i, mt, rows, nb, ncols, acc, ("acc", ai))

    def setup_consts(self):
        P = self.P
        self.wcnt = self.scnt = self.acnt = 0
        identf = P.sb("identf", [128, 128], F32)
        ident = P.sb("ident", [128, 128], BF16)
        P.op("pool", lambda e: e.memset(identf[:], 0.0), writes=["identf"])
        P.op("pool", lambda e: e.affine_select(out=identf[:], in_=identf[:], pattern=[[-1, 128]],
                                               compare_op=ALU.not_equal, fill=1.0, base=0, channel_multiplier=1),
             reads=["identf"], writes=["identf"])
        P.op("dve", lambda e: e.tensor_copy(out=ident[:], in_=identf[:]), reads=["identf"], writes=["ident"])
        self.ident, self.identf = ident, identf
        self.pp = [P.ps(f"pp{i}", [128, 1024], F32) for i in range(4)]
        self.consts_attn()

    def build(self):
        c, P, nc = self.c, self.P, self.nc
        D, M, T, L = c["D"], c["M"], c["T"], c["L"]
        table, tot = big_layout(c)
        self.table = table
        EI = lambda n, s, dt=F32: P.dram(n, s, dt, "ExternalInput").ap()
        self.x_in = EI("x", [T, D])
        self.ctx_in = EI("ctx", [L, D])
        self.cc_in = EI("cc", [2, D])
        self.small = {}
        for l in range(c["depth"]):
            self.small[f"ada_b{l}"] = EI(f"ada_b{l}", [1, 6 * D])
            self.small[f"norm_mix{l}"] = EI(f"norm_mix{l}", [1, D])
            self.small[f"norm_ffn{l}"] = EI(f"norm_ffn{l}", [1, D])
            self.small[f"router{l}"] = EI(f"router{l}", [D, c["E"]])
        self.small["norm_final"] = EI("norm_final", [1, D])
        for l in range(c["depth"]):
            if l % 2 == 0:
                self.small[f"gamma{l}"] = EI(f"gamma{l}", [1, 2 * c["RH"]])
                self.small[f"ret_norm{l}"] = EI(f"ret_norm{l}", [1, c["RD"]])
                self.small[f"gmlp_norm{l}"] = EI(f"gmlp_norm{l}", [1, c["GD"]])
                self.small[f"gmlp_wsT{l}"] = EI(f"gmlp_wsT{l}", [8, 128, 128])
                self.small[f"gmlp_bsT{l}"] = EI(f"gmlp_bsT{l}", [128, 8])
            else:
                self.small[f"gla_wup{l}"] = EI(f"gla_wup{l}", [2, 16, c["GQK"]])
                self.small[f"gla_bup{l}"] = EI(f"gla_bup{l}", [1, 2, c["GQK"]])
                self.small[f"gla_norm{l}"] = EI(f"gla_norm{l}", [1, c["GV"]])
                self.small[f"na_bias{l}"] = EI(f"na_bias{l}", [6, c["NAH"], 128, self.nkt() * 128])
        self.small["rot_cs"] = EI("rot_cs", [T, 128])
        self.small["rot_sn"] = EI("rot_sn", [T, 128])
        self.out = P.dram("out", [T, D], F32, "ExternalOutput").ap()
        self.setup_consts()
        E, F = c["E"], c["F"]
        self.W = {}
        for l in range(c["depth"]):
            self.W[f"ada{l}"] = EI(f"ada{l}", [D, 6 * D])
            self.W[f"win{l}"] = EI(f"win{l}", [D, c["EVC"] if l % 2 == 0 else c["ODC"]])
            self.W[f"wout{l}"] = EI(f"wout{l}", [D, D])
            wg = EI(f"wg{l}", [E * D, F]); wu = EI(f"wu{l}", [E * D, F]); wd = EI(f"wd{l}", [E * F, D])
            for e in range(E):
                self.W[f"wg{l}_{e}"] = wg[e * D:(e + 1) * D, :]
                self.W[f"wu{l}_{e}"] = wu[e * D:(e + 1) * D, :]
                self.W[f"wd{l}_{e}"] = wd[e * F:(e + 1) * F, :]
        self.Wm = lambda name: self.W[name]
        self.XC = self.D_("XC", [M, D], F32)
        self.X1 = self.D_("X1", [M, D], F32)
        for mt in range(c["NTT"]):
            src = self.x_in[mt * 128:(mt + 1) * 128, :] if mt < c["NT"] else \
                self.ctx_in[(mt - c["NT"]) * 128:(mt - c["NT"] + 1) * 128, :]
            P.dma("sp", self.XC[mt * 128:(mt + 1) * 128, :], src, writes=[("XC", mt)])
        self.phase_mod()
        for l in range(c["depth"]):
            self.layer(l)
            if f"stop_l{l}" in c["debug"]:
                break
        self.phase_final()
        return P.emit()

    def phase_mod(self):
        c, P = self.c, self.P
        D = c["D"]
        self.MOD = []
        P.push()
        ccs = P.sb("S5", [128, 2048], F32)
        P.op("pool", lambda e: e.memset(ccs[:], 0.0), writes=["S5"])
        P.dma("sp", ccs[0:2, 0:D], self.cc_in[:, :], reads=["S5"], writes=["S5"])
        for l in range(c["depth"]):
            MODl = self.D_(f"MOD{l}", [2, 6 * D], F32)
            self.MOD.append(MODl)
            bias = None

            def prod(i, mt, rows, xb, xkey):
                P.op("act", lambda e: e.activation(out=xb[:, 0:D], in_=ccs[:, 0:D], func=AF.Silu),
                     reads=["S5"], writes=[xkey])

            def epi(i, mt, rows, nb, ncols, acc, akey, l=l, MODl=MODl, bias=bias):
                st = P.sb("mod_st", [2, 512], F32)
                bst = P.sb("mod_bst", [2, 512], F32)
                bsrc = self.small[f"ada_b{l}"][0:1, nb * 512:nb * 512 + ncols]
                P.dma("sp", bst[0:1, 0:ncols], bsrc, writes=["mod_bst0"])
                P.dma("sp", bst[1:2, 0:ncols], bsrc, writes=["mod_bst1"])
                P.op("dve", lambda e: e.tensor_tensor(out=st[0:2, 0:ncols], in0=acc[0:2, 0:ncols],
                                                      in1=bst[0:2, 0:ncols], op=ALU.add),
                     reads=[akey, "mod_bst0", "mod_bst1"], writes=["mod_st"])
                P.dma("sp", MODl[:, nb * 512:nb * 512 + ncols], st[0:2, 0:ncols], reads=["mod_st"],
                      writes=[("MOD", l, nb)])
            self.linear(f"mod{l}", [(0, 128)], D, 6 * D, prod, self.Wm(f"ada{l}"), epi)
        P.pop()

    def load_modvec(self, name, l, row, seg, gain=None, plus1=False):
        c, P = self.c, self.P
        D = c["D"]
        t = P.sb(name, [128, 2048], F32)
        src = self.MOD[l][row:row + 1, seg * D:(seg + 1) * D]
        rk = [("MOD", l, nb) for nb in range(seg * D // 512, (seg + 1) * D // 512)]
        P.dma("sp", t[:, 0:D], src.partition_broadcast(128), reads=rk, writes=[name])
        if gain is not None:
            g = P.sb("S4", [128, 2048], F32)
            P.dma("sp", g[:, 0:D], self.small[gain].partition_broadcast(128), writes=["S4"])
            P.op("dve", lambda e: e.scalar_tensor_tensor(out=t[:, 0:D], in0=t[:, 0:D], scalar=1.0 if plus1 else 0.0, in1=g[:, 0:D],
                                                         op0=ALU.add, op1=ALU.mult),
                 reads=[name, "S4"], writes=[name])
        return t

    def norm_tile(self, src_ap, src_keys, A, Akey, Bv, Bkey, out_bf, out_key, xname="S5"):
        c, P = self.c, self.P
        D = c["D"]
        xt = P.sb(xname, [128, 2048], F32)
        junk = P.sb("nt_junk", [128, 2048], BF16)
        ss = P.sb("nt_ss", [128, 1], F32)
        rs = P.sb("nt_rs", [128, 1], F32)
        P.dma("sp", xt[:, 0:D], src_ap, reads=src_keys, writes=[xname])
        P.op("act", lambda e: e.activation(out=junk[:, 0:D], in_=xt[:, 0:D], func=AF.Square, accum_out=ss[:]),
             reads=[xname], writes=["nt_junk", "nt_ss"])
        P.op("dve", lambda e: e.tensor_scalar(out=rs[:], in0=ss[:], scalar1=1.0 / D, scalar2=1e-6,
                                              op0=ALU.mult, op1=ALU.add), reads=["nt_ss"], writes=["nt_rs"])
        P.op("act", lambda e: e.activation(out=rs[:], in_=rs[:], func=AF.Sqrt), reads=["nt_rs"], writes=["nt_rs"])
        P.op("dve", lambda e: e.reciprocal(out=rs[:], in_=rs[:]), reads=["nt_rs"], writes=["nt_rs"])
        P.op("dve", lambda e: e.scalar_tensor_tensor(out=xt[:, 0:D], in0=xt[:, 0:D], scalar=rs[:, 0:1], in1=A[:, 0:D],
                                                     op0=ALU.mult, op1=ALU.mult),
             reads=[xname, "nt_rs", Akey], writes=[xname])
        P.op("pool", lambda e: e.tensor_tensor(out=out_bf[:, 0:D], in0=xt[:, 0:D], in1=Bv[:, 0:D], op=ALU.add),
             reads=[xname, Bkey], writes=[out_key])

    def layer(self, l):
        c, P = self.c, self.P
        D, M = c["D"], c["M"]
        last = (l == c["depth"] - 1)
        even = (l % 2 == 0)
        NCOL = c["EVC"] if even else c["ODC"]
        PB = self.D_(f"PB{l}", [M, NCOL], BF16)
        self.PB = PB
        P.push()
        A1 = self.load_modvec("S0", l, 0, 1, gain=f"norm_mix{l}", plus1=True)
        B1 = self.load_modvec("S1", l, 0, 0)
        A1c = self.load_modvec("S2", l, 1, 1, gain=f"norm_mix{l}", plus1=True)
        B1c = self.load_modvec("S3", l, 1, 0)
        tiles = [(mt, 128) for mt in range(c["NTT"])]

        def prod(i, mt, rows, xb, xkey):
            isc = mt >= c["NT"]
            self.norm_tile(self.XC[mt * 128:(mt + 1) * 128, :], [("XC", mt)],
                           A1c if isc else A1, "S2" if isc else "S0",
                           B1c if isc else B1, "S3" if isc else "S1", xb, xkey)

        def epi(i, mt, rows, nb, ncols, acc, akey):
            oi = self.ocnt % 2
            self.ocnt += 1
            st = P.sb(f"p1_st{oi}", [128, 512], BF16)
            P.op("act", lambda e: e.copy(out=st[:, 0:ncols], in_=acc[:, 0:ncols]), reads=[akey],
                 writes=[("p1_st", oi)])
            P.dma("sp", PB[mt * 128:(mt + 1) * 128, nb * 512:nb * 512 + ncols], st[:, 0:ncols],
                  reads=[("p1_st", oi)], writes=[("PB", mt, nb)])
        self.ocnt = 0
        self.linear(f"win{l}", tiles, D, NCOL, prod, self.Wm(f"win{l}"), epi, G=8)
        P.pop()
        if "stop_p1" in c["debug"] or f"stop_p1_{l}" in c["debug"]:
            return
        self.MIX = self.D_(f"MIX{l}", [M, D], BF16)
        for fn in ((lambda: self.gmlp(l, last)), (lambda: self.decay_attn(l, "ret", last))) if even else \
                ((lambda: self.nattn(l)), (lambda: self.decay_attn(l, "gla", last))):
            if (not even) and (("nattn" in fn.__code__.co_names and "skip_na" in c["debug"]) or ("decay_attn" in fn.__code__.co_names and "skip_gla" in c["debug"])):
                continue
            P.push()
            fn()
            P.pop()
        if "stop_p2" in c["debug"] or f"stop_p2_{l}" in c["debug"]:
            return
        ntl = c["NT"] if last else c["NTT"]
        tiles = [(mt, 128) for mt in range(ntl)]
        P.push()
        G1 = self.load_modvec("S0", l, 0, 2)
        G1c = self.load_modvec("S1", l, 1, 2) if not last else None
        X1 = self.X1
        mixkeys = lambda mt: [("MIX", mt, k_, s_) for (k_, s_) in ((("gmlp", 0), ("ret", 0)) if even else (("na", 0), ("gla", 0)))]

        def prod3(i, mt, rows, xb, xkey):
            P.dma("sp", xb[:, 0:D], self.MIX[mt * 128:(mt + 1) * 128, :], reads=mixkeys(mt), writes=[xkey])

        def epi3(i, mt, rows, nb, ncols, acc, akey):
            oi = self.ocnt % 2
            self.ocnt += 1
            xt = P.sb(f"p3_x{oi}", [128, 512], F32)
            g = G1c if mt >= c["NT"] else G1
            gk = "S1" if mt >= c["NT"] else "S0"
            P.dma("sp", xt[:, 0:ncols], self.XC[mt * 128:(mt + 1) * 128, nb * 512:nb * 512 + ncols], reads=[("XC", mt)], writes=[("p3_x", oi)])
            st = P.sb(f"p3_st{oi}", [128, 512], F32)
            P.op("dve", lambda e: e.tensor_tensor(out=st[:, 0:ncols], in0=acc[:, 0:ncols], in1=g[:, nb * 512:nb * 512 + ncols], op=ALU.mult),
                 reads=[akey, gk], writes=[("p3_st", oi)])
            P.op("pool", lambda e: e.tensor_tensor(out=st[:, 0:ncols], in0=st[:, 0:ncols], in1=xt[:, 0:ncols], op=ALU.add),
                 reads=[("p3_st", oi), ("p3_x", oi)], writes=[("p3_st", oi)])
            P.dma("sp", X1[mt * 128:(mt + 1) * 128, nb * 512:nb * 512 + ncols], st[:, 0:ncols], reads=[("p3_st", oi)], writes=[("X1", mt, nb)])
        self.linear(f"wout{l}", tiles, D, D, prod3, self.Wm(f"wout{l}"), epi3, G=8)
        P.pop()
        if "stop_p3" in c["debug"] or f"stop_p3_{l}" in c["debug"]:
            return
        self.moe(l, last, tiles)

    def moe(self, l, last, tiles):
        c, P = self.c, self.P
        D, M, E, F, NT = c["D"], c["M"], c["E"], c["F"], c["NT"]
        self.consts_attn()
        capL, capC = c["capL"], (0 if last else c["capC"])
        SLOTS = capL + capC
        H2 = self.D_(f"H2_{l}", [M, D], BF16)
        AFF = self.D_(f"AFF{l}", [M, E], F32)
        P.push()
        affT = P.sb("moe_affT", [16, M], F32)
        idxT = P.sb("moe_idxT", [128, 34, 16], I32)
        wT = P.sb("moe_wT", [128, 34, 16], F32)
        P.push()
        A2 = self.load_modvec("S0", l, 0, 4, gain=f"norm_ffn{l}", plus1=True)
        B2 = self.load_modvec("S1", l, 0, 3)
        if not last:
            A2c = self.load_modvec("S2", l, 1, 4, gain=f"norm_ffn{l}", plus1=True)
            B2c = self.load_modvec("S3", l, 1, 3)
        x1keys = lambda mt: [("X1", mt, nb) for nb in range(D // 512)]

        def prod(i, mt, rows, xb, xkey):
            isc = mt >= NT
            self.norm_tile(self.X1[mt * 128:(mt + 1) * 128, :], x1keys(mt), A2c if isc else A2, "S2" if isc else "S0",
                           B2c if isc else B2, "S3" if isc else "S1", xb, xkey)
            P.dma("sp", H2[mt * 128:(mt + 1) * 128, :], xb[:, 0:D], reads=[xkey], writes=[("H2", mt)])

        def epi(i, mt, rows, nb, ncols, acc, akey):
            lg = P.sb("moe_lg", [128, 16], F32)
            mx = P.sb("moe_mx", [128, 1], F32)
            sm = P.sb("moe_sm", [128, 1], F32)
            P.op("dve", lambda e: e.tensor_reduce(out=mx[:], in_=acc[:, 0:E], axis=mybir.AxisListType.X, op=ALU.max), reads=[akey], writes=["moe_mx"])
            P.op("dve", lambda e: e.tensor_scalar(out=mx[:], in0=mx[:], scalar1=-1.0, scalar2=None, op0=ALU.mult), reads=["moe_mx"], writes=["moe_mx"])
            P.op("act", lambda e: e.activation(out=lg[:, 0:E], in_=acc[:, 0:E], func=AF.Exp, bias=mx[:, 0:1], accum_out=sm[:]),
                 reads=[akey, "moe_mx"], writes=["moe_lg", "moe_sm"])
            P.op("dve", lambda e: e.reciprocal(out=sm[:], in_=sm[:]), reads=["moe_sm"], writes=["moe_sm"])
            P.op("dve", lambda e: e.tensor_scalar(out=lg[:, 0:E], in0=lg[:, 0:E], scalar1=sm[:, 0:1], scalar2=None, op0=ALU.mult),
                 reads=["moe_lg", "moe_sm"], writes=["moe_lg"])
            P.dma("sp", AFF[mt * 128:(mt + 1) * 128, :], lg[:, 0:E], reads=["moe_lg"], writes=[("AFF", mt)])
            tpf = self.pp[2]
            P.op("pe", lambda e: e.transpose(out=tpf[0:E, 0:128], in_=lg[:, 0:E], identity=self.identf[:, :]), reads=["moe_lg", "identf"], writes=[("tp", 0)])
            P.op("act", lambda e: e.copy(out=affT[0:E, mt * 128:(mt + 1) * 128], in_=tpf[0:E, 0:128]), reads=[("tp", 0)], writes=[("affT", mt)])
        self.linear(f"router{l}", tiles, D, E, prod, self.small[f"router{l}"], epi)
        P.pop()
        P.push()
        ones16 = P.sb("mo_ones", [16, 2048], F32)
        P.op("pool", lambda e: e.memset(ones16[:], 1.0), writes=["mo_ones"])
        work = P.sb("moe_work", [16, M], F32)
        m8 = P.sb("moe_m8", [16, 8], F32)
        idxf = P.sb("moe_idxf", [16, M], F32)
        segs = [(0, c["T"], capL, 0)] + ([] if last else [(c["T"], c["L"], capC, capL)])
        ntl = len(tiles)
        for (o0, n, cap, slot0) in segs:
            rk = [("affT", mt) for mt in range(o0 // 128, (o0 + n) // 128)]
            P.op("dve", lambda e, o0=o0, n=n: e.tensor_copy(out=work[0:E, o0:o0 + n], in_=affT[0:E, o0:o0 + n]), reads=rk, writes=["moe_work"])
            for it in range(cap // 8):
                P.op("dve", lambda e, o0=o0, n=n: e.max(out=m8[0:E, :], in_=work[0:E, o0:o0 + n]), reads=["moe_work"], writes=["moe_m8"])
                if it < cap // 8 - 1:
                    P.op("dve", lambda e, o0=o0, n=n: e.match_replace(out=work[0:E, o0:o0 + n], in_to_replace=m8[0:E, :], in_values=work[0:E, o0:o0 + n],
                                                                      imm_value=-1.0), reads=["moe_work", "moe_m8"], writes=["moe_work"])
            sel = work
            P.op("dve", lambda e, o0=o0, n=n: e.tensor_scalar(out=sel[0:E, o0:o0 + n], in0=affT[0:E, o0:o0 + n], scalar1=m8[0:E, 7:8], scalar2=None,
                                                              op0=ALU.is_ge), reads=rk + ["moe_m8", "moe_work"], writes=["moe_work"])
            for q0 in range(0, n, 2048):
                qn = min(2048, n - q0)
                init = 0.0 if q0 == 0 else idxf[0:E, o0 + q0 - 1:o0 + q0]
                P.op("dve", lambda e, a=o0 + q0, qn=qn, init=init: e.tensor_tensor_scan(out=idxf[0:E, a:a + qn], data0=ones16[0:E, 0:qn], data1=sel[0:E, a:a + qn],
                                                                                        initial=init, op0=ALU.mult, op1=ALU.add), reads=["moe_work", "mo_ones", "moe_idxf"], writes=["moe_idxf"])
            P.op("dve", lambda e, o0=o0, n=n, cap=cap: e.scalar_tensor_tensor(out=sel[0:E, o0:o0 + n], in0=idxf[0:E, o0:o0 + n], scalar=float(cap) + 0.5,
                                                                              in1=sel[0:E, o0:o0 + n], op0=ALU.is_le, op1=ALU.mult),
                 reads=["moe_work", "moe_idxf"], writes=["moe_work"])
            P.op("dve", lambda e, o0=o0, n=n, slot0=slot0: e.tensor_scalar(out=idxf[0:E, o0:o0 + n], in0=idxf[0:E, o0:o0 + n], scalar1=float(slot0 - 1) - BIG,
                                                                           scalar2=None, op0=ALU.add), reads=["moe_idxf"], writes=["moe_idxf"])
            P.op("dve", lambda e, o0=o0, n=n: e.tensor_tensor(out=idxf[0:E, o0:o0 + n], in0=idxf[0:E, o0:o0 + n], in1=sel[0:E, o0:o0 + n], op=ALU.mult),
                 reads=["moe_idxf", "moe_work"], writes=["moe_idxf"])
            P.op("dve", lambda e, o0=o0, n=n: e.tensor_scalar(out=idxf[0:E, o0:o0 + n], in0=idxf[0:E, o0:o0 + n], scalar1=BIG, scalar2=None, op0=ALU.add),
                 reads=["moe_idxf"], writes=["moe_idxf"])
        tpf = self.pp[2]
        for (mt, rows) in tiles:
            P.op("pe", lambda e, mt=mt: e.transpose(out=tpf[:, 0:E], in_=idxf[0:E, mt * 128:(mt + 1) * 128], identity=self.identf[0:E, 0:E]),
                 reads=["moe_idxf", "identf"], writes=[("tp", 0)])
            P.op("pe", lambda e, mt=mt: e.transpose(out=tpf[:, 512:512 + E], in_=affT[0:E, mt * 128:(mt + 1) * 128], identity=self.identf[0:E, 0:E]),
                 reads=[("affT", mt), "identf"], writes=[("tp", 1)])
            P.op("dve", lambda e, mt=mt: e.tensor_copy(out=idxT[:, mt, :], in_=tpf[:, 0:E]), reads=[("tp", 0)], writes=[("idxT", mt)])
            P.op("dve", lambda e, mt=mt: e.tensor_scalar(out=wT[:, mt, :], in0=tpf[:, 0:E], scalar1=BIG / 2, scalar2=None, op0=ALU.is_lt),
                 reads=[("tp", 0)], writes=[("wT", mt)])
            P.op("dve", lambda e, mt=mt: e.tensor_tensor(out=wT[:, mt, :], in0=wT[:, mt, :], in1=tpf[:, 512:512 + E], op=ALU.mult),
                 reads=[("wT", mt), ("tp", 1)], writes=[("wT", mt)])
        P.pop()
        P.push()
        XSe = [self.D_(f"XS{l}_{ex}", [SLOTS, D], BF16) for ex in range(E)]
        YSe = [self.D_(f"YS{l}_{ex}", [SLOTS, D], F32) for ex in range(E)]
        for (mt, rows) in tiles:
            hb = P.sb(f"lin_xbf{mt % 2}", [128, 2048], BF16)
            hk = ("xbf", mt % 2)
            P.dma("sp", hb[:, 0:D], H2[mt * 128:(mt + 1) * 128, :], reads=[("H2", mt)], writes=[hk])
            for ex in range(E):
                P.op("pool", lambda e, mt=mt, ex=ex, hb=hb: e.indirect_dma_start(
                    out=XSe[ex][:, :], out_offset=bass.IndirectOffsetOnAxis(ap=idxT[:, mt, ex:ex + 1], axis=0),
                    in_=hb[:, 0:D], in_offset=None, bounds_check=self.bcreg(e, SLOTS - 1), oob_is_err=False),
                    reads=[hk, ("idxT", mt)], writes=[("XS", ex, mt)], dma=True)
        P.pop()
        P.push()
        stiles = [(j, min(128, SLOTS - j * 128)) for j in range((SLOTS + 127) // 128)]
        HS = self.D_(f"HS{l}", [SLOTS, F], BF16)
        GA = self.D_(f"GA{l}", [SLOTS, F], BF16)
        for ex in range(E):
            xsk = [("XS", ex, mt) for (mt, _) in tiles]

            def prodx(i, j, rows, xb, xkey, ex=ex):
                P.dma("sp", xb[0:rows, 0:D], XSe[ex][j * 128:j * 128 + rows, :], reads=xsk, writes=[xkey])

            def epig(i, j, rows, nb, ncols, acc, akey, ex=ex):
                oi = self.ocnt % 2
                self.ocnt += 1
                st = P.sb(f"p1_st{oi}", [128, 512], BF16)
                P.op("act", lambda e: e.activation(out=st[0:rows, 0:ncols], in_=acc[0:rows, 0:ncols], func=AF.Silu), reads=[akey], writes=[("p1_st", oi)])
                P.dma("sp", GA[j * 128:j * 128 + rows, nb * 512:nb * 512 + ncols], st[0:rows, 0:ncols], reads=[("p1_st", oi)], writes=[("GA", j, nb)])

            def epiu(i, j, rows, nb, ncols, acc, akey, ex=ex):
                oi = self.ocnt % 2
                self.ocnt += 1
                ga = P.sb(f"p1_st{oi}", [128, 512], BF16)
                st = P.sb(f"mo_hs{oi}", [128, 512], BF16)
                P.dma("sp", ga[0:rows, 0:ncols], GA[j * 128:j * 128 + rows, nb * 512:nb * 512 + ncols], reads=[("GA", j, nb)], writes=[("p1_st", oi)])
                P.op("dve", lambda e: e.tensor_tensor(out=st[0:rows, 0:ncols], in0=acc[0:rows, 0:ncols], in1=ga[0:rows, 0:ncols], op=ALU.mult),
                     reads=[akey, ("p1_st", oi)], writes=[("mo_hs", oi)])
                P.dma("sp", HS[j * 128:j * 128 + rows, nb * 512:nb * 512 + ncols], st[0:rows, 0:ncols], reads=[("mo_hs", oi)], writes=[("HS", j, nb)])

            def prodh(i, j, rows, xb, xkey):
                P.dma("sp", xb[0:rows, 0:F], HS[j * 128:j * 128 + rows, :], reads=[("HS", j, nb) for nb in range((F + 511) // 512)], writes=[xkey])

            def epid(i, j, rows, nb, ncols, acc, akey, ex=ex):
                oi = self.ocnt % 2
                self.ocnt += 1
                st = P.sb(f"p3_st{oi}", [128, 512], F32)
                P.op("act", lambda e: e.copy(out=st[0:rows, 0:ncols], in_=acc[0:rows, 0:ncols]), reads=[akey], writes=[("p3_st", oi)])
                P.dma("sp", YSe[ex][j * 128:j * 128 + rows, nb * 512:nb * 512 + ncols], st[0:rows, 0:ncols],
                      reads=[("p3_st", oi)], writes=[("YS", ex, j, nb)])
            self.linear(f"g{l}_{ex}", stiles, D, F, prodx, self.Wm(f"wg{l}_{ex}"), epig, G=len(stiles))
            self.linear(f"u{l}_{ex}", stiles, D, F, prodx, self.Wm(f"wu{l}_{ex}"), epiu, G=len(stiles))
            self.linear(f"d{l}_{ex}", stiles, F, D, prodh, self.Wm(f"wd{l}_{ex}"), epid, G=len(stiles))
        P.pop()
        P.push()
        G2 = self.load_modvec("S0", l, 0, 5)
        G2c = self.load_modvec("S1", l, 1, 5) if not last else None
        for (mt, rows) in tiles:
            isc = mt >= NT
            accs = P.sb("S2", [128, 2048], F32)
            x1t = P.sb("S3", [128, 2048], F32)
            P.dma("sp", x1t[:, 0:D], self.X1[mt * 128:(mt + 1) * 128, :], reads=x1keys(mt), writes=["S3"])
            P.op("pool", lambda e: e.memset(accs[:, 0:D], 0.0), writes=["S2"])
            for ex in range(E):
                bi = ex % 2
                buf = P.sb(f"S{4 + bi}", [128, 2048], F32)
                if mt == tiles[0][0] and ex < 2:
                    P.op("pool", lambda e, buf=buf: e.memset(buf[:, 0:D], 0.0), writes=[f"S{4 + bi}"])
                yk = [("YS", ex, j, nb) for (j, _) in stiles for nb in range(D // 512)]
                P.op("pool", lambda e, mt=mt, ex=ex, buf=buf: e.indirect_dma_start(
                    out=buf[:, 0:D], out_offset=None, in_=YSe[ex][:, :],
                    in_offset=bass.IndirectOffsetOnAxis(ap=idxT[:, mt, ex:ex + 1], axis=0), bounds_check=self.bcreg(e, SLOTS - 1), oob_is_err=False),
                    reads=yk + [("idxT", mt)], writes=[f"S{4 + bi}"], dma=True)
                P.op("dve", lambda e, mt=mt, ex=ex, buf=buf: e.scalar_tensor_tensor(out=accs[:, 0:D], in0=buf[:, 0:D], scalar=wT[:, mt, ex:ex + 1], in1=accs[:, 0:D],
                                                                                    op0=ALU.mult, op1=ALU.add), reads=[f"S{4 + bi}", ("wT", mt), "S2"], writes=["S2"])
            g = G2c if isc else G2
            P.op("pool", lambda e, g=g: e.tensor_tensor(out=accs[:, 0:D], in0=accs[:, 0:D], in1=g[:, 0:D], op=ALU.mult), reads=["S2", "S1" if isc else "S0"], writes=["S2"])
            P.op("dve", lambda e: e.tensor_tensor(out=accs[:, 0:D], in0=accs[:, 0:D], in1=x1t[:, 0:D], op=ALU.add), reads=["S2", "S3"], writes=["S2"])
            P.dma("sp", self.XC[mt * 128:(mt + 1) * 128, :], accs[:, 0:D], reads=["S2"], writes=[("XC", mt)])
        P.pop()
        P.pop()

    def consts_attn(self):
        P = self.P
        if hasattr(self, "maskF"):
            return
        ones = P.sb("c_ones", [128, 128], F32)
        self.maskF = P.sb("c_maskF", [128, 128], F32)
        self.maskB = P.sb("c_maskB", [128, 128], F32)
        P.op("pool", lambda e: e.memset(ones[:], 1.0), writes=["c_ones"])
        P.op("pool", lambda e: e.affine_select(out=self.maskF[:], in_=ones[:], pattern=[[1, 128]], compare_op=ALU.is_ge,
                                               fill=0.0, base=0, channel_multiplier=-1), reads=["c_ones"], writes=["c_maskF", "c_maskB"])
        P.op("pool", lambda e: e.affine_select(out=self.maskB[:], in_=ones[:], pattern=[[-1, 128]], compare_op=ALU.is_ge,
                                               fill=0.0, base=0, channel_multiplier=1), reads=["c_ones"], writes=["c_maskF", "c_maskB"])
        self.ones = ones
        pi = P.sb("c_pi", [128, 1], I32)
        pf = P.sb("c_pf", [128, 8], F32)
        P.op("pool", lambda e: e.iota(out=pi[:], pattern=[[0, 1]], base=0, channel_multiplier=1), writes=["c_pi"])
        P.op("dve", lambda e: e.tensor_copy(out=pf[:, 0:1], in_=pi[:]), reads=["c_pi"], writes=["c_pf"])
        for j, (m, a) in enumerate([(1.0, 1.0), (-1.0, -1.0), (1.0, -127.0), (-1.0, 128.0), (1.0, -128.0), (-1.0, 0.0)]):
            P.op("dve", lambda e, j=j, m=m, a=a: e.tensor_scalar(out=pf[:, j + 1:j + 2], in0=pf[:, 0:1], scalar1=m, scalar2=a,
                                                               op0=ALU.mult, op1=ALU.add), reads=["c_pf"], writes=["c_pf"])
        self.pf = pf

    def decay_attn(self, l, kind, last):
        c, P = self.c, self.P
        PB = self.PB
        NT, NTT = c["NT"], c["NTT"]
        C = 128
        if kind == "ret":
            H, dk, dv = c["RH"], 128, 128
            qo, go, ko, vo = 0, c["RD"], 2 * c["RD"] + 2 * c["GD"], 3 * c["RD"] + 2 * c["GD"]
            qscale = 128 ** -0.5
            mixo = 0
            gnorm_name = f"ret_norm{l}"
            NCOLS = c["EVC"]
        else:
            H, dk, dv = 8, c["GDK"], c["GDV"]
            qo = c["NAD"]
            go = c["NAD"] + c["GQK"]
            ko = c["ODQ"] + 2 * c["NAD"]
            vo = ko + c["GQK"]
            lro = vo + c["GV"]
            qscale = dk ** -0.5
            mixo = c["NAD"]
            gnorm_name = f"gla_norm{l}"
            NCOLS = c["ODC"]
        hp = 128 // dk
        npk = H // hp
        HK, HV = H * dk, H * dv
        lnq = math.log(qscale)
        OF = self.D_(f"OF{l}", [c["M"], HV], F32)
        lat_units = list(range(NT))
        ctx_units = list(range(NT, NTT))
        gn = P.sb("at_gn", [128, 1024], F32)
        P.dma("sp", gn[:, 0:HV], self.small[gnorm_name].partition_broadcast(128), writes=["at_gn"])
        S32 = P.sb("at_S32", [128, 1024], F32)
        Sbf = P.sb("at_Sbf", [128, 1024], BF16)
        qt = P.sb("at_q", [128, 1024], BF16)
        kt = P.sb("at_k", [128, 1024], BF16)
        vt = P.sb("at_v", [128, 1024], BF16)
        gt = P.sb("at_g", [128, 1024], BF16)
        qin = P.sb("at_qin", [128, 1024], BF16)
        kin = P.sb("at_kin", [128, 1024], BF16)
        kout = P.sb("at_kout", [128, 1024], BF16)
        qT = P.sb("at_qT", [128, 8, 128], BF16)
        kT = P.sb("at_kT", [128, 8, 128], BF16)
        attT = P.sb("at_attT", [128, 8, 128], BF16)
        osb = P.sb("S4", [128, 2048], F32)
        ofl = P.sb("S5", [128, 2048], F32)
        pf = self.pf
        if hp > 1:
            rowm = P.sb("at_rowm", [128, 4], F32)
            P.op("pool", lambda e: e.memset(rowm[:, :], 0.0), writes=["at_rowm"])
            for j in range(hp):
                P.op("pool", lambda e, j=j: e.memset(rowm[j * dk:(j + 1) * dk, j:j + 1], 1.0), reads=["at_rowm"], writes=["at_rowm"])
        if kind == "ret":
            gl = P.sb("at_gl", [128, 16], F32)
            P.dma("sp", gl[:, 0:2 * H], self.small[f"gamma{l}"].partition_broadcast(128), writes=["at_gl"])
            P.op("act", lambda e: e.activation(out=gl[:, 0:2 * H], in_=gl[:, 0:2 * H], func=AF.Exp, scale=-1.0),
                 reads=["at_gl"], writes=["at_gl"])
            P.op("act", lambda e: e.activation(out=gl[:, 0:2 * H], in_=gl[:, 0:2 * H], func=AF.Ln, bias=1.0),
                 reads=["at_gl"], writes=["at_gl"])
            tb = P.sb("at_tb", [128, 2, 4, 8], F32)
            for d in range(2):
                sp_d = gl[:, d * H:(d + 1) * H]
                cols = [(2, lnq), (1, 0.0), (3, 0.0)] if d == 0 else [(5, lnq), (4, 0.0), (6, 0.0)]
                for j, (pc, bias) in enumerate(cols):
                    P.op("act", lambda e, d=d, j=j, pc=pc, bias=bias, sp_d=sp_d: e.activation(
                        out=tb[:, d, j, 0:H], in_=sp_d, func=AF.Exp, scale=pf[:, pc:pc + 1], bias=bias),
                        reads=["at_gl", "c_pf"], writes=["at_tb"])
                P.op("act", lambda e, d=d, sp_d=sp_d: e.activation(out=tb[:, d, 3, 0:H], in_=sp_d, func=AF.Exp, scale=-128.0),
                     reads=["at_gl"], writes=["at_tb"])
            rcs = P.sb("at_rcs", [128, 128], F32)
            rsn = P.sb("at_rsn", [128, 128], F32)
        else:
            lrT = P.sb("at_lrT", [128, 128], BF16)
            wup = P.sb("at_wup", [128, 2, 512], F32)
            wuph = P.sb("at_wuph", [128, 2, 512], BF16)
            wupl = P.sb("at_wupl", [128, 2, 512], BF16)
            P.op("pool", lambda e: e.memset(lrT[:, :], 1.0), writes=["at_lrT"])
            P.op("pool", lambda e: e.memset(wup[:, :, :], 0.0), writes=["at_wup"])
            P.dma("sp", wup[0:16, :, 0:HK], self.small[f"gla_wup{l}"].rearrange("d r n -> r d n"), reads=["at_wup"], writes=["at_wup"])
            P.dma("sp", wup[16:17, :, 0:HK], self.small[f"gla_bup{l}"], reads=["at_wup"], writes=["at_wup"])
            P.op("dve", lambda e: e.tensor_copy(out=wuph[:, :, :], in_=wup[:, :, :]), reads=["at_wup"], writes=["at_wuph"])
            P.op("dve", lambda e: e.tensor_tensor(out=wupl[:, :, :], in0=wup[:, :, :], in1=wuph[:, :, :], op=ALU.subtract),
                 reads=["at_wup", "at_wuph"], writes=["at_wupl"])
            sph = P.sb("at_sph", [128, 512], BF16)
            spl = P.sb("at_spl", [128, 512], BF16)
            sp = P.sb("at_sp", [128, 512], F32)
            bsb = P.sb("at_bsb", [128, 512], F32)
            EQ = P.sb("at_EQ", [128, 512], F32)
            EKI = P.sb("at_EKI", [128, 512], F32)
            EKO = P.sb("at_EKO", [128, 512], F32)
            dec = P.sb("at_dec", [128, 4, 2], F32)
            LmF = P.sb("at_LmF", [128, 128], BF16)
            LmB = P.sb("at_LmB", [128, 128], BF16)
            Em = P.sb("at_Em", [128, 128], BF16)
            nsc = P.sb("at_nsc", [128, 2], BF16)
            P.op("dve", lambda e: e.tensor_scalar(out=LmF[:], in0=self.maskF[:, :], scalar1=-1.0 / 16, scalar2=None,
                                                  op0=ALU.mult), reads=["c_maskF"], writes=["at_LmF"])
            P.op("dve", lambda e: e.tensor_scalar(out=LmB[:], in0=self.maskB[:, :], scalar1=-1.0 / 16, scalar2=None,
                                                  op0=ALU.mult), reads=["c_maskB"], writes=["at_LmB"])
            P.op("pool", lambda e: e.memset(Em[:], -1.0 / 16), writes=["at_Em"])
            P.op("pool", lambda e: e.memset(nsc[:], -1.0 / 16), writes=["at_nsc"])

        if kind == "gla":
            SPD = self.D_(f"SPD{l}", [2 * c["M"], HK], F32)
            for mt in range(NTT):
                r0 = mt * 128
                pk = [("PB", mt, nb) for nb in range((NCOLS + 511) // 512)]
                P.dma("sp", gt[:, 0:32], PB[r0:r0 + C, lro:lro + 32], reads=pk, writes=["at_g"])
                for d in range(2):
                    tpb = self.tp(0)
                    P.op("pe", lambda e, d=d, tpb=tpb: e.transpose(out=tpb[0:16, 0:C], in_=gt[:, d * 16:(d + 1) * 16], identity=self.ident[:, :]),
                         reads=["at_g", "ident"], writes=[("tp", 0)])
                    P.op("act", lambda e, tpb=tpb: e.copy(out=lrT[0:16, :], in_=tpb[0:16, 0:C]), reads=[("tp", 0)], writes=["at_lrT"])
                    zps = self.accb(2)
                    P.op("pe", lambda e, d=d: e.matmul(out=zps[:, 0:HK], lhsT=lrT[:, :], rhs=wuph[:, d, 0:HK], start=True, stop=False),
                         reads=["at_lrT", "at_wuph"], writes=[("acc", 2)])
                    P.op("pe", lambda e, d=d: e.matmul(out=zps[:, 0:HK], lhsT=lrT[:, :], rhs=wupl[:, d, 0:HK], start=False, stop=True),
                         reads=["at_lrT", "at_wupl", ("acc", 2)], writes=[("acc", 2)])
                    P.op("act", lambda e: e.activation(out=sp[:, 0:HK], in_=zps[:, 0:HK], func=AF.Exp, scale=-1.0),
                         reads=[("acc", 2)], writes=["at_sp"])
                    P.op("act", lambda e: e.activation(out=sp[:, 0:HK], in_=sp[:, 0:HK], func=AF.Ln, bias=1.0),
                         reads=["at_sp"], writes=["at_sp"])
                    P.dma("sp", SPD[d * c["M"] + r0:d * c["M"] + r0 + C, :], sp[:, 0:HK], reads=["at_sp"], writes=[("SPD", d, mt)])
            P.ops.append(("*", None, (), (), "bar"))
            if c.get("gla_cut") == 1:
                return

        for d in range(2):
            order = (ctx_units + lat_units) if d == 0 else (ctx_units[::-1] + lat_units[::-1])
            mask = self.maskF if d == 0 else self.maskB
            mkey = "c_maskF" if d == 0 else "c_maskB"
            P.op("pool", lambda e: e.memset(S32[:], 0.0), writes=["at_S32"])
            P.op("pool", lambda e: e.memset(Sbf[:], 0.0), writes=["at_Sbf"])
            for mt in order:
                isc = mt >= NT
                r0 = mt * 128
                need_out = (not isc) or (not last)
                pk = [("PB", mt, nb) for nb in range((NCOLS + 511) // 512)]
                P.dma("sp", kt[:, 0:HK], PB[r0:r0 + C, ko:ko + HK], reads=pk, writes=["at_k"])
                P.dma("sp", vt[:, 0:HV], PB[r0:r0 + C, vo:vo + HV], reads=pk, writes=["at_v"])
                if need_out:
                    P.dma("sp", qt[:, 0:HK], PB[r0:r0 + C, qo:qo + HK], reads=pk, writes=["at_q"])
                qsrc, ksrc, qk_, kk_ = qt, kt, "at_q", "at_k"
                if kind == "ret":
                    if not isc:
                        P.dma("sp", rcs[:], self.small["rot_cs"][r0:r0 + 128, :], writes=["at_rcs"])
                        P.dma("sp", rsn[:], self.small["rot_sn"][r0:r0 + 128, :], writes=["at_rsn"])
                        for (src, skey, dstname) in ((qt, "at_q", "S0"), (kt, "at_k", "S1")):
                            dst = P.sb(dstname, [128, 2048], F32)
                            t1 = dst[:, 0:HK].rearrange("p (h x) -> p h x", h=H)
                            t2 = dst[:, 1024:1024 + HK].rearrange("p (h a b x) -> p h a b x", h=H, a=2, b=2)
                            sv = src[:, 0:HK].rearrange("p (h x) -> p h x", h=H)
                            sv5 = src[:, 0:HK].rearrange("p (h a b x) -> p h a b x", h=H, a=2, b=2)
                            csb = rcs[:].unsqueeze(1).broadcast_to([128, H, 128])
                            sn5 = rsn[:].rearrange("p (a b x) -> p a b x", a=2, b=2)
                            P.op("dve", lambda e, t1=t1, sv=sv, csb=csb: e.tensor_tensor(out=t1, in0=sv, in1=csb, op=ALU.mult),
                                 reads=[skey, "at_rcs"], writes=[dstname])
                            for b_ in range(2):
                                snb = sn5[:, :, b_, :].unsqueeze(1).broadcast_to([128, H, 2, 32])
                                P.op("pool", lambda e, t2=t2, sv5=sv5, snb=snb, b_=b_: e.tensor_tensor(
                                    out=t2[:, :, :, b_, :], in0=sv5[:, :, :, 1 - b_, :], in1=snb, op=ALU.mult),
                                    reads=[skey, "at_rsn"], writes=[dstname + "b"])
                            P.op("dve", lambda e, dst=dst: e.tensor_tensor(out=dst[:, 0:HK], in0=dst[:, 0:HK],
                                                                           in1=dst[:, 1024:1024 + HK], op=ALU.add),
                                 reads=[dstname, dstname + "b"], writes=[dstname])
                        qsrc, ksrc, qk_, kk_ = P.sb("S0", [128, 2048], F32), P.sb("S1", [128, 2048], F32), "S0", "S1"
                    bq = lambda j, d=d: tb[:, d, j, 0:H].unsqueeze(2).broadcast_to([128, H, dk])
                    tq, tki, tko = bq(0), bq(1), bq(2)
                    v3 = lambda t: t[:, 0:HK].rearrange("p (h x) -> p h x", h=H)
                    tkeys = ["at_tb"]
                    decap = tb[:, d, 3, 0:H]
                    dkeys = ["at_tb"]
                else:
                    P.dma("sp", sp[:, 0:HK], SPD[d * c["M"] + r0:d * c["M"] + r0 + C, :], reads=[("SPD", d, mt)], writes=["at_sp"])
                    if c.get("gla_cut") == 21:
                        continue
                    Lm = LmF if d == 0 else LmB
                    bps, eps_ = self.accb(2), self.accb(3)
                    P.op("dve", lambda e: e.tensor_copy(out=sph[:, 0:HK], in_=sp[:, 0:HK]), reads=["at_sp"], writes=["at_sph"])
                    P.op("dve", lambda e: e.tensor_tensor(out=spl[:, 0:HK], in0=sp[:, 0:HK], in1=sph[:, 0:HK], op=ALU.subtract),
                         reads=["at_sp", "at_sph"], writes=["at_spl"])
                    for (dst_, akey_, lhs_, lk_) in ((bps, ("acc", 2), Lm, ["at_LmF", "at_LmB"]), (eps_, ("acc", 3), Em, ["at_Em"])):
                        P.op("pe", lambda e, dst_=dst_, lhs_=lhs_: e.matmul(out=dst_[:, 0:HK], lhsT=lhs_[:, :], rhs=sph[:, 0:HK], start=True, stop=False),
                             reads=["at_sph"] + lk_, writes=[akey_])
                        P.op("pe", lambda e, dst_=dst_, lhs_=lhs_: e.matmul(out=dst_[:, 0:HK], lhsT=lhs_[:, :], rhs=spl[:, 0:HK], start=False, stop=True),
                             reads=["at_spl", akey_] + lk_, writes=[akey_])
                    if c.get("gla_cut") == 22:
                        continue
                    P.op("act", lambda e: e.activation(out=EQ[:, 0:HK], in_=bps[:, 0:HK], func=AF.Exp, bias=lnq),
                         reads=[("acc", 2)], writes=["at_EQ"])
                    P.op("act", lambda e: e.activation(out=EKI[:, 0:HK], in_=bps[:, 0:HK], func=AF.Exp, scale=-1.0),
                         reads=[("acc", 2)], writes=["at_EKI"])
                    if c.get("gla_cut") == 23:
                        continue
                    P.op("act", lambda e: e.activation(out=bsb[:, 0:HK], in_=eps_[:, 0:HK], func=AF.Exp), reads=[("acc", 3)], writes=["at_bsb"])
                    P.op("dve", lambda e: e.tensor_tensor(out=EKO[:, 0:HK], in0=bsb[:, 0:HK], in1=EKI[:, 0:HK], op=ALU.mult),
                         reads=["at_bsb", "at_EKI"], writes=["at_EKO"])
                    if c.get("gla_cut") == 2:
                        continue
                    dps = self.pp[2]
                    for p_ in range(npk):
                        P.op("pe", lambda e, p_=p_: e.matmul(out=dps[:, 512 + 2 * p_:512 + 2 * p_ + 2], lhsT=sph[:, p_ * 128:(p_ + 1) * 128], rhs=nsc[:, 0:2],
                                                             start=True, stop=False), reads=["at_sph", "at_nsc"], writes=[("tp", 1)])
                        P.op("pe", lambda e, p_=p_: e.matmul(out=dps[:, 512 + 2 * p_:512 + 2 * p_ + 2], lhsT=spl[:, p_ * 128:(p_ + 1) * 128], rhs=nsc[:, 0:2],
                                                             start=False, stop=True), reads=["at_spl", "at_nsc", ("tp", 1)], writes=[("tp", 1)])
                    P.op("act", lambda e: e.activation(out=dec[:, 0:npk, :], in_=dps[:, 512:512 + 2 * npk].rearrange("p (h x) -> p h x", h=npk), func=AF.Exp),
                         reads=[("tp", 1)], writes=["at_dec"])
                    if c.get("gla_cut") == 3:
                        continue
                    v3 = lambda t: t[:, 0:HK]
                    tq, tki, tko = EQ[:, 0:HK], EKI[:, 0:HK], EKO[:, 0:HK]
                    tkeys = ["at_EQ", "at_EKI", "at_EKO"]
                    decap = dec[:, 0:npk, 0]
                    dkeys = ["at_dec"]
                if need_out:
                    P.op("dve", lambda e, qsrc=qsrc, tq=tq, v3=v3: e.tensor_tensor(out=v3(qin), in0=v3(qsrc), in1=tq, op=ALU.mult),
                         reads=[qk_] + tkeys, writes=["at_qin"])
                    P.op("pool", lambda e, ksrc=ksrc, tki=tki, v3=v3: e.tensor_tensor(out=v3(kin), in0=v3(ksrc), in1=tki, op=ALU.mult),
                         reads=[kk_] + tkeys, writes=["at_kin"])
                P.op("dve", lambda e, ksrc=ksrc, tko=tko, v3=v3: e.tensor_tensor(out=v3(kout), in0=v3(ksrc), in1=tko, op=ALU.mult),
                     reads=[kk_] + tkeys, writes=["at_kout"])
                if c.get("gla_cut") == 4 and kind == "gla":
                    continue
                if need_out:
                    for (src, skey, dstT, dkey, tpi) in ((qin, "at_qin", qT, "at_qT", 0), (kin, "at_kin", kT, "at_kT", 1)):
                        tp = self.tp(tpi)
                        for p_ in range(npk):
                            P.op("pe", lambda e, tp=tp, src=src, p_=p_: e.transpose(out=tp[:, p_ * 128:(p_ + 1) * 128],
                                                                                   in_=src[:, p_ * 128:(p_ + 1) * 128], identity=self.ident[:, :]),
                                 reads=[skey, "ident"], writes=[("tp", tpi)])
                        tpv = tp[:, 0:npk * 128].rearrange("p (h x) -> p h x", h=npk)
                        if hp == 1:
                            P.op("act", lambda e, tpv=tpv, dstT=dstT: e.copy(out=dstT[:, 0:H, :], in_=tpv), reads=[("tp", tpi)], writes=[dkey])
                        else:
                            for j in range(hp):
                                P.op("act", lambda e, tpv=tpv, dstT=dstT, j=j: e.activation(out=dstT[:, j:H:hp, :], in_=tpv, func=AF.Copy, scale=rowm[:, j:j + 1]),
                                     reads=[("tp", tpi), "at_rowm"], writes=[dkey + str(j)])
                    if c.get("gla_cut") == 5 and kind == "gla":
                        continue
                    tkq = ["at_qT"] + [f"at_qT{j}" for j in range(hp)]
                    tkk = ["at_kT"] + [f"at_kT{j}" for j in range(hp)]
                    nbh = 4
                    for bk in range((H + nbh - 1) // nbh):
                        acc = self.accb(bk)
                        hs = list(range(bk * nbh, min(H, (bk + 1) * nbh)))
                        for h in hs:
                            P.op("pe", lambda e, acc=acc, h=h, bk=bk: e.matmul(out=acc[:, (h - bk * nbh) * C:(h - bk * nbh + 1) * C],
                                                                               lhsT=kT[:, h, :], rhs=qT[:, h, :], start=True, stop=True),
                                 reads=tkq + tkk, writes=[("acc", bk)])
                        P.op("dve", lambda e, acc=acc, hs=hs, mask=mask: e.tensor_tensor(
                            out=attT[:, hs[0]:hs[-1] + 1, :], in0=acc[:, 0:len(hs) * C].rearrange("p (h x) -> p h x", h=len(hs)),
                            in1=mask[:, :].unsqueeze(1).broadcast_to([C, len(hs), C]), op=ALU.mult),
                            reads=[("acc", bk), mkey], writes=["at_attT"])
                    if c.get("gla_cut") == 6 and kind == "gla":
                        continue
                    ops_ = self.pp[3]
                    for h in range(H):
                        P.op("pe", lambda e, h=h: e.matmul(out=ops_[:, h * dv:(h + 1) * dv], lhsT=attT[:, h, :],
                                                           rhs=vt[:, h * dv:(h + 1) * dv], start=True, stop=False),
                             reads=["at_attT", "at_v"], writes=["pp3"])
                        P.op("pe", lambda e, h=h: e.matmul(out=ops_[:, h * dv:(h + 1) * dv], lhsT=qT[:, h, :],
                                                           rhs=Sbf[:, (h // hp) * dv:(h // hp + 1) * dv], start=False, stop=True),
                             reads=tkq + ["at_Sbf", "pp3"], writes=["pp3"])
                if c.get("gla_cut") == 7 and kind == "gla":
                    continue
                kvp = self.pp[1]
                for h in range(H):
                    P.op("pe", lambda e, h=h: e.matmul(out=kvp[:, h * dv:(h + 1) * dv], lhsT=kout[:, (h // hp) * 128:(h // hp + 1) * 128],
                                                       rhs=vt[:, h * dv:(h + 1) * dv], start=True, stop=True),
                         reads=["at_kout", "at_v"], writes=[("acc", 2), ("acc", 3)])
                for j in range(hp):
                    rr = slice(j * dk, (j + 1) * dk)
                    s3 = S32[rr, 0:npk * dv].rearrange("p (h x) -> p h x", h=npk)
                    k3 = kvp[rr, 0:HV].rearrange("p (a b x) -> p a b x", a=npk, b=hp)[:, :, j, :]
                    P.op("dve", lambda e, s3=s3, decap=decap, rr=rr: e.tensor_tensor(out=s3, in0=s3, in1=decap[rr, :].unsqueeze(2).broadcast_to([dk, npk, dv]),
                                                                                    op=ALU.mult), reads=["at_S32"] + dkeys, writes=["at_S32"])
                    P.op("dve", lambda e, s3=s3, k3=k3: e.tensor_tensor(out=s3, in0=k3, in1=s3, op=ALU.add),
                         reads=["at_S32", ("acc", 2), ("acc", 3)], writes=["at_S32"])
                P.op("act", lambda e: e.copy(out=Sbf[:, 0:npk * dv], in_=S32[:, 0:npk * dv]), reads=["at_S32", "pp3"], writes=["at_Sbf"])
                if not need_out:
                    continue
                okey = ("OF", mt)
                if d == 0:
                    P.op("act", lambda e: e.copy(out=osb[:, 0:HV], in_=self.pp[3][:, 0:HV]), reads=["pp3"], writes=["S4"])
                    P.dma("sp", OF[r0:r0 + C, :], osb[:, 0:HV], reads=["S4"], writes=[okey])
                    continue
                P.dma("sp", ofl[:, 0:HV], OF[r0:r0 + C, :], reads=[okey], writes=["S5"])
                P.dma("sp", gt[:, 0:HV], PB[r0:r0 + C, go:go + HV], reads=pk, writes=["at_g"])
                P.op("dve", lambda e: e.tensor_tensor(out=osb[:, 0:HV], in0=self.pp[3][:, 0:HV], in1=ofl[:, 0:HV], op=ALU.add),
                     reads=["pp3", "S5"], writes=["S4"])
                if f"OFB{l}" in c["debug"]:
                    if not hasattr(self, "OFB"):
                        self.OFB = self.D_(f"OFB{l}", [c["M"], HV], F32)
                    P.dma("sp", self.OFB[r0:r0 + C, :], osb[:, 0:HV], reads=["S4"], writes=[("OFB", mt)])
                sq = ofl
                ss = P.sb("at_ss", [128, 8], F32)
                P.op("pool", lambda e: e.tensor_tensor(out=sq[:, 0:HV], in0=osb[:, 0:HV], in1=osb[:, 0:HV], op=ALU.mult),
                     reads=["S4"], writes=["S5"])
                P.op("dve", lambda e: e.tensor_reduce(out=ss[:, 0:H], in_=sq[:, 0:HV].rearrange("p (h x) -> p h x", h=H),
                                                      axis=mybir.AxisListType.X, op=ALU.add), reads=["S5"], writes=["at_ss"])
                P.op("dve", lambda e: e.tensor_scalar(out=ss[:, 0:H], in0=ss[:, 0:H], scalar1=1.0 / dv, scalar2=1e-6,
                                                      op0=ALU.mult, op1=ALU.add), reads=["at_ss"], writes=["at_ss"])
                P.op("act", lambda e: e.activation(out=ss[:, 0:H], in_=ss[:, 0:H], func=AF.Sqrt), reads=["at_ss"], writes=["at_ss"])
                P.op("dve", lambda e: e.reciprocal(out=ss[:, 0:H], in_=ss[:, 0:H]), reads=["at_ss"], writes=["at_ss"])
                o3 = osb[:, 0:HV].rearrange("p (h x) -> p h x", h=H)
                P.op("dve", lambda e, o3=o3: e.tensor_tensor(out=o3, in0=o3, in1=ss[:, 0:H].unsqueeze(2).broadcast_to([C, H, dv]), op=ALU.mult),
                     reads=["S4", "at_ss"], writes=["S4"])
                P.op("pool", lambda e: e.tensor_tensor(out=osb[:, 0:HV], in0=osb[:, 0:HV], in1=gn[:, 0:HV], op=ALU.mult),
                     reads=["S4", "at_gn"], writes=["S4"])
                P.op("act", lambda e: e.activation(out=sq[:, 0:HV], in_=gt[:, 0:HV], func=AF.Silu), reads=["at_g"], writes=["S5"])
                ob = P.sb("at_ob", [128, 1024], BF16)
                P.op("dve", lambda e, ob=ob: e.tensor_tensor(out=ob[:, 0:HV], in0=osb[:, 0:HV], in1=sq[:, 0:HV], op=ALU.mult),
                     reads=["S4", "S5"], writes=["at_ob"])
                P.dma("sp", self.MIX[r0:r0 + C, mixo:mixo + HV], ob[:, 0:HV], reads=["at_ob"], writes=[("MIX", mt, kind, 0)])

    def nattn(self, l):
        c, P = self.c, self.P
        PB = self.PB
        NT, NTT, T, L = c["NT"], c["NTT"], c["T"], c["L"]
        NAH, NAD = c["NAH"], c["NAD"]
        nkt = self.nkt()
        Wk = nkt * 128
        qo, ko = 0, c["ODQ"]
        vo = ko + NAD
        pkeys = lambda mt: [("PB", mt, nb) for nb in range((c["ODC"] + 511) // 512)]
        QT = self.D_(f"QT{l}", [NAH, 128, T], BF16)
        KT = self.D_(f"KT{l}", [NAH, 128, c["M"]], BF16)
        cls_of, _ = na_geometry(c)
        for mt in range(NTT):
            for (which, off, dst, scale) in (("q", qo, QT, 128 ** -0.5), ("k", ko, KT, 1.0)):
                if which == "q" and mt >= NT:
                    continue
                src = P.sb(f"na_src{which}", [128, 1024], BF16)
                stg = P.sb(f"na_stg{which}", [128, 8, 128], BF16)
                tpi = 0 if which == "q" else 1
                tp = self.tp(tpi)
                P.dma("sp", src[:, 0:NAD], PB[mt * 128:(mt + 1) * 128, off:off + NAD], reads=pkeys(mt), writes=[f"na_src{which}"])
                for h in range(NAH):
                    P.op("pe", lambda e, tp=tp, src=src, h=h: e.transpose(out=tp[:, h * 128:(h + 1) * 128], in_=src[:, h * 128:(h + 1) * 128], identity=self.ident[:, :]),
                         reads=[f"na_src{which}", "ident"], writes=[("tp", tpi)])
                P.op("act", lambda e, tp=tp, stg=stg, scale=scale: e.activation(out=stg[:, 0:NAH, :], in_=tp[:, 0:NAH * 128].rearrange("p (h x) -> p h x", h=NAH),
                                                                              func=AF.Copy, scale=scale), reads=[("tp", tpi)], writes=[f"na_stg{which}"])
                P.dma("sp", dst[:, :, mt * 128:(mt + 1) * 128].rearrange("h d t -> d h t"), stg[:, 0:NAH, :], reads=[f"na_stg{which}"], writes=[(which + "T", mt)])
        vctx = P.sb("na_vctx", [128, c["NCT"], 1024], BF16)
        kctx = P.sb("na_kctx", [128, 8, L], BF16)
        for j in range(c["NCT"]):
            P.dma("sp", vctx[:, j, 0:NAD], PB[(NT + j) * 128:(NT + j + 1) * 128, vo:vo + NAD], reads=pkeys(NT + j), writes=[("na_vctx", j)])
        P.dma("sp", kctx[:, 0:NAH, :], KT[:, :, T:T + L].rearrange("h d t -> d h t"), reads=[("kT", NT + j) for j in range(c["NCT"])], writes=["na_kctx"])
        vwin = P.sb("na_vwin", [128, 5, 1024], BF16)
        kTh = P.sb("na_kTh", [128, 640], BF16)
        qTh = P.sb("na_qTh", [128, 128], BF16)
        bias = P.sb("na_bias", [128, 640], F32)
        sc = P.sb("na_sc", [128, 896], F32)
        pb = P.sb("na_pb", [128, 896], BF16)
        pT = P.sb("na_pT", [128, 7, 128], BF16)
        osb = P.sb("na_osb", [128, 1024], BF16)
        mx = P.sb("na_mx", [128, 1], F32)
        sm = P.sb("na_sm", [128, 1], F32)
        nj = nkt + c["NCT"]
        for mt in range(NT):
            kt0 = min(max(mt - 2, 0), NT - nkt)
            for j in range(nkt):
                P.dma("sp", vwin[:, j, 0:NAD], PB[(kt0 + j) * 128:(kt0 + j + 1) * 128, vo:vo + NAD], reads=pkeys(kt0 + j), writes=[("na_vwin", j)])
            for h in range(NAH):
                P.dma("sp", qTh[:, :], QT[h, :, mt * 128:(mt + 1) * 128], reads=[("qT", mt)], writes=["na_qTh"])
                P.dma("sp", kTh[:, 0:Wk], KT[h, :, kt0 * 128:kt0 * 128 + Wk], reads=[("kT", kt0 + j) for j in range(nkt)], writes=["na_kTh"])
                P.dma("sp", bias[:, 0:Wk], self.small[f"na_bias{l}"][cls_of[mt], h, :, :], writes=["na_bias"])
                sps = self.pp[0]
                for c0 in range(0, Wk, 512):
                    cn = min(512, Wk - c0)
                    P.op("pe", lambda e, c0=c0, cn=cn: e.matmul(out=sps[:, c0:c0 + cn], lhsT=qTh[:, :], rhs=kTh[:, c0:c0 + cn], start=True, stop=True),
                         reads=["na_qTh", "na_kTh"], writes=[("acc", c0 // 512)])
                P.op("pe", lambda e, h=h: e.matmul(out=sps[:, Wk:Wk + L], lhsT=qTh[:, :], rhs=kctx[:, h, :], start=True, stop=True),
                     reads=["na_qTh", "na_kctx", ("acc", 1)], writes=[("acc", 1)])
                P.op("dve", lambda e: e.tensor_tensor(out=sc[:, 0:Wk], in0=sps[:, 0:Wk], in1=bias[:, 0:Wk], op=ALU.add),
                     reads=[("acc", 0), ("acc", 1), "na_bias"], writes=["na_sc"])
                P.op("act", lambda e: e.copy(out=sc[:, Wk:Wk + L], in_=sps[:, Wk:Wk + L]), reads=[("acc", 1)], writes=["na_scc"])
                P.op("dve", lambda e: e.tensor_reduce(out=mx[:], in_=sc[:, 0:Wk + L], axis=mybir.AxisListType.X, op=ALU.max), reads=["na_sc", "na_scc"], writes=["na_mx"])
                P.op("dve", lambda e: e.tensor_scalar(out=mx[:], in0=mx[:], scalar1=-1.0, scalar2=None, op0=ALU.mult), reads=["na_mx"], writes=["na_mx"])
                P.op("act", lambda e: e.activation(out=pb[:, 0:Wk + L], in_=sc[:, 0:Wk + L], func=AF.Exp, bias=mx[:, 0:1], accum_out=sm[:]),
                     reads=["na_sc", "na_scc", "na_mx"], writes=["na_pb", "na_sm"])
                P.op("dve", lambda e: e.reciprocal(out=sm[:], in_=sm[:]), reads=["na_sm"], writes=["na_sm"])
                tp = self.tp(0)
                for j in range(nj):
                    P.op("pe", lambda e, j=j: e.transpose(out=tp[:, j * 128:(j + 1) * 128], in_=pb[:, j * 128:(j + 1) * 128], identity=self.ident[:, :]),
                         reads=["na_pb", "ident"], writes=[("tp", 0)])
                P.op("act", lambda e: e.copy(out=pT[:, 0:nj, :], in_=tp[:, 0:nj * 128].rearrange("p (j x) -> p j x", j=nj)), reads=[("tp", 0)], writes=["na_pT"])
                ops_ = self.accb(2)
                for j in range(nj):
                    rhs = vwin[:, j, h * 128:(h + 1) * 128] if j < nkt else vctx[:, j - nkt, h * 128:(h + 1) * 128]
                    rk = ("na_vwin", j) if j < nkt else ("na_vctx", j - nkt)
                    P.op("pe", lambda e, j=j, rhs=rhs: e.matmul(out=ops_[:, 0:128], lhsT=pT[:, j, :], rhs=rhs, start=(j == 0), stop=(j == nj - 1)),
                         reads=["na_pT", rk], writes=[("acc", 2)])
                P.op("dve", lambda e, h=h: e.tensor_scalar(out=osb[:, h * 128:(h + 1) * 128], in0=ops_[:, 0:128], scalar1=sm[:, 0:1], scalar2=None, op0=ALU.mult),
                     reads=[("acc", 2), "na_sm"], writes=[("na_osb", h)])
            P.dma("sp", self.MIX[mt * 128:(mt + 1) * 128, 0:NAD], osb[:, 0:NAD], reads=[("na_osb", h) for h in range(NAH)], writes=[("MIX", mt, "na", 0)])

    def gmlp(self, l, last):
        c, P = self.c, self.P
        PB = self.PB
        GD, GW, RD = c["GD"], c["GW"], c["RD"]
        uo, vo = 2 * RD, 2 * RD + GD
        wsT = P.sb("gm_wsT", [128, 8, 128], BF16)
        wsf = P.sb("S0", [128, 2048], F32)
        P.dma("sp", wsf[:, 0:1024].rearrange("p (g x) -> p g x", g=8), self.small[f"gmlp_wsT{l}"].rearrange("g s p -> s g p"), writes=["S0"])
        P.op("dve", lambda e: e.tensor_copy(out=wsT[:], in_=wsf[:, 0:1024].rearrange("p (g x) -> p g x", g=8)), reads=["S0"], writes=["gm_wsT"])
        bsT = P.sb("gm_bsT", [128, 8], F32)
        P.dma("sp", bsT[:], self.small[f"gmlp_bsT{l}"], writes=["gm_bsT"])
        gng = P.sb("S1", [128, 2048], F32)
        P.dma("sp", gng[:, 0:GD], self.small[f"gmlp_norm{l}"].partition_broadcast(128), writes=["S1"])
        ntl = c["NT"] if last else c["NTT"]
        K2 = 2 * math.sqrt(2.0 / math.pi)
        for mt in range(ntl):
            pk = [("PB", mt, nb) for nb in range(c["EVC"] // 512)]
            r0 = mt * 128
            raw = P.sb("at_q", [128, 1024], BF16)
            raw2 = P.sb("at_k", [128, 1024], BF16)
            P.dma("sp", raw[:, 0:GD], PB[r0:r0 + 128, uo:uo + GD], reads=pk, writes=["at_q"])
            P.dma("sp", raw2[:, 0:GD], PB[r0:r0 + 128, vo:vo + GD], reads=pk, writes=["at_k"])
            gl = {}
            for (src, skey, dname) in ((raw, "at_q", "S2"), (raw2, "at_k", "S3")):
                dst = P.sb(dname, [128, 2048], F32)
                a, b = dst[:, 0:GD], dst[:, 1024:1024 + GD]
                eng = "dve" if dname == "S2" else "pool"
                P.op(eng, lambda e, a=a, src=src: e.tensor_tensor(out=a, in0=src[:, 0:GD], in1=src[:, 0:GD], op=ALU.mult), reads=[skey], writes=[dname])
                P.op(eng, lambda e, a=a: e.tensor_scalar(out=a, in0=a, scalar1=0.044715, scalar2=1.0, op0=ALU.mult, op1=ALU.add), reads=[dname], writes=[dname])
                P.op(eng, lambda e, a=a, src=src: e.tensor_tensor(out=a, in0=a, in1=src[:, 0:GD], op=ALU.mult), reads=[dname, skey], writes=[dname])
                P.op("act", lambda e, a=a, b=b: e.activation(out=b, in_=a, func=AF.Sigmoid, scale=K2), reads=[dname], writes=[dname + "b"])
                P.op(eng, lambda e, a=a, b=b, src=src: e.tensor_tensor(out=a, in0=b, in1=src[:, 0:GD], op=ALU.mult), reads=[dname + "b", skey], writes=[dname])
                gl[dname] = a
            ug, vg = gl["S2"], gl["S3"]
            ss = P.sb("nt_ss", [128, 1], F32)
            rs = P.sb("nt_rs", [128, 1], F32)
            junk = P.sb("nt_junk", [128, 2048], BF16)
            P.op("act", lambda e: e.activation(out=junk[:, 0:GD], in_=vg, func=AF.Square, accum_out=ss[:]), reads=["S3"], writes=["nt_junk", "nt_ss"])
            P.op("dve", lambda e: e.tensor_scalar(out=rs[:], in0=ss[:], scalar1=1.0 / GD, scalar2=1e-6, op0=ALU.mult, op1=ALU.add), reads=["nt_ss"], writes=["nt_rs"])
            P.op("act", lambda e: e.activation(out=rs[:], in_=rs[:], func=AF.Sqrt), reads=["nt_rs"], writes=["nt_rs"])
            P.op("dve", lambda e: e.reciprocal(out=rs[:], in_=rs[:]), reads=["nt_rs"], writes=["nt_rs"])
            vn = P.sb("at_v", [128, 1024], BF16)
            P.op("dve", lambda e: e.scalar_tensor_tensor(out=vn[:, 0:GD], in0=vg, scalar=rs[:, 0:1], in1=gng[:, 0:GD], op0=ALU.mult, op1=ALU.mult),
                 reads=["S3", "nt_rs", "S1"], writes=["at_v"])
            ob = P.sb("at_ob", [128, 1024], BF16)
            for g in range(8):
                bank = g * GW // 512
                acc = self.accb(bank)
                col = g * GW - bank * 512
                P.op("pe", lambda e, acc=acc, g=g, col=col: e.matmul(out=acc[:, col:col + GW], lhsT=wsT[:, g, :], rhs=vn[:, g * GW:(g + 1) * GW], start=True, stop=True),
                     reads=["gm_wsT", "at_v"], writes=[("acc", bank)])
                P.op("dve", lambda e, acc=acc, g=g, col=col: e.scalar_tensor_tensor(out=ob[:, g * GW:(g + 1) * GW], in0=acc[:, col:col + GW], scalar=bsT[:, g:g + 1],
                                                                                    in1=ug[:, g * GW:(g + 1) * GW], op0=ALU.add, op1=ALU.mult),
                     reads=[("acc", bank), "gm_bsT", "S2"], writes=["at_ob"])
            P.dma("sp", self.MIX[r0:r0 + 128, RD:RD + GD], ob[:, 0:GD], reads=["at_ob"], writes=[("MIX", mt, "gmlp", 0)])

    def phase_final(self):
        c, P = self.c, self.P
        D = c["D"]
        P.push()
        g = P.sb("S0", [128, 2048], F32)
        P.dma("sp", g[:, 0:D], self.small["norm_final"].partition_broadcast(128), writes=["S0"])
        for mt in range(c["NT"]):
            i = mt % 2
            ob = P.sb(f"S{1 + i}", [128, 2048], F32)
            xt = P.sb("S5", [128, 2048], F32)
            junk = P.sb("nt_junk", [128, 2048], BF16)
            ss = P.sb("nt_ss", [128, 1], F32)
            rs = P.sb("nt_rs", [128, 1], F32)
            P.dma("sp", xt[:, 0:D], self.XC[mt * 128:(mt + 1) * 128, :], reads=[("XC", mt)], writes=["S5"])
            P.op("act", lambda e: e.activation(out=junk[:, 0:D], in_=xt[:, 0:D], func=AF.Square, accum_out=ss[:]),
                 reads=["S5"], writes=["nt_junk", "nt_ss"])
            P.op("dve", lambda e: e.tensor_scalar(out=rs[:], in0=ss[:], scalar1=1.0 / D, scalar2=1e-6,
                                                  op0=ALU.mult, op1=ALU.add), reads=["nt_ss"], writes=["nt_rs"])
            P.op("act", lambda e: e.activation(out=rs[:], in_=rs[:], func=AF.Sqrt), reads=["nt_rs"], writes=["nt_rs"])
            P.op("dve", lambda e: e.reciprocal(out=rs[:], in_=rs[:]), reads=["nt_rs"], writes=["nt_rs"])
            P.op("dve", lambda e, ob=ob: e.scalar_tensor_tensor(out=ob[:, 0:D], in0=xt[:, 0:D], scalar=rs[:, 0:1], in1=g[:, 0:D],
                                                                op0=ALU.mult, op1=ALU.mult),
                 reads=["S5", "nt_rs", "S0"], writes=[f"S{1 + i}"])
            P.dma("sp", self.out[mt * 128:(mt + 1) * 128, :], ob[:, 0:D], reads=[f"S{1 + i}"], writes=[("out", mt)])
        P.pop()


def na_geometry(c):
    NT, rows = c["NT"], c["rows"]
    nkt = min(5, NT)
    Wk = nkt * 128
    wr = min(8, rows)
    uniq, cls_of, maps = {}, [], []
    for mt in range(NT):
        kt0 = min(max(mt - 2, 0), NT - nkt)
        m = np.full((128, Wk), -1, np.int64)
        for p in range(128):
            t = mt * 128 + p
            r, col = t // 64, t % 64
            rstart = min(max(r - wr // 2, 0), rows - wr)
            cstart = min(max(col - 8, 0), 64 - 16)
            for r2 in range(rstart, rstart + wr):
                kk0 = r2 * 64 - kt0 * 128
                dr = r2 - r + 7
                for c2 in range(cstart, cstart + 16):
                    kk = kk0 + c2
                    assert 0 <= kk < Wk
                    m[p, kk] = dr * 31 + (c2 - col + 15)
        key = m.tobytes()
        if key not in uniq:
            uniq[key] = len(maps)
            maps.append(m)
        cls_of.append(uniq[key])
    return cls_of, maps


def host_inputs(c, inputs, ncores):
    f32 = lambda a: np.ascontiguousarray(a, dtype=np.float32)
    E, D, F = c["E"], c["D"], c["F"]
    shared = {"norm_final": f32(inputs["norm_final"])[None, :]}
    for l in range(c["depth"]):
        li = l // 2
        shared[f"ada{l}"] = f32(inputs["ada_w"][l])
        shared[f"win{l}"] = f32(inputs["ev_w_in"][li] if l % 2 == 0 else inputs["od_w_in"][li])
        shared[f"wout{l}"] = f32(inputs["ev_w_out"][li] if l % 2 == 0 else inputs["od_w_out"][li])
        shared[f"wg{l}"] = f32(inputs["moe_w_gate"][l]).reshape(E * D, F)
        shared[f"wu{l}"] = f32(inputs["moe_w_up"][l]).reshape(E * D, F)
        shared[f"wd{l}"] = f32(inputs["moe_w_down"][l]).reshape(E * F, D)
        shared[f"ada_b{l}"] = f32(inputs["ada_b"][l])[None, :]
        shared[f"norm_mix{l}"] = f32(inputs["norm_mix"][l])[None, :]
        shared[f"norm_ffn{l}"] = f32(inputs["norm_ffn"][l])[None, :]
        shared[f"router{l}"] = f32(inputs["router_w"][l])
        if l % 2 == 0:
            shared[f"gamma{l}"] = f32(inputs["ret_gamma_logit"][li]).reshape(1, -1)
            shared[f"ret_norm{l}"] = f32(inputs["ret_norm"][li])[None, :]
            shared[f"gmlp_norm{l}"] = f32(inputs["gmlp_norm"][li])[None, :]
            shared[f"gmlp_wsT{l}"] = f32(np.transpose(inputs["gmlp_ws"][li], (0, 2, 1)))
            shared[f"gmlp_bsT{l}"] = f32(np.transpose(inputs["gmlp_bs"][li], (1, 0)))
        else:
            shared[f"gla_wup{l}"] = f32(inputs["gla_w_up"][li])
            shared[f"gla_bup{l}"] = f32(inputs["gla_b_up"][li])[None]
            shared[f"gla_norm{l}"] = f32(inputs["gla_norm"][li])[None, :]
            cls_of, maps = na_geometry(c)
            rpb = f32(inputs["na_rpb"][li]).reshape(c["NAH"], -1)
            tabs = np.empty((6, c["NAH"], 128, maps[0].shape[1]), np.float32)
            tabs[:] = NEG
            for ci, m in enumerate(maps):
                valid = m >= 0
                for h in range(c["NAH"]):
                    tabs[ci, h][valid] = rpb[h][m[valid]]
            shared[f"na_bias{l}"] = tabs
    T = c["T"]
    t = np.arange(T)
    freq = (10000.0 ** (-np.arange(32, dtype=np.float32) / 32)).astype(np.float32)
    angr = ((t // 64).astype(np.float32)[:, None] * freq).astype(np.float32)
    angc = ((t % 64).astype(np.float32)[:, None] * freq).astype(np.float32)
    shared["rot_cs"] = np.concatenate([np.cos(angr), np.cos(angr), np.cos(angc), np.cos(angc)], 1).astype(np.float32)
    shared["rot_sn"] = np.concatenate([-np.sin(angr), np.sin(angr), -np.sin(angc), np.sin(angc)], 1).astype(np.float32)
    maps = []
    for core in range(ncores):
        s = core % 4
        m = dict(shared)
        m.update({"x": f32(inputs["x"][s]), "ctx": f32(inputs["ctx"][s]),
                  "cc": np.stack([inputs["c"][s], inputs["c_ctx"]]).astype(np.float32)})
        maps.append(m)
    return maps


def run(c, inputs):
    kb = K(c)
    nc = kb.build()
    maps = host_inputs(c, {k: np.asarray(v) for k, v in inputs.items()}, NCORES)
    res = run_bass_kernel_spmd(nc, maps, core_ids=list(range(NCORES)))
    return kb, res


def kernel(**inputs):
    c = make_cfg()
    kb, res = run(c, inputs)
    return np.stack([res.results[s]["out"] for s in range(4)]).astype(np.float32)
```

```python
import math
import numpy as np
from contextlib import ExitStack
import concourse.bass as bass
import concourse.mybir as mybir
from concourse.bass_utils import run_bass_kernel_spmd

F32 = mybir.dt.float32
BF16 = mybir.dt.bfloat16
I32 = mybir.dt.int32
U32 = mybir.dt.uint32
ALU = mybir.AluOpType
AF = mybir.ActivationFunctionType
ENGS = ("pe", "act", "dve", "pool", "sp")
NCORES = 4
BIG = 60000.0
NEG = -30000.0


class Prog:
    def __init__(self):
        self.nc = bass.Bass("TRN2", target_bir_lowering=False)
        self.es = ExitStack()
        self.ops = []
        self.n_dma_sems = 12
        self.bufs = {}
        self.arena = None

    def dram(self, name, shape, dt, kind="Internal"):
        return self.nc.dram_tensor(name, list(shape), dt, kind=kind)

    ARENA_BYTES = 176 * 1024

    def sb(self, name, shape, dt):
        if name in self.bufs:
            return self.bufs[name]
        if self.arena is None:
            self.arena = self.es.enter_context(self.nc.sbuf_tensor("arena", [128, self.ARENA_BYTES // 4], F32))
            self.bump = 0
            self.scopes = []
        esz = mybir.dt.size(dt)
        n = 1
        for d in shape[1:]:
            n *= d
        nbytes = (n * esz + 31) // 32 * 32
        assert self.bump + nbytes <= self.ARENA_BYTES, (name, self.bump, nbytes)
        v = self.arena[0:shape[0], self.bump // 4:(self.bump + nbytes) // 4]
        if dt != F32:
            v = v.bitcast(dt)
        v = v[:, 0:n]
        if len(shape) == 3:
            v = v.rearrange("p (a b) -> p a b", a=shape[1])
        elif len(shape) == 4:
            v = v.rearrange("p (a b c) -> p a b c", a=shape[1], b=shape[2])
        self.bump += nbytes
        self.peak = max(getattr(self, "peak", 0), self.bump)
        self.bufs[name] = v
        if self.scopes:
            self.scopes[-1][1].append(name)
        return v

    def push(self):
        if self.arena is None:
            self.sb("_dummy", [128, 8], F32)
        self.scopes.append((self.bump, []))

    def pop(self):
        bump, names = self.scopes.pop()
        for n in names:
            del self.bufs[n]
        self.bump = bump
        self.ops.append(("*", None, (), (), "bar"))

    def ps(self, name, shape, dt=F32):
        if name not in self.bufs:
            self.bufs[name] = self.es.enter_context(self.nc.psum_tensor(name, list(shape), dt))
        return self.bufs[name]

    def op(self, eng, fn, reads=(), writes=(), dma=False):
        self.ops.append((eng, fn, tuple(reads), tuple(writes), dma))

    def dma(self, q, out, in_, reads=(), writes=(), **kw):
        self.op(q, lambda e: e.dma_start(out=out, in_=in_, **kw), reads, writes, dma=True)

    def emit(self):
        nc, es = self.nc, self.es
        sem_c = {e: es.enter_context(nc.semaphore("s_" + e)) for e in ENGS}
        sem_d = {e: [es.enter_context(nc.semaphore(f"d_{e}{i}")) for i in range(self.n_dma_sems)]
                 for e in ("sp", "act", "pool")}
        sem_cc = es.enter_context(nc.semaphore("s_cc"))
        cnt_cc = [0]
        cnt_c = {e: 0 for e in ENGS}
        cnt_d = {e: [0] * self.n_dma_sems for e in sem_d}
        rr = {e: 0 for e in sem_d}
        last_w, readers = {}, {}
        known = {e: {} for e in ENGS}
        streams = {e: [] for e in ENGS}
        for (eng, fn, reads, writes, is_dma) in self.ops:
            if is_dma == "bar":
                alltok = [(sem_c[e], cnt_c[e]) for e in ENGS if cnt_c[e]]
                alltok += [(sem_d[e][k], cnt_d[e][k] * 16) for e in sem_d for k in range(self.n_dma_sems) if cnt_d[e][k]]
                if cnt_cc[0]:
                    alltok.append((sem_cc, cnt_cc[0]))
                for e in ENGS:
                    need = [(s_, v_) for (s_, v_) in alltok if known[e].get(id(s_), 0) < v_]
                    for (s_, v_) in need:
                        known[e][id(s_)] = v_
                    if need:
                        streams[e].append((need, None, None, 0))
                last_w, readers = {}, {}
                continue
            toks = []
            for r in reads:
                if r in last_w:
                    toks.append(last_w[r])
            for w in writes:
                if w in last_w:
                    toks.append(last_w[w])
                toks.extend(readers.get(w, ()))
            if is_dma == "cc":
                sem = sem_cc
                cnt_cc[0] += 1
                tok = (sem, cnt_cc[0])
                inc = 1
            elif is_dma:
                k = rr[eng]
                rr[eng] = (k + 1) % self.n_dma_sems
                sem = sem_d[eng][k]
                if cnt_d[eng][k] > 0:
                    toks.append((sem, cnt_d[eng][k] * 16))
                cnt_d[eng][k] += 1
                tok = (sem, cnt_d[eng][k] * 16)
                inc = 16
            else:
                sem = sem_c[eng]
                cnt_c[eng] += 1
                tok = (sem, cnt_c[eng])
                inc = 1
            need = {}
            for (s, v) in toks:
                if known[eng].get(id(s), 0) >= v:
                    continue
                if need.get(id(s), (None, 0))[1] < v:
                    need[id(s)] = (s, v)
            for (s, v) in need.values():
                known[eng][id(s)] = v
            streams[eng].append((list(need.values()), fn, sem, inc))
            for r in reads:
                readers.setdefault(r, []).append(tok)
            for w in writes:
                last_w[w] = tok
                readers[w] = []
        fin = []
        for e in ENGS:
            if cnt_c[e]:
                fin.append((sem_c[e], cnt_c[e]))
        for e in sem_d:
            for k in range(self.n_dma_sems):
                if cnt_d[e][k]:
                    fin.append((sem_d[e][k], cnt_d[e][k] * 16))
        if cnt_cc[0]:
            fin.append((sem_cc, cnt_cc[0]))
        self.n_instr = {e: len(streams[e]) for e in ENGS}
        self.n_instr['peak_sbuf'] = getattr(self, 'peak', 0)
        with nc.Block() as block:
            def mk(e):
                def body(engine):
                    for (waits, fn, sem, inc) in streams[e]:
                        for (s, v) in waits:
                            engine.wait_ge(s, v)
                        if fn is not None:
                            fn(engine).then_inc(sem, inc)
                    if e == "sp":
                        for (s, v) in fin:
                            engine.wait_ge(s, v)
                return body
            block.tensor(mk("pe"))
            block.scalar(mk("act"))
            block.vector(mk("dve"))
            block.gpsimd(mk("pool"))
            block.sync(mk("sp"))
        self.es.close()
        return nc


def make_cfg(T=4096, L=256, D=2048, F=2048, E=16, depth=2, debug=()):
    c = dict(T=T, L=L, D=D, F=F, E=E, depth=depth, debug=tuple(debug))
    c["NT"], c["NCT"] = T // 128, L // 128
    c["NTT"] = c["NT"] + c["NCT"]
    c["M"] = T + L
    c["HD"] = 128
    c["RH"] = D // 2 // 128
    c["RD"] = c["RH"] * 128
    c["GD"] = D - c["RD"]
    c["GG"] = 8
    c["GW"] = c["GD"] // 8
    c["NAH"] = D // 2 // 128
    c["NAD"] = c["NAH"] * 128
    c["GH"] = 8
    c["GDV"] = (D - c["NAD"]) // 8
    c["GDK"] = c["GDV"] // 2
    c["GQK"] = 8 * c["GDK"]
    c["GV"] = 8 * c["GDV"]
    c["EVC"] = 4 * c["RD"] + 2 * c["GD"]
    c["ODQ"] = c["NAD"] + c["GQK"] + c["GV"]
    c["ODC"] = c["ODQ"] + 2 * c["NAD"] + c["GQK"] + c["GV"] + 32
    c["capL"] = 2 * T // E
    c["capC"] = 2 * L // E
    c["rows"] = T // 64
    return c


def big_layout(c):
    D, F, E = c["D"], c["F"], c["E"]
    items = []
    for l in range(c["depth"]):
        items.append((f"ada{l}", D, 6 * D))
        items.append((f"win{l}", D, c["EVC"] if l % 2 == 0 else c["ODC"]))
        items.append((f"wout{l}", D, D))
        for e in range(E):
            items.append((f"wg{l}_{e}", D, F))
            items.append((f"wu{l}_{e}", D, F))
            items.append((f"wd{l}_{e}", F, D))
    off, table = 0, {}
    for (n, K, N) in items:
        table[n] = (off, K, N)
        off += K * N
    CH = 8 * 2048 * 16
    tot = (off + CH - 1) // CH * CH
    return table, tot


class K:
    def __init__(self, c):
        self.c = c
        self.P = Prog()
        self.nc = self.P.nc
        self.dbg = {}

    def D_(self, name, shape, dt, kind=None):
        if kind is None:
            kind = "ExternalOutput" if name in self.c["debug"] else "Internal"
        t = self.P.dram(name, shape, dt, kind)
        if kind == "ExternalOutput":
            self.dbg[name] = t
        return t.ap()

    def bcreg(self, eng, val):
        if not hasattr(self, "_bcregs"):
            self._bcregs = {}
        if val not in self._bcregs:
            self._bcregs[val] = eng.to_reg(val)
        return self._bcregs[val]

    def nkt(self):
        return min(5, self.c["NT"])

    def accb(self, i):
        return self.pp[i // 2][:, (i % 2) * 512:(i % 2) * 512 + 512]

    def tp(self, i):
        return self.pp[2][:].bitcast(BF16)[:, i * 1024:(i + 1) * 1024]

    def linear(self, tag, tiles, Kd, N, producer, W, epilogue, G=4, wkey=()):
        P = self.P
        KC = Kd // 128
        ident = self.ident
        NB = (N + 511) // 512
        ngrp = (len(tiles) + G - 1) // G
        for g in range(ngrp):
            grp = tiles[g * G:(g + 1) * G]
            xT = P.sb("lin_xT", [128, 16, max(G, 5) * 128], BF16)
            for j, (mt, rows) in enumerate(grp):
                i = g * G + j
                xb = P.sb(f"lin_xbf{i % 2}", [128, 2048], BF16)
                xkey = ("xbf", i % 2)
                producer(i, mt, rows, xb, xkey)
                for kc in range(KC):
                    tpi = (i * KC + kc) % 2
                    tp = self.tp(tpi)
                    P.op("pe", lambda e, tp=tp, xb=xb, kc=kc, rows=rows: e.transpose(
                        out=tp[:, 0:rows], in_=xb[0:rows, kc * 128:(kc + 1) * 128], identity=ident[0:rows, 0:rows]),
                        reads=[xkey, "ident"], writes=[("tp", tpi)])
                    ce = "act" if kc % 2 == 0 else "dve"
                    if ce == "act":
                        P.op("act", lambda e, tp=tp, kc=kc, j=j, rows=rows: e.copy(
                            out=xT[:, kc, j * 128:j * 128 + rows], in_=tp[:, 0:rows]),
                            reads=[("tp", tpi)], writes=[("xT", kc, j)])
                    else:
                        P.op("dve", lambda e, tp=tp, kc=kc, j=j, rows=rows: e.tensor_copy(
                            out=xT[:, kc, j * 128:j * 128 + rows], in_=tp[:, 0:rows]),
                            reads=[("tp", tpi)], writes=[("xT", kc, j)])
            def load_block(nb):
                ncols = min(512, N - nb * 512)
                wi = self.wcnt % 2
                self.wcnt += 1
                wb = P.sb(f"lin_wb{wi}", [128, 16, 512], BF16)
                for hf in range((KC + 7) // 8):
                    k0, k1 = hf * 8, min(KC, hf * 8 + 8)
                    si = self.scnt % 2
                    self.scnt += 1
                    wst = P.sb(f"lin_wst{si}", [128, 8, 512], F32)
                    src = W[k0 * 128:k1 * 128, nb * 512:nb * 512 + ncols].rearrange("(c p) n -> p c n", p=128)
                    P.dma("sp", wst[:, 0:k1 - k0, 0:ncols], src, reads=list(wkey), writes=[("wst", si)])
                    ce = "pool"
                    if ce == "pool":
                        P.op("pool", lambda e, wb=wb, wst=wst, k0=k0, k1=k1, ncols=ncols: e.tensor_copy(
                            out=wb[:, k0:k1, 0:ncols], in_=wst[:, 0:k1 - k0, 0:ncols]),
                            reads=[("wst", si)], writes=[("wb", wi, hf)])
                    else:
                        P.op("act", lambda e, wb=wb, wst=wst, k0=k0, k1=k1, ncols=ncols: e.copy(
                            out=wb[:, k0:k1, 0:ncols], in_=wst[:, 0:k1 - k0, 0:ncols]),
                            reads=[("wst", si)], writes=[("wb", wi, hf)])
                return (wb, wi, ncols)
            cur = load_block(0)
            for nb in range(NB):
                nxt = load_block(nb + 1) if nb + 1 < NB else None
                wb, wi, ncols = cur
                cur = nxt
                for j, (mt, rows) in enumerate(grp):
                    i = g * G + j
                    ai = self.acnt % 4
                    self.acnt += 1
                    acc = self.accb(ai)
                    for kc in range(KC):
                        P.op("pe", lambda e, acc=acc, kc=kc, j=j, rows=rows, wb=wb, ncols=ncols: e.matmul(
                            out=acc[0:rows, 0:ncols], lhsT=xT[:, kc, j * 128:j * 128 + rows], rhs=wb[:, kc, 0:ncols],
                            start=(kc == 0), stop=(kc == KC - 1)),
                            reads=[("xT", kc, j), ("wb", wi, kc // 8)], writes=[("acc", ai)])
                    epilogue(i, mt, rows, nb, ncols, acc, ("acc", ai))

    def setup_consts(self):
        P = self.P
        self.wcnt = self.scnt = self.acnt = 0
        identf = P.sb("identf", [128, 128], F32)
        ident = P.sb("ident", [128, 128], BF16)
        P.op("pool", lambda e: e.memset(identf[:], 0.0), writes=["identf"])
        P.op("pool", lambda e: e.affine_select(out=identf[:], in_=identf[:], pattern=[[-1, 128]],
                                               compare_op=ALU.not_equal, fill=1.0, base=0, channel_multiplier=1),
             reads=["identf"], writes=["identf"])
        P.op("dve", lambda e: e.tensor_copy(out=ident[:], in_=identf[:]), reads=["identf"], writes=["ident"])
        self.ident, self.identf = ident, identf
        self.pp = [P.ps(f"pp{i}", [128, 1024], F32) for i in range(4)]
        self.consts_attn()

    def build(self):
        c, P, nc = self.c, self.P, self.nc
        D, M, T, L = c["D"], c["M"], c["T"], c["L"]
        table, tot = big_layout(c)
        self.table = table
        EI = lambda n, s, dt=F32: P.dram(n, s, dt, "ExternalInput").ap()
        self.x_in = EI("x", [T, D])
        self.ctx_in = EI("ctx", [L, D])
        self.cc_in = EI("cc", [2, D])
        self.small = {}
        for l in range(c["depth"]):
            self.small[f"ada_b{l}"] = EI(f"ada_b{l}", [1, 6 * D])
            self.small[f"norm_mix{l}"] = EI(f"norm_mix{l}", [1, D])
            self.small[f"norm_ffn{l}"] = EI(f"norm_ffn{l}", [1, D])
            self.small[f"router{l}"] = EI(f"router{l}", [D, c["E"]])
        self.small["norm_final"] = EI("norm_final", [1, D])
        for l in range(c["depth"]):
            if l % 2 == 0:
                self.small[f"gamma{l}"] = EI(f"gamma{l}", [1, 2 * c["RH"]])
                self.small[f"ret_norm{l}"] = EI(f"ret_norm{l}", [1, c["RD"]])
                self.small[f"gmlp_norm{l}"] = EI(f"gmlp_norm{l}", [1, c["GD"]])
                self.small[f"gmlp_wsT{l}"] = EI(f"gmlp_wsT{l}", [8, 128, 128])
                self.small[f"gmlp_bsT{l}"] = EI(f"gmlp_bsT{l}", [128, 8])
            else:
                self.small[f"gla_wup{l}"] = EI(f"gla_wup{l}", [2, 16, c["GQK"]])
                self.small[f"gla_bup{l}"] = EI(f"gla_bup{l}", [1, 2, c["GQK"]])
                self.small[f"gla_norm{l}"] = EI(f"gla_norm{l}", [1, c["GV"]])
                self.small[f"na_bias{l}"] = EI(f"na_bias{l}", [6, c["NAH"], 128, self.nkt() * 128])
        self.small["rot_cs"] = EI("rot_cs", [T, 128])
        self.small["rot_sn"] = EI("rot_sn", [T, 128])
        self.out = P.dram("out", [T, D], F32, "ExternalOutput").ap()
        self.setup_consts()
        E, F = c["E"], c["F"]
        self.W = {}
        for l in range(c["depth"]):
            self.W[f"ada{l}"] = EI(f"ada{l}", [D, 6 * D])
            self.W[f"win{l}"] = EI(f"win{l}", [D, c["EVC"] if l % 2 == 0 else c["ODC"]])
            self.W[f"wout{l}"] = EI(f"wout{l}", [D, D])
            wg = EI(f"wg{l}", [E * D, F]); wu = EI(f"wu{l}", [E * D, F]); wd = EI(f"wd{l}", [E * F, D])
            for e in range(E):
                self.W[f"wg{l}_{e}"] = wg[e * D:(e + 1) * D, :]
                self.W[f"wu{l}_{e}"] = wu[e * D:(e + 1) * D, :]
                self.W[f"wd{l}_{e}"] = wd[e * F:(e + 1) * F, :]
        self.Wm = lambda name: self.W[name]
        self.XC = self.D_("XC", [M, D], F32)
        self.X1 = self.D_("X1", [M, D], F32)
        for mt in range(c["NTT"]):
            src = self.x_in[mt * 128:(mt + 1) * 128, :] if mt < c["NT"] else \
                self.ctx_in[(mt - c["NT"]) * 128:(mt - c["NT"] + 1) * 128, :]
            P.dma("sp", self.XC[mt * 128:(mt + 1) * 128, :], src, writes=[("XC", mt)])
        self.phase_mod()
        for l in range(c["depth"]):
            self.layer(l)
            if f"stop_l{l}" in c["debug"]:
                break
        self.phase_final()
        return P.emit()

    def phase_mod(self):
        c, P = self.c, self.P
        D = c["D"]
        self.MOD = []
        P.push()
        ccs = P.sb("S5", [128, 2048], F32)
        P.op("pool", lambda e: e.memset(ccs[:], 0.0), writes=["S5"])
        P.dma("sp", ccs[0:2, 0:D], self.cc_in[:, :], reads=["S5"], writes=["S5"])
        for l in range(c["depth"]):
            MODl = self.D_(f"MOD{l}", [2, 6 * D], F32)
            self.MOD.append(MODl)
            bias = None

            def prod(i, mt, rows, xb, xkey):
                P.op("act", lambda e: e.activation(out=xb[:, 0:D], in_=ccs[:, 0:D], func=AF.Silu),
                     reads=["S5"], writes=[xkey])

            def epi(i, mt, rows, nb, ncols, acc, akey, l=l, MODl=MODl, bias=bias):
                st = P.sb("mod_st", [2, 512], F32)
                bst = P.sb("mod_bst", [2, 512], F32)
                bsrc = self.small[f"ada_b{l}"][0:1, nb * 512:nb * 512 + ncols]
                P.dma("sp", bst[0:1, 0:ncols], bsrc, writes=["mod_bst0"])
                P.dma("sp", bst[1:2, 0:ncols], bsrc, writes=["mod_bst1"])
                P.op("dve", lambda e: e.tensor_tensor(out=st[0:2, 0:ncols], in0=acc[0:2, 0:ncols],
                                                      in1=bst[0:2, 0:ncols], op=ALU.add),
                     reads=[akey, "mod_bst0", "mod_bst1"], writes=["mod_st"])
                P.dma("sp", MODl[:, nb * 512:nb * 512 + ncols], st[0:2, 0:ncols], reads=["mod_st"],
                      writes=[("MOD", l, nb)])
            self.linear(f"mod{l}", [(0, 128)], D, 6 * D, prod, self.Wm(f"ada{l}"), epi)
        P.pop()

    def load_modvec(self, name, l, row, seg, gain=None, plus1=False):
        c, P = self.c, self.P
        D = c["D"]
        t = P.sb(name, [128, 2048], F32)
        src = self.MOD[l][row:row + 1, seg * D:(seg + 1) * D]
        rk = [("MOD", l, nb) for nb in range(seg * D // 512, (seg + 1) * D // 512)]
        P.dma("sp", t[:, 0:D], src.partition_broadcast(128), reads=rk, writes=[name])
        if gain is not None:
            g = P.sb("S4", [128, 2048], F32)
            P.dma("sp", g[:, 0:D], self.small[gain].partition_broadcast(128), writes=["S4"])
            P.op("dve", lambda e: e.scalar_tensor_tensor(out=t[:, 0:D], in0=t[:, 0:D], scalar=1.0 if plus1 else 0.0, in1=g[:, 0:D],
                                                         op0=ALU.add, op1=ALU.mult),
                 reads=[name, "S4"], writes=[name])
        return t

    def norm_tile(self, src_ap, src_keys, A, Akey, Bv, Bkey, out_bf, out_key, xname="S5"):
        c, P = self.c, self.P
        D = c["D"]
        xt = P.sb(xname, [128, 2048], F32)
        junk = P.sb("nt_junk", [128, 2048], BF16)
        ss = P.sb("nt_ss", [128, 1], F32)
        rs = P.sb("nt_rs", [128, 1], F32)
        P.dma("sp", xt[:, 0:D], src_ap, reads=src_keys, writes=[xname])
        P.op("act", lambda e: e.activation(out=junk[:, 0:D], in_=xt[:, 0:D], func=AF.Square, accum_out=ss[:]),
             reads=[xname], writes=["nt_junk", "nt_ss"])
        P.op("dve", lambda e: e.tensor_scalar(out=rs[:], in0=ss[:], scalar1=1.0 / D, scalar2=1e-6,
                                              op0=ALU.mult, op1=ALU.add), reads=["nt_ss"], writes=["nt_rs"])
        P.op("act", lambda e: e.activation(out=rs[:], in_=rs[:], func=AF.Sqrt), reads=["nt_rs"], writes=["nt_rs"])
        P.op("dve", lambda e: e.reciprocal(out=rs[:], in_=rs[:]), reads=["nt_rs"], writes=["nt_rs"])
        P.op("dve", lambda e: e.scalar_tensor_tensor(out=xt[:, 0:D], in0=xt[:, 0:D], scalar=rs[:, 0:1], in1=A[:, 0:D],
                                                     op0=ALU.mult, op1=ALU.mult),
             reads=[xname, "nt_rs", Akey], writes=[xname])
        P.op("pool", lambda e: e.tensor_tensor(out=out_bf[:, 0:D], in0=xt[:, 0:D], in1=Bv[:, 0:D], op=ALU.add),
             reads=[xname, Bkey], writes=[out_key])

    def layer(self, l):
        c, P = self.c, self.P
        D, M = c["D"], c["M"]
        last = (l == c["depth"] - 1)
        even = (l % 2 == 0)
        NCOL = c["EVC"] if even else c["ODC"]
        PB = self.D_(f"PB{l}", [M, NCOL], BF16)
        self.PB = PB
        P.push()
        A1 = self.load_modvec("S0", l, 0, 1, gain=f"norm_mix{l}", plus1=True)
        B1 = self.load_modvec("S1", l, 0, 0)
        A1c = self.load_modvec("S2", l, 1, 1, gain=f"norm_mix{l}", plus1=True)
        B1c = self.load_modvec("S3", l, 1, 0)
        tiles = [(mt, 128) for mt in range(c["NTT"])]

        def prod(i, mt, rows, xb, xkey):
            isc = mt >= c["NT"]
            self.norm_tile(self.XC[mt * 128:(mt + 1) * 128, :], [("XC", mt)],
                           A1c if isc else A1, "S2" if isc else "S0",
                           B1c if isc else B1, "S3" if isc else "S1", xb, xkey)

        def epi(i, mt, rows, nb, ncols, acc, akey):
            oi = self.ocnt % 2
            self.ocnt += 1
            st = P.sb(f"p1_st{oi}", [128, 512], BF16)
            P.op("act", lambda e: e.copy(out=st[:, 0:ncols], in_=acc[:, 0:ncols]), reads=[akey],
                 writes=[("p1_st", oi)])
            P.dma("sp", PB[mt * 128:(mt + 1) * 128, nb * 512:nb * 512 + ncols], st[:, 0:ncols],
                  reads=[("p1_st", oi)], writes=[("PB", mt, nb)])
        self.ocnt = 0
        self.linear(f"win{l}", tiles, D, NCOL, prod, self.Wm(f"win{l}"), epi, G=8)
        P.pop()
        if "stop_p1" in c["debug"] or f"stop_p1_{l}" in c["debug"]:
            return
        self.MIX = self.D_(f"MIX{l}", [M, D], BF16)
        for fn in ((lambda: self.gmlp(l, last)), (lambda: self.decay_attn(l, "ret", last))) if even else \
                ((lambda: self.nattn(l)), (lambda: self.decay_attn(l, "gla", last))):
            if (not even) and (("nattn" in fn.__code__.co_names and "skip_na" in c["debug"]) or ("decay_attn" in fn.__code__.co_names and "skip_gla" in c["debug"])):
                continue
            P.push()
            fn()
            P.pop()
        if "stop_p2" in c["debug"] or f"stop_p2_{l}" in c["debug"]:
            return
        ntl = c["NT"] if last else c["NTT"]
        tiles = [(mt, 128) for mt in range(ntl)]
        P.push()
        G1 = self.load_modvec("S0", l, 0, 2)
        G1c = self.load_modvec("S1", l, 1, 2) if not last else None
        X1 = self.X1
        mixkeys = lambda mt: [("MIX", mt, k_, s_) for (k_, s_) in ((("gmlp", 0), ("ret", 0)) if even else (("na", 0), ("gla", 0)))]

        def prod3(i, mt, rows, xb, xkey):
            P.dma("sp", xb[:, 0:D], self.MIX[mt * 128:(mt + 1) * 128, :], reads=mixkeys(mt), writes=[xkey])

        def epi3(i, mt, rows, nb, ncols, acc, akey):
            oi = self.ocnt % 2
            self.ocnt += 1
            xt = P.sb(f"p3_x{oi}", [128, 512], F32)
            g = G1c if mt >= c["NT"] else G1
            gk = "S1" if mt >= c["NT"] else "S0"
            P.dma("sp", xt[:, 0:ncols], self.XC[mt * 128:(mt + 1) * 128, nb * 512:nb * 512 + ncols], reads=[("XC", mt)], writes=[("p3_x", oi)])
            st = P.sb(f"p3_st{oi}", [128, 512], F32)
            P.op("dve", lambda e: e.tensor_tensor(out=st[:, 0:ncols], in0=acc[:, 0:ncols], in1=g[:, nb * 512:nb * 512 + ncols], op=ALU.mult),
                 reads=[akey, gk], writes=[("p3_st", oi)])
            P.op("pool", lambda e: e.tensor_tensor(out=st[:, 0:ncols], in0=st[:, 0:ncols], in1=xt[:, 0:ncols], op=ALU.add),
                 reads=[("p3_st", oi), ("p3_x", oi)], writes=[("p3_st", oi)])
            P.dma("sp", X1[mt * 128:(mt + 1) * 128, nb * 512:nb * 512 + ncols], st[:, 0:ncols], reads=[("p3_st", oi)], writes=[("X1", mt, nb)])
        self.linear(f"wout{l}", tiles, D, D, prod3, self.Wm(f"wout{l}"), epi3, G=8)
        P.pop()
        if "stop_p3" in c["debug"] or f"stop_p3_{l}" in c["debug"]:
            return
        self.moe(l, last, tiles)

    def moe(self, l, last, tiles):
        c, P = self.c, self.P
        D, M, E, F, NT = c["D"], c["M"], c["E"], c["F"], c["NT"]
        self.consts_attn()
        capL, capC = c["capL"], (0 if last else c["capC"])
        SLOTS = capL + capC
        H2 = self.D_(f"H2_{l}", [M, D], BF16)
        AFF = self.D_(f"AFF{l}", [M, E], F32)
        P.push()
        affT = P.sb("moe_affT", [16, M], F32)
        idxT = P.sb("moe_idxT", [128, 34, 16], I32)
        wT = P.sb("moe_wT", [128, 34, 16], F32)
        P.push()
        A2 = self.load_modvec("S0", l, 0, 4, gain=f"norm_ffn{l}", plus1=True)
        B2 = self.load_modvec("S1", l, 0, 3)
        if not last:
            A2c = self.load_modvec("S2", l, 1, 4, gain=f"norm_ffn{l}", plus1=True)
            B2c = self.load_modvec("S3", l, 1, 3)
        x1keys = lambda mt: [("X1", mt, nb) for nb in range(D // 512)]

        def prod(i, mt, rows, xb, xkey):
            isc = mt >= NT
            self.norm_tile(self.X1[mt * 128:(mt + 1) * 128, :], x1keys(mt), A2c if isc else A2, "S2" if isc else "S0",
                           B2c if isc else B2, "S3" if isc else "S1", xb, xkey)
            P.dma("sp", H2[mt * 128:(mt + 1) * 128, :], xb[:, 0:D], reads=[xkey], writes=[("H2", mt)])

        def epi(i, mt, rows, nb, ncols, acc, akey):
            lg = P.sb("moe_lg", [128, 16], F32)
            mx = P.sb("moe_mx", [128, 1], F32)
            sm = P.sb("moe_sm", [128, 1], F32)
            P.op("dve", lambda e: e.tensor_reduce(out=mx[:], in_=acc[:, 0:E], axis=mybir.AxisListType.X, op=ALU.max), reads=[akey], writes=["moe_mx"])
            P.op("dve", lambda e: e.tensor_scalar(out=mx[:], in0=mx[:], scalar1=-1.0, scalar2=None, op0=ALU.mult), reads=["moe_mx"], writes=["moe_mx"])
            P.op("act", lambda e: e.activation(out=lg[:, 0:E], in_=acc[:, 0:E], func=AF.Exp, bias=mx[:, 0:1], accum_out=sm[:]),
                 reads=[akey, "moe_mx"], writes=["moe_lg", "moe_sm"])
            P.op("dve", lambda e: e.reciprocal(out=sm[:], in_=sm[:]), reads=["moe_sm"], writes=["moe_sm"])
            P.op("dve", lambda e: e.tensor_scalar(out=lg[:, 0:E], in0=lg[:, 0:E], scalar1=sm[:, 0:1], scalar2=None, op0=ALU.mult),
                 reads=["moe_lg", "moe_sm"], writes=["moe_lg"])
            P.dma("sp", AFF[mt * 128:(mt + 1) * 128, :], lg[:, 0:E], reads=["moe_lg"], writes=[("AFF", mt)])
            tpf = self.pp[2]
            P.op("pe", lambda e: e.transpose(out=tpf[0:E, 0:128], in_=lg[:, 0:E], identity=self.identf[:, :]), reads=["moe_lg", "identf"], writes=[("tp", 0)])
            P.op("act", lambda e: e.copy(out=affT[0:E, mt * 128:(mt + 1) * 128], in_=tpf[0:E, 0:128]), reads=[("tp", 0)], writes=[("affT", mt)])
        self.linear(f"router{l}", tiles, D, E, prod, self.small[f"router{l}"], epi)
        P.pop()
        P.push()
        ones16 = P.sb("mo_ones", [16, 2048], F32)
        P.op("pool", lambda e: e.memset(ones16[:], 1.0), writes=["mo_ones"])
        work = P.sb("moe_work", [16, M], F32)
        m8 = P.sb("moe_m8", [16, 8], F32)
        idxf = P.sb("moe_idxf", [16, M], F32)
        segs = [(0, c["T"], capL, 0)] + ([] if last else [(c["T"], c["L"], capC, capL)])
        ntl = len(tiles)
        for (o0, n, cap, slot0) in segs:
            rk = [("affT", mt) for mt in range(o0 // 128, (o0 + n) // 128)]
            P.op("dve", lambda e, o0=o0, n=n: e.tensor_copy(out=work[0:E, o0:o0 + n], in_=affT[0:E, o0:o0 + n]), reads=rk, writes=["moe_work"])
            for it in range(cap // 8):
                P.op("dve", lambda e, o0=o0, n=n: e.max(out=m8[0:E, :], in_=work[0:E, o0:o0 + n]), reads=["moe_work"], writes=["moe_m8"])
                if it < cap // 8 - 1:
                    P.op("dve", lambda e, o0=o0, n=n: e.match_replace(out=work[0:E, o0:o0 + n], in_to_replace=m8[0:E, :], in_values=work[0:E, o0:o0 + n],
                                                                      imm_value=-1.0), reads=["moe_work", "moe_m8"], writes=["moe_work"])
            sel = work
            P.op("dve", lambda e, o0=o0, n=n: e.tensor_scalar(out=sel[0:E, o0:o0 + n], in0=affT[0:E, o0:o0 + n], scalar1=m8[0:E, 7:8], scalar2=None,
                                                              op0=ALU.is_ge), reads=rk + ["moe_m8", "moe_work"], writes=["moe_work"])
            for q0 in range(0, n, 2048):
                qn = min(2048, n - q0)
                init = 0.0 if q0 == 0 else idxf[0:E, o0 + q0 - 1:o0 + q0]
                P.op("dve", lambda e, a=o0 + q0, qn=qn, init=init: e.tensor_tensor_scan(out=idxf[0:E, a:a + qn], data0=ones16[0:E, 0:qn], data1=sel[0:E, a:a + qn],
                                                                                        initial=init, op0=ALU.mult, op1=ALU.add), reads=["moe_work", "mo_ones", "moe_idxf"], writes=["moe_idxf"])
            P.op("dve", lambda e, o0=o0, n=n, cap=cap: e.scalar_tensor_tensor(out=sel[0:E, o0:o0 + n], in0=idxf[0:E, o0:o0 + n], scalar=float(cap) + 0.5,
                                                                              in1=sel[0:E, o0:o0 + n], op0=ALU.is_le, op1=ALU.mult),
                 reads=["moe_work", "moe_idxf"], writes=["moe_work"])
            P.op("dve", lambda e, o0=o0, n=n, slot0=slot0: e.tensor_scalar(out=idxf[0:E, o0:o0 + n], in0=idxf[0:E, o0:o0 + n], scalar1=float(slot0 - 1) - BIG,
                                                                           scalar2=None, op0=ALU.add), reads=["moe_idxf"], writes=["moe_idxf"])
            P.op("dve", lambda e, o0=o0, n=n: e.tensor_tensor(out=idxf[0:E, o0:o0 + n], in0=idxf[0:E, o0:o0 + n], in1=sel[0:E, o0:o0 + n], op=ALU.mult),
                 reads=["moe_idxf", "moe_work"], writes=["moe_idxf"])
            P.op("dve", lambda e, o0=o0, n=n: e.tensor_scalar(out=idxf[0:E, o0:o0 + n], in0=idxf[0:E, o0:o0 + n], scalar1=BIG, scalar2=None, op0=ALU.add),
                 reads=["moe_idxf"], writes=["moe_idxf"])
        tpf = self.pp[2]
        for (mt, rows) in tiles:
            P.op("pe", lambda e, mt=mt: e.transpose(out=tpf[:, 0:E], in_=idxf[0:E, mt * 128:(mt + 1) * 128], identity=self.identf[0:E, 0:E]),
                 reads=["moe_idxf", "identf"], writes=[("tp", 0)])
            P.op("pe", lambda e, mt=mt: e.transpose(out=tpf[:, 512:512 + E], in_=affT[0:E, mt * 128:(mt + 1) * 128], identity=self.identf[0:E, 0:E]),
                 reads=[("affT", mt), "identf"], writes=[("tp", 1)])
            P.op("dve", lambda e, mt=mt: e.tensor_copy(out=idxT[:, mt, :], in_=tpf[:, 0:E]), reads=[("tp", 0)], writes=[("idxT", mt)])
            P.op("dve", lambda e, mt=mt: e.tensor_scalar(out=wT[:, mt, :], in0=tpf[:, 0:E], scalar1=BIG / 2, scalar2=None, op0=ALU.is_lt),
                 reads=[("tp", 0)], writes=[("wT", mt)])
            P.op("dve", lambda e, mt=mt: e.tensor_tensor(out=wT[:, mt, :], in0=wT[:, mt, :], in1=tpf[:, 512:512 + E], op=ALU.mult),
                 reads=[("wT", mt), ("tp", 1)], writes=[("wT", mt)])
        P.pop()
        P.push()
        XSe = [self.D_(f"XS{l}_{ex}", [SLOTS, D], BF16) for ex in range(E)]
        YSe = [self.D_(f"YS{l}_{ex}", [SLOTS, D], F32) for ex in range(E)]
        for (mt, rows) in tiles:
            hb = P.sb(f"lin_xbf{mt % 2}", [128, 2048], BF16)
            hk = ("xbf", mt % 2)
            P.dma("sp", hb[:, 0:D], H2[mt * 128:(mt + 1) * 128, :], reads=[("H2", mt)], writes=[hk])
            for ex in range(E):
                P.op("pool", lambda e, mt=mt, ex=ex, hb=hb: e.indirect_dma_start(
                    out=XSe[ex][:, :], out_offset=bass.IndirectOffsetOnAxis(ap=idxT[:, mt, ex:ex + 1], axis=0),
                    in_=hb[:, 0:D], in_offset=None, bounds_check=self.bcreg(e, SLOTS - 1), oob_is_err=False),
                    reads=[hk, ("idxT", mt)], writes=[("XS", ex, mt)], dma=True)
        P.pop()
        P.push()
        stiles = [(j, min(128, SLOTS - j * 128)) for j in range((SLOTS + 127) // 128)]
        HS = self.D_(f"HS{l}", [SLOTS, F], BF16)
        GA = self.D_(f"GA{l}", [SLOTS, F], BF16)
        for ex in range(E):
            xsk = [("XS", ex, mt) for (mt, _) in tiles]

            def prodx(i, j, rows, xb, xkey, ex=ex):
                P.dma("sp", xb[0:rows, 0:D], XSe[ex][j * 128:j * 128 + rows, :], reads=xsk, writes=[xkey])

            def epig(i, j, rows, nb, ncols, acc, akey, ex=ex):
                oi = self.ocnt % 2
                self.ocnt += 1
                st = P.sb(f"p1_st{oi}", [128, 512], BF16)
                P.op("act", lambda e: e.activation(out=st[0:rows, 0:ncols], in_=acc[0:rows, 0:ncols], func=AF.Silu), reads=[akey], writes=[("p1_st", oi)])
                P.dma("sp", GA[j * 128:j * 128 + rows, nb * 512:nb * 512 + ncols], st[0:rows, 0:ncols], reads=[("p1_st", oi)], writes=[("GA", j, nb)])

            def epiu(i, j, rows, nb, ncols, acc, akey, ex=ex):
                oi = self.ocnt % 2
                self.ocnt += 1
                ga = P.sb(f"p1_st{oi}", [128, 512], BF16)
                st = P.sb(f"mo_hs{oi}", [128, 512], BF16)
                P.dma("sp", ga[0:rows, 0:ncols], GA[j * 128:j * 128 + rows, nb * 512:nb * 512 + ncols], reads=[("GA", j, nb)], writes=[("p1_st", oi)])
                P.op("dve", lambda e: e.tensor_tensor(out=st[0:rows, 0:ncols], in0=acc[0:rows, 0:ncols], in1=ga[0:rows, 0:ncols], op=ALU.mult),
                     reads=[akey, ("p1_st", oi)], writes=[("mo_hs", oi)])
                P.dma("sp", HS[j * 128:j * 128 + rows, nb * 512:nb * 512 + ncols], st[0:rows, 0:ncols], reads=[("mo_hs", oi)], writes=[("HS", j, nb)])

            def prodh(i, j, rows, xb, xkey):
                P.dma("sp", xb[0:rows, 0:F], HS[j * 128:j * 128 + rows, :], reads=[("HS", j, nb) for nb in range((F + 511) // 512)], writes=[xkey])

            def epid(i, j, rows, nb, ncols, acc, akey, ex=ex):
                oi = self.ocnt % 2
                self.ocnt += 1
                st = P.sb(f"p3_st{oi}", [128, 512], F32)
                P.op("act", lambda e: e.copy(out=st[0:rows, 0:ncols], in_=acc[0:rows, 0:ncols]), reads=[akey], writes=[("p3_st", oi)])
                P.dma("sp", YSe[ex][j * 128:j * 128 + rows, nb * 512:nb * 512 + ncols], st[0:rows, 0:ncols],
                      reads=[("p3_st", oi)], writes=[("YS", ex, j, nb)])
            self.linear(f"g{l}_{ex}", stiles, D, F, prodx, self.Wm(f"wg{l}_{ex}"), epig, G=len(stiles))
            self.linear(f"u{l}_{ex}", stiles, D, F, prodx, self.Wm(f"wu{l}_{ex}"), epiu, G=len(stiles))
            self.linear(f"d{l}_{ex}", stiles, F, D, prodh, self.Wm(f"wd{l}_{ex}"), epid, G=len(stiles))
        P.pop()
        P.push()
        G2 = self.load_modvec("S0", l, 0, 5)
        G2c = self.load_modvec("S1", l, 1, 5) if not last else None
        for (mt, rows) in tiles:
            isc = mt >= NT
            accs = P.sb("S2", [128, 2048], F32)
            x1t = P.sb("S3", [128, 2048], F32)
            P.dma("sp", x1t[:, 0:D], self.X1[mt * 128:(mt + 1) * 128, :], reads=x1keys(mt), writes=["S3"])
            P.op("pool", lambda e: e.memset(accs[:, 0:D], 0.0), writes=["S2"])
            for ex in range(E):
                bi = ex % 2
                buf = P.sb(f"S{4 + bi}", [128, 2048], F32)
                if mt == tiles[0][0] and ex < 2:
                    P.op("pool", lambda e, buf=buf: e.memset(buf[:, 0:D], 0.0), writes=[f"S{4 + bi}"])
                yk = [("YS", ex, j, nb) for (j, _) in stiles for nb in range(D // 512)]
                P.op("pool", lambda e, mt=mt, ex=ex, buf=buf: e.indirect_dma_start(
                    out=buf[:, 0:D], out_offset=None, in_=YSe[ex][:, :],
                    in_offset=bass.IndirectOffsetOnAxis(ap=idxT[:, mt, ex:ex + 1], axis=0), bounds_check=self.bcreg(e, SLOTS - 1), oob_is_err=False),
                    reads=yk + [("idxT", mt)], writes=[f"S{4 + bi}"], dma=True)
                P.op("dve", lambda e, mt=mt, ex=ex, buf=buf: e.scalar_tensor_tensor(out=accs[:, 0:D], in0=buf[:, 0:D], scalar=wT[:, mt, ex:ex + 1], in1=accs[:, 0:D],
                                                                                    op0=ALU.mult, op1=ALU.add), reads=[f"S{4 + bi}", ("wT", mt), "S2"], writes=["S2"])
            g = G2c if isc else G2
            P.op("pool", lambda e, g=g: e.tensor_tensor(out=accs[:, 0:D], in0=accs[:, 0:D], in1=g[:, 0:D], op=ALU.mult), reads=["S2", "S1" if isc else "S0"], writes=["S2"])
            P.op("dve", lambda e: e.tensor_tensor(out=accs[:, 0:D], in0=accs[:, 0:D], in1=x1t[:, 0:D], op=ALU.add), reads=["S2", "S3"], writes=["S2"])
            P.dma("sp", self.XC[mt * 128:(mt + 1) * 128, :], accs[:, 0:D], reads=["S2"], writes=[("XC", mt)])
        P.pop()
        P.pop()

    def consts_attn(self):
        P = self.P
        if hasattr(self, "maskF"):
            return
        ones = P.sb("c_ones", [128, 128], F32)
        self.maskF = P.sb("c_maskF", [128, 128], F32)
        self.maskB = P.sb("c_maskB", [128, 128], F32)
        P.op("pool", lambda e: e.memset(ones[:], 1.0), writes=["c_ones"])
        P.op("pool", lambda e: e.affine_select(out=self.maskF[:], in_=ones[:], pattern=[[1, 128]], compare_op=ALU.is_ge,
                                               fill=0.0, base=0, channel_multiplier=-1), reads=["c_ones"], writes=["c_maskF", "c_maskB"])
        P.op("pool", lambda e: e.affine_select(out=self.maskB[:], in_=ones[:], pattern=[[-1, 128]], compare_op=ALU.is_ge,
                                               fill=0.0, base=0, channel_multiplier=1), reads=["c_ones"], writes=["c_maskF", "c_maskB"])
        self.ones = ones
        pi = P.sb("c_pi", [128, 1], I32)
        pf = P.sb("c_pf", [128, 8], F32)
        P.op("pool", lambda e: e.iota(out=pi[:], pattern=[[0, 1]], base=0, channel_multiplier=1), writes=["c_pi"])
        P.op("dve", lambda e: e.tensor_copy(out=pf[:, 0:1], in_=pi[:]), reads=["c_pi"], writes=["c_pf"])
        for j, (m, a) in enumerate([(1.0, 1.0), (-1.0, -1.0), (1.0, -127.0), (-1.0, 128.0), (1.0, -128.0), (-1.0, 0.0)]):
            P.op("dve", lambda e, j=j, m=m, a=a: e.tensor_scalar(out=pf[:, j + 1:j + 2], in0=pf[:, 0:1], scalar1=m, scalar2=a,
                                                               op0=ALU.mult, op1=ALU.add), reads=["c_pf"], writes=["c_pf"])
        self.pf = pf

    def decay_attn(self, l, kind, last):
        c, P = self.c, self.P
        PB = self.PB
        NT, NTT = c["NT"], c["NTT"]
        C = 128
        if kind == "ret":
            H, dk, dv = c["RH"], 128, 128
            qo, go, ko, vo = 0, c["RD"], 2 * c["RD"] + 2 * c["GD"], 3 * c["RD"] + 2 * c["GD"]
            qscale = 128 ** -0.5
            mixo = 0
            gnorm_name = f"ret_norm{l}"
            NCOLS = c["EVC"]
        else:
            H, dk, dv = 8, c["GDK"], c["GDV"]
            qo = c["NAD"]
            go = c["NAD"] + c["GQK"]
            ko = c["ODQ"] + 2 * c["NAD"]
            vo = ko + c["GQK"]
            lro = vo + c["GV"]
            qscale = dk ** -0.5
            mixo = c["NAD"]
            gnorm_name = f"gla_norm{l}"
            NCOLS = c["ODC"]
        hp = 128 // dk
        npk = H // hp
        HK, HV = H * dk, H * dv
        lnq = math.log(qscale)
        OF = self.D_(f"OF{l}", [c["M"], HV], F32)
        lat_units = list(range(NT))
        ctx_units = list(range(NT, NTT))
        gn = P.sb("at_gn", [128, 1024], F32)
        P.dma("sp", gn[:, 0:HV], self.small[gnorm_name].partition_broadcast(128), writes=["at_gn"])
        S32 = P.sb("at_S32", [128, 1024], F32)
        Sbf = P.sb("at_Sbf", [128, 1024], BF16)
        qt = P.sb("at_q", [128, 1024], BF16)
        kt = P.sb("at_k", [128, 1024], BF16)
        vt = P.sb("at_v", [128, 1024], BF16)
        gt = P.sb("at_g", [128, 1024], BF16)
        qin = P.sb("at_qin", [128, 1024], BF16)
        kin = P.sb("at_kin", [128, 1024], BF16)
        kout = P.sb("at_kout", [128, 1024], BF16)
        qT = P.sb("at_qT", [128, 8, 128], BF16)
        kT = P.sb("at_kT", [128, 8, 128], BF16)
        attT = P.sb("at_attT", [128, 8, 128], BF16)
        osb = P.sb("S4", [128, 2048], F32)
        ofl = P.sb("S5", [128, 2048], F32)
        pf = self.pf
        if hp > 1:
            rowm = P.sb("at_rowm", [128, 4], F32)
            P.op("pool", lambda e: e.memset(rowm[:, :], 0.0), writes=["at_rowm"])
            for j in range(hp):
                P.op("pool", lambda e, j=j: e.memset(rowm[j * dk:(j + 1) * dk, j:j + 1], 1.0), reads=["at_rowm"], writes=["at_rowm"])
        if kind == "ret":
            gl = P.sb("at_gl", [128, 16], F32)
            P.dma("sp", gl[:, 0:2 * H], self.small[f"gamma{l}"].partition_broadcast(128), writes=["at_gl"])
            P.op("act", lambda e: e.activation(out=gl[:, 0:2 * H], in_=gl[:, 0:2 * H], func=AF.Exp, scale=-1.0),
                 reads=["at_gl"], writes=["at_gl"])
            P.op("act", lambda e: e.activation(out=gl[:, 0:2 * H], in_=gl[:, 0:2 * H], func=AF.Ln, bias=1.0),
                 reads=["at_gl"], writes=["at_gl"])
            tb = P.sb("at_tb", [128, 2, 4, 8], F32)
            for d in range(2):
                sp_d = gl[:, d * H:(d + 1) * H]
                cols = [(2, lnq), (1, 0.0), (3, 0.0)] if d == 0 else [(5, lnq), (4, 0.0), (6, 0.0)]
                for j, (pc, bias) in enumerate(cols):
                    P.op("act", lambda e, d=d, j=j, pc=pc, bias=bias, sp_d=sp_d: e.activation(
                        out=tb[:, d, j, 0:H], in_=sp_d, func=AF.Exp, scale=pf[:, pc:pc + 1], bias=bias),
                        reads=["at_gl", "c_pf"], writes=["at_tb"])
                P.op("act", lambda e, d=d, sp_d=sp_d: e.activation(out=tb[:, d, 3, 0:H], in_=sp_d, func=AF.Exp, scale=-128.0),
                     reads=["at_gl"], writes=["at_tb"])
            rcs = P.sb("at_rcs", [128, 128], F32)
            rsn = P.sb("at_rsn", [128, 128], F32)
        else:
            lrT = P.sb("at_lrT", [128, 128], BF16)
            wup = P.sb("at_wup", [128, 2, 512], F32)
            wuph = P.sb("at_wuph", [128, 2, 512], BF16)
            wupl = P.sb("at_wupl", [128, 2, 512], BF16)
            P.op("pool", lambda e: e.memset(lrT[:, :], 1.0), writes=["at_lrT"])
            P.op("pool", lambda e: e.memset(wup[:, :, :], 0.0), writes=["at_wup"])
            P.dma("sp", wup[0:16, :, 0:HK], self.small[f"gla_wup{l}"].rearrange("d r n -> r d n"), reads=["at_wup"], writes=["at_wup"])
            P.dma("sp", wup[16:17, :, 0:HK], self.small[f"gla_bup{l}"], reads=["at_wup"], writes=["at_wup"])
            P.op("dve", lambda e: e.tensor_copy(out=wuph[:, :, :], in_=wup[:, :, :]), reads=["at_wup"], writes=["at_wuph"])
            P.op("dve", lambda e: e.tensor_tensor(out=wupl[:, :, :], in0=wup[:, :, :], in1=wuph[:, :, :], op=ALU.subtract),
                 reads=["at_wup", "at_wuph"], writes=["at_wupl"])
            sph = P.sb("at_sph", [128, 512], BF16)
            spl = P.sb("at_spl", [128, 512], BF16)
            sp = P.sb("at_sp", [128, 512], F32)
            bsb = P.sb("at_bsb", [128, 512], F32)
            EQ = P.sb("at_EQ", [128, 512], F32)
            EKI = P.sb("at_EKI", [128, 512], F32)
            EKO = P.sb("at_EKO", [128, 512], F32)
            dec = P.sb("at_dec", [128, 4, 2], F32)
            LmF = P.sb("at_LmF", [128, 128], BF16)
            LmB = P.sb("at_LmB", [128, 128], BF16)
            Em = P.sb("at_Em", [128, 128], BF16)
            nsc = P.sb("at_nsc", [128, 2], BF16)
            P.op("dve", lambda e: e.tensor_scalar(out=LmF[:], in0=self.maskF[:, :], scalar1=-1.0 / 16, scalar2=None,
                                                  op0=ALU.mult), reads=["c_maskF"], writes=["at_LmF"])
            P.op("dve", lambda e: e.tensor_scalar(out=LmB[:], in0=self.maskB[:, :], scalar1=-1.0 / 16, scalar2=None,
                                                  op0=ALU.mult), reads=["c_maskB"], writes=["at_LmB"])
            P.op("pool", lambda e: e.memset(Em[:], -1.0 / 16), writes=["at_Em"])
            P.op("pool", lambda e: e.memset(nsc[:], -1.0 / 16), writes=["at_nsc"])

        if kind == "gla":
            SPD = self.D_(f"SPD{l}", [2 * c["M"], HK], F32)
            for mt in range(NTT):
                r0 = mt * 128
                pk = [("PB", mt, nb) for nb in range((NCOLS + 511) // 512)]
                P.dma("sp", gt[:, 0:32], PB[r0:r0 + C, lro:lro + 32], reads=pk, writes=["at_g"])
                for d in range(2):
                    tpb = self.tp(0)
                    P.op("pe", lambda e, d=d, tpb=tpb: e.transpose(out=tpb[0:16, 0:C], in_=gt[:, d * 16:(d + 1) * 16], identity=self.ident[:, :]),
                         reads=["at_g", "ident"], writes=[("tp", 0)])
                    P.op("act", lambda e, tpb=tpb: e.copy(out=lrT[0:16, :], in_=tpb[0:16, 0:C]), reads=[("tp", 0)], writes=["at_lrT"])
                    zps = self.accb(2)
                    P.op("pe", lambda e, d=d: e.matmul(out=zps[:, 0:HK], lhsT=lrT[:, :], rhs=wuph[:, d, 0:HK], start=True, stop=False),
                         reads=["at_lrT", "at_wuph"], writes=[("acc", 2)])
                    P.op("pe", lambda e, d=d: e.matmul(out=zps[:, 0:HK], lhsT=lrT[:, :], rhs=wupl[:, d, 0:HK], start=False, stop=True),
                         reads=["at_lrT", "at_wupl", ("acc", 2)], writes=[("acc", 2)])
                    P.op("act", lambda e: e.activation(out=sp[:, 0:HK], in_=zps[:, 0:HK], func=AF.Exp, scale=-1.0),
                         reads=[("acc", 2)], writes=["at_sp"])
                    P.op("act", lambda e: e.activation(out=sp[:, 0:HK], in_=sp[:, 0:HK], func=AF.Ln, bias=1.0),
                         reads=["at_sp"], writes=["at_sp"])
                    P.dma("sp", SPD[d * c["M"] + r0:d * c["M"] + r0 + C, :], sp[:, 0:HK], reads=["at_sp"], writes=[("SPD", d, mt)])
            P.ops.append(("*", None, (), (), "bar"))
            if c.get("gla_cut") == 1:
                return

        for d in range(2):
            order = (ctx_units + lat_units) if d == 0 else (ctx_units[::-1] + lat_units[::-1])
            mask = self.maskF if d == 0 else self.maskB
            mkey = "c_maskF" if d == 0 else "c_maskB"
            P.op("pool", lambda e: e.memset(S32[:], 0.0), writes=["at_S32"])
            P.op("pool", lambda e: e.memset(Sbf[:], 0.0), writes=["at_Sbf"])
            for mt in order:
                isc = mt >= NT
                r0 = mt * 128
                need_out = (not isc) or (not last)
                pk = [("PB", mt, nb) for nb in range((NCOLS + 511) // 512)]
                P.dma("sp", kt[:, 0:HK], PB[r0:r0 + C, ko:ko + HK], reads=pk, writes=["at_k"])
                P.dma("sp", vt[:, 0:HV], PB[r0:r0 + C, vo:vo + HV], reads=pk, writes=["at_v"])
                if need_out:
                    P.dma("sp", qt[:, 0:HK], PB[r0:r0 + C, qo:qo + HK], reads=pk, writes=["at_q"])
                qsrc, ksrc, qk_, kk_ = qt, kt, "at_q", "at_k"
                if kind == "ret":
                    if not isc:
                        P.dma("sp", rcs[:], self.small["rot_cs"][r0:r0 + 128, :], writes=["at_rcs"])
                        P.dma("sp", rsn[:], self.small["rot_sn"][r0:r0 + 128, :], writes=["at_rsn"])
                        for (src, skey, dstname) in ((qt, "at_q", "S0"), (kt, "at_k", "S1")):
                            dst = P.sb(dstname, [128, 2048], F32)
                            t1 = dst[:, 0:HK].rearrange("p (h x) -> p h x", h=H)
                            t2 = dst[:, 1024:1024 + HK].rearrange("p (h a b x) -> p h a b x", h=H, a=2, b=2)
                            sv = src[:, 0:HK].rearrange("p (h x) -> p h x", h=H)
                            sv5 = src[:, 0:HK].rearrange("p (h a b x) -> p h a b x", h=H, a=2, b=2)
                            csb = rcs[:].unsqueeze(1).broadcast_to([128, H, 128])
                            sn5 = rsn[:].rearrange("p (a b x) -> p a b x", a=2, b=2)
                            P.op("dve", lambda e, t1=t1, sv=sv, csb=csb: e.tensor_tensor(out=t1, in0=sv, in1=csb, op=ALU.mult),
                                 reads=[skey, "at_rcs"], writes=[dstname])
                            for b_ in range(2):
                                snb = sn5[:, :, b_, :].unsqueeze(1).broadcast_to([128, H, 2, 32])
                                P.op("pool", lambda e, t2=t2, sv5=sv5, snb=snb, b_=b_: e.tensor_tensor(
                                    out=t2[:, :, :, b_, :], in0=sv5[:, :, :, 1 - b_, :], in1=snb, op=ALU.mult),
                                    reads=[skey, "at_rsn"], writes=[dstname + "b"])
                            P.op("dve", lambda e, dst=dst: e.tensor_tensor(out=dst[:, 0:HK], in0=dst[:, 0:HK],
                                                                           in1=dst[:, 1024:1024 + HK], op=ALU.add),
                                 reads=[dstname, dstname + "b"], writes=[dstname])
                        qsrc, ksrc, qk_, kk_ = P.sb("S0", [128, 2048], F32), P.sb("S1", [128, 2048], F32), "S0", "S1"
                    bq = lambda j, d=d: tb[:, d, j, 0:H].unsqueeze(2).broadcast_to([128, H, dk])
                    tq, tki, tko = bq(0), bq(1), bq(2)
                    v3 = lambda t: t[:, 0:HK].rearrange("p (h x) -> p h x", h=H)
                    tkeys = ["at_tb"]
                    decap = tb[:, d, 3, 0:H]
                    dkeys = ["at_tb"]
                else:
                    P.dma("sp", sp[:, 0:HK], SPD[d * c["M"] + r0:d * c["M"] + r0 + C, :], reads=[("SPD", d, mt)], writes=["at_sp"])
                    if c.get("gla_cut") == 21:
                        continue
                    Lm = LmF if d == 0 else LmB
                    bps, eps_ = self.accb(2), self.accb(3)
                    P.op("dve", lambda e: e.tensor_copy(out=sph[:, 0:HK], in_=sp[:, 0:HK]), reads=["at_sp"], writes=["at_sph"])
                    P.op("dve", lambda e: e.tensor_tensor(out=spl[:, 0:HK], in0=sp[:, 0:HK], in1=sph[:, 0:HK], op=ALU.subtract),
                         reads=["at_sp", "at_sph"], writes=["at_spl"])
                    for (dst_, akey_, lhs_, lk_) in ((bps, ("acc", 2), Lm, ["at_LmF", "at_LmB"]), (eps_, ("acc", 3), Em, ["at_Em"])):
                        P.op("pe", lambda e, dst_=dst_, lhs_=lhs_: e.matmul(out=dst_[:, 0:HK], lhsT=lhs_[:, :], rhs=sph[:, 0:HK], start=True, stop=False),
                             reads=["at_sph"] + lk_, writes=[akey_])
                        P.op("pe", lambda e, dst_=dst_, lhs_=lhs_: e.matmul(out=dst_[:, 0:HK], lhsT=lhs_[:, :], rhs=spl[:, 0:HK], start=False, stop=True),
                             reads=["at_spl", akey_] + lk_, writes=[akey_])
                    if c.get("gla_cut") == 22:
                        continue
                    P.op("act", lambda e: e.activation(out=EQ[:, 0:HK], in_=bps[:, 0:HK], func=AF.Exp, bias=lnq),
                         reads=[("acc", 2)], writes=["at_EQ"])
                    P.op("act", lambda e: e.activation(out=EKI[:, 0:HK], in_=bps[:, 0:HK], func=AF.Exp, scale=-1.0),
                         reads=[("acc", 2)], writes=["at_EKI"])
                    if c.get("gla_cut") == 23:
                        continue
                    P.op("act", lambda e: e.activation(out=bsb[:, 0:HK], in_=eps_[:, 0:HK], func=AF.Exp), reads=[("acc", 3)], writes=["at_bsb"])
                    P.op("dve", lambda e: e.tensor_tensor(out=EKO[:, 0:HK], in0=bsb[:, 0:HK], in1=EKI[:, 0:HK], op=ALU.mult),
                         reads=["at_bsb", "at_EKI"], writes=["at_EKO"])
                    if c.get("gla_cut") == 2:
                        continue
                    dps = self.pp[2]
                    for p_ in range(npk):
                        P.op("pe", lambda e, p_=p_: e.matmul(out=dps[:, 512 + 2 * p_:512 + 2 * p_ + 2], lhsT=sph[:, p_ * 128:(p_ + 1) * 128], rhs=nsc[:, 0:2],
                                                             start=True, stop=False), reads=["at_sph", "at_nsc"], writes=[("tp", 1)])
                        P.op("pe", lambda e, p_=p_: e.matmul(out=dps[:, 512 + 2 * p_:512 + 2 * p_ + 2], lhsT=spl[:, p_ * 128:(p_ + 1) * 128], rhs=nsc[:, 0:2],
                                                             start=False, stop=True), reads=["at_spl", "at_nsc", ("tp", 1)], writes=[("tp", 1)])
                    P.op("act", lambda e: e.activation(out=dec[:, 0:npk, :], in_=dps[:, 512:512 + 2 * npk].rearrange("p (h x) -> p h x", h=npk), func=AF.Exp),
                         reads=[("tp", 1)], writes=["at_dec"])
                    if c.get("gla_cut") == 3:
                        continue
                    v3 = lambda t: t[:, 0:HK]
                    tq, tki, tko = EQ[:, 0:HK], EKI[:, 0:HK], EKO[:, 0:HK]
                    tkeys = ["at_EQ", "at_EKI", "at_EKO"]
                    decap = dec[:, 0:npk, 0]
                    dkeys = ["at_dec"]
                if need_out:
                    P.op("dve", lambda e, qsrc=qsrc, tq=tq, v3=v3: e.tensor_tensor(out=v3(qin), in0=v3(qsrc), in1=tq, op=ALU.mult),
                         reads=[qk_] + tkeys, writes=["at_qin"])
                    P.op("pool", lambda e, ksrc=ksrc, tki=tki, v3=v3: e.tensor_tensor(out=v3(kin), in0=v3(ksrc), in1=tki, op=ALU.mult),
                         reads=[kk_] + tkeys, writes=["at_kin"])
                P.op("dve", lambda e, ksrc=ksrc, tko=tko, v3=v3: e.tensor_tensor(out=v3(kout), in0=v3(ksrc), in1=tko, op=ALU.mult),
                     reads=[kk_] + tkeys, writes=["at_kout"])
                if c.get("gla_cut") == 4 and kind == "gla":
                    continue
                if need_out:
                    for (src, skey, dstT, dkey, tpi) in ((qin, "at_qin", qT, "at_qT", 0), (kin, "at_kin", kT, "at_kT", 1)):
                        tp = self.tp(tpi)
                        for p_ in range(npk):
                            P.op("pe", lambda e, tp=tp, src=src, p_=p_: e.transpose(out=tp[:, p_ * 128:(p_ + 1) * 128],
                                                                                   in_=src[:, p_ * 128:(p_ + 1) * 128], identity=self.ident[:, :]),
                                 reads=[skey, "ident"], writes=[("tp", tpi)])
                        tpv = tp[:, 0:npk * 128].rearrange("p (h x) -> p h x", h=npk)
                        if hp == 1:
                            P.op("act", lambda e, tpv=tpv, dstT=dstT: e.copy(out=dstT[:, 0:H, :], in_=tpv), reads=[("tp", tpi)], writes=[dkey])
                        else:
                            for j in range(hp):
                                P.op("act", lambda e, tpv=tpv, dstT=dstT, j=j: e.activation(out=dstT[:, j:H:hp, :], in_=tpv, func=AF.Copy, scale=rowm[:, j:j + 1]),
                                     reads=[("tp", tpi), "at_rowm"], writes=[dkey + str(j)])
                    if c.get("gla_cut") == 5 and kind == "gla":
                        continue
                    tkq = ["at_qT"] + [f"at_qT{j}" for j in range(hp)]
                    tkk = ["at_kT"] + [f"at_kT{j}" for j in range(hp)]
                    nbh = 4
                    for bk in range((H + nbh - 1) // nbh):
                        acc = self.accb(bk)
                        hs = list(range(bk * nbh, min(H, (bk + 1) * nbh)))
                        for h in hs:
                            P.op("pe", lambda e, acc=acc, h=h, bk=bk: e.matmul(out=acc[:, (h - bk * nbh) * C:(h - bk * nbh + 1) * C],
                                                                               lhsT=kT[:, h, :], rhs=qT[:, h, :], start=True, stop=True),
                                 reads=tkq + tkk, writes=[("acc", bk)])
                        P.op("dve", lambda e, acc=acc, hs=hs, mask=mask: e.tensor_tensor(
                            out=attT[:, hs[0]:hs[-1] + 1, :], in0=acc[:, 0:len(hs) * C].rearrange("p (h x) -> p h x", h=len(hs)),
                            in1=mask[:, :].unsqueeze(1).broadcast_to([C, len(hs), C]), op=ALU.mult),
                            reads=[("acc", bk), mkey], writes=["at_attT"])
                    if c.get("gla_cut") == 6 and kind == "gla":
                        continue
                    ops_ = self.pp[3]
                    for h in range(H):
                        P.op("pe", lambda e, h=h: e.matmul(out=ops_[:, h * dv:(h + 1) * dv], lhsT=attT[:, h, :],
                                                           rhs=vt[:, h * dv:(h + 1) * dv], start=True, stop=False),
                             reads=["at_attT", "at_v"], writes=["pp3"])
                        P.op("pe", lambda e, h=h: e.matmul(out=ops_[:, h * dv:(h + 1) * dv], lhsT=qT[:, h, :],
                                                           rhs=Sbf[:, (h // hp) * dv:(h // hp + 1) * dv], start=False, stop=True),
                             reads=tkq + ["at_Sbf", "pp3"], writes=["pp3"])
                if c.get("gla_cut") == 7 and kind == "gla":
                    continue
                kvp = self.pp[1]
                for h in range(H):
                    P.op("pe", lambda e, h=h: e.matmul(out=kvp[:, h * dv:(h + 1) * dv], lhsT=kout[:, (h // hp) * 128:(h // hp + 1) * 128],
                                                       rhs=vt[:, h * dv:(h + 1) * dv], start=True, stop=True),
                         reads=["at_kout", "at_v"], writes=[("acc", 2), ("acc", 3)])
                for j in range(hp):
                    rr = slice(j * dk, (j + 1) * dk)
                    s3 = S32[rr, 0:npk * dv].rearrange("p (h x) -> p h x", h=npk)
                    k3 = kvp[rr, 0:HV].rearrange("p (a b x) -> p a b x", a=npk, b=hp)[:, :, j, :]
                    P.op("dve", lambda e, s3=s3, decap=decap, rr=rr: e.tensor_tensor(out=s3, in0=s3, in1=decap[rr, :].unsqueeze(2).broadcast_to([dk, npk, dv]),
                                                                                    op=ALU.mult), reads=["at_S32"] + dkeys, writes=["at_S32"])
                    P.op("dve", lambda e, s3=s3, k3=k3: e.tensor_tensor(out=s3, in0=k3, in1=s3, op=ALU.add),
                         reads=["at_S32", ("acc", 2), ("acc", 3)], writes=["at_S32"])
                P.op("act", lambda e: e.copy(out=Sbf[:, 0:npk * dv], in_=S32[:, 0:npk * dv]), reads=["at_S32", "pp3"], writes=["at_Sbf"])
                if not need_out:
                    continue
                okey = ("OF", mt)
                if d == 0:
                    P.op("act", lambda e: e.copy(out=osb[:, 0:HV], in_=self.pp[3][:, 0:HV]), reads=["pp3"], writes=["S4"])
                    P.dma("sp", OF[r0:r0 + C, :], osb[:, 0:HV], reads=["S4"], writes=[okey])
                    continue
                P.dma("sp", ofl[:, 0:HV], OF[r0:r0 + C, :], reads=[okey], writes=["S5"])
                P.dma("sp", gt[:, 0:HV], PB[r0:r0 + C, go:go + HV], reads=pk, writes=["at_g"])
                P.op("dve", lambda e: e.tensor_tensor(out=osb[:, 0:HV], in0=self.pp[3][:, 0:HV], in1=ofl[:, 0:HV], op=ALU.add),
                     reads=["pp3", "S5"], writes=["S4"])
                if f"OFB{l}" in c["debug"]:
                    if not hasattr(self, "OFB"):
                        self.OFB = self.D_(f"OFB{l}", [c["M"], HV], F32)
                    P.dma("sp", self.OFB[r0:r0 + C, :], osb[:, 0:HV], reads=["S4"], writes=[("OFB", mt)])
                sq = ofl
                ss = P.sb("at_ss", [128, 8], F32)
                P.op("pool", lambda e: e.tensor_tensor(out=sq[:, 0:HV], in0=osb[:, 0:HV], in1=osb[:, 0:HV], op=ALU.mult),
                     reads=["S4"], writes=["S5"])
                P.op("dve", lambda e: e.tensor_reduce(out=ss[:, 0:H], in_=sq[:, 0:HV].rearrange("p (h x) -> p h x", h=H),
                                                      axis=mybir.AxisListType.X, op=ALU.add), reads=["S5"], writes=["at_ss"])
                P.op("dve", lambda e: e.tensor_scalar(out=ss[:, 0:H], in0=ss[:, 0:H], scalar1=1.0 / dv, scalar2=1e-6,
                                                      op0=ALU.mult, op1=ALU.add), reads=["at_ss"], writes=["at_ss"])
                P.op("act", lambda e: e.activation(out=ss[:, 0:H], in_=ss[:, 0:H], func=AF.Sqrt), reads=["at_ss"], writes=["at_ss"])
                P.op("dve", lambda e: e.reciprocal(out=ss[:, 0:H], in_=ss[:, 0:H]), reads=["at_ss"], writes=["at_ss"])
                o3 = osb[:, 0:HV].rearrange("p (h x) -> p h x", h=H)
                P.op("dve", lambda e, o3=o3: e.tensor_tensor(out=o3, in0=o3, in1=ss[:, 0:H].unsqueeze(2).broadcast_to([C, H, dv]), op=ALU.mult),
                     reads=["S4", "at_ss"], writes=["S4"])
                P.op("pool", lambda e: e.tensor_tensor(out=osb[:, 0:HV], in0=osb[:, 0:HV], in1=gn[:, 0:HV], op=ALU.mult),
                     reads=["S4", "at_gn"], writes=["S4"])
                P.op("act", lambda e: e.activation(out=sq[:, 0:HV], in_=gt[:, 0:HV], func=AF.Silu), reads=["at_g"], writes=["S5"])
                ob = P.sb("at_ob", [128, 1024], BF16)
                P.op("dve", lambda e, ob=ob: e.tensor_tensor(out=ob[:, 0:HV], in0=osb[:, 0:HV], in1=sq[:, 0:HV], op=ALU.mult),
                     reads=["S4", "S5"], writes=["at_ob"])
                P.dma("sp", self.MIX[r0:r0 + C, mixo:mixo + HV], ob[:, 0:HV], reads=["at_ob"], writes=[("MIX", mt, kind, 0)])

    def nattn(self, l):
        c, P = self.c, self.P
        PB = self.PB
        NT, NTT, T, L = c["NT"], c["NTT"], c["T"], c["L"]
        NAH, NAD = c["NAH"], c["NAD"]
        nkt = self.nkt()
        Wk = nkt * 128
        qo, ko = 0, c["ODQ"]
        vo = ko + NAD
        pkeys = lambda mt: [("PB", mt, nb) for nb in range((c["ODC"] + 511) // 512)]
        QT = self.D_(f"QT{l}", [NAH, 128, T], BF16)
        KT = self.D_(f"KT{l}", [NAH, 128, c["M"]], BF16)
        cls_of, _ = na_geometry(c)
        for mt in range(NTT):
            for (which, off, dst, scale) in (("q", qo, QT, 128 ** -0.5), ("k", ko, KT, 1.0)):
                if which == "q" and mt >= NT:
                    continue
                src = P.sb(f"na_src{which}", [128, 1024], BF16)
                stg = P.sb(f"na_stg{which}", [128, 8, 128], BF16)
                tpi = 0 if which == "q" else 1
                tp = self.tp(tpi)
                P.dma("sp", src[:, 0:NAD], PB[mt * 128:(mt + 1) * 128, off:off + NAD], reads=pkeys(mt), writes=[f"na_src{which}"])
                for h in range(NAH):
                    P.op("pe", lambda e, tp=tp, src=src, h=h: e.transpose(out=tp[:, h * 128:(h + 1) * 128], in_=src[:, h * 128:(h + 1) * 128], identity=self.ident[:, :]),
                         reads=[f"na_src{which}", "ident"], writes=[("tp", tpi)])
                P.op("act", lambda e, tp=tp, stg=stg, scale=scale: e.activation(out=stg[:, 0:NAH, :], in_=tp[:, 0:NAH * 128].rearrange("p (h x) -> p h x", h=NAH),
                                                                              func=AF.Copy, scale=scale), reads=[("tp", tpi)], writes=[f"na_stg{which}"])
                P.dma("sp", dst[:, :, mt * 128:(mt + 1) * 128].rearrange("h d t -> d h t"), stg[:, 0:NAH, :], reads=[f"na_stg{which}"], writes=[(which + "T", mt)])
        vctx = P.sb("na_vctx", [128, c["NCT"], 1024], BF16)
        kctx = P.sb("na_kctx", [128, 8, L], BF16)
        for j in range(c["NCT"]):
            P.dma("sp", vctx[:, j, 0:NAD], PB[(NT + j) * 128:(NT + j + 1) * 128, vo:vo + NAD], reads=pkeys(NT + j), writes=[("na_vctx", j)])
        P.dma("sp", kctx[:, 0:NAH, :], KT[:, :, T:T + L].rearrange("h d t -> d h t"), reads=[("kT", NT + j) for j in range(c["NCT"])], writes=["na_kctx"])
        vwin = P.sb("na_vwin", [128, 5, 1024], BF16)
        kTh = P.sb("na_kTh", [128, 640], BF16)
        qTh = P.sb("na_qTh", [128, 128], BF16)
        bias = P.sb("na_bias", [128, 640], F32)
        sc = P.sb("na_sc", [128, 896], F32)
        pb = P.sb("na_pb", [128, 896], BF16)
        pT = P.sb("na_pT", [128, 7, 128], BF16)
        osb = P.sb("na_osb", [128, 1024], BF16)
        mx = P.sb("na_mx", [128, 1], F32)
        sm = P.sb("na_sm", [128, 1], F32)
        nj = nkt + c["NCT"]
        for mt in range(NT):
            kt0 = min(max(mt - 2, 0), NT - nkt)
            for j in range(nkt):
                P.dma("sp", vwin[:, j, 0:NAD], PB[(kt0 + j) * 128:(kt0 + j + 1) * 128, vo:vo + NAD], reads=pkeys(kt0 + j), writes=[("na_vwin", j)])
            for h in range(NAH):
                P.dma("sp", qTh[:, :], QT[h, :, mt * 128:(mt + 1) * 128], reads=[("qT", mt)], writes=["na_qTh"])
                P.dma("sp", kTh[:, 0:Wk], KT[h, :, kt0 * 128:kt0 * 128 + Wk], reads=[("kT", kt0 + j) for j in range(nkt)], writes=["na_kTh"])
                P.dma("sp", bias[:, 0:Wk], self.small[f"na_bias{l}"][cls_of[mt], h, :, :], writes=["na_bias"])
                sps = self.pp[0]
                for c0 in range(0, Wk, 512):
                    cn = min(512, Wk - c0)
                    P.op("pe", lambda e, c0=c0, cn=cn: e.matmul(out=sps[:, c0:c0 + cn], lhsT=qTh[:, :], rhs=kTh[:, c0:c0 + cn], start=True, stop=True),
                         reads=["na_qTh", "na_kTh"], writes=[("acc", c0 // 512)])
                P.op("pe", lambda e, h=h: e.matmul(out=sps[:, Wk:Wk + L], lhsT=qTh[:, :], rhs=kctx[:, h, :], start=True, stop=True),
                     reads=["na_qTh", "na_kctx", ("acc", 1)], writes=[("acc", 1)])
                P.op("dve", lambda e: e.tensor_tensor(out=sc[:, 0:Wk], in0=sps[:, 0:Wk], in1=bias[:, 0:Wk], op=ALU.add),
                     reads=[("acc", 0), ("acc", 1), "na_bias"], writes=["na_sc"])
                P.op("act", lambda e: e.copy(out=sc[:, Wk:Wk + L], in_=sps[:, Wk:Wk + L]), reads=[("acc", 1)], writes=["na_scc"])
                P.op("dve", lambda e: e.tensor_reduce(out=mx[:], in_=sc[:, 0:Wk + L], axis=mybir.AxisListType.X, op=ALU.max), reads=["na_sc", "na_scc"], writes=["na_mx"])
                P.op("dve", lambda e: e.tensor_scalar(out=mx[:], in0=mx[:], scalar1=-1.0, scalar2=None, op0=ALU.mult), reads=["na_mx"], writes=["na_mx"])
                P.op("act", lambda e: e.activation(out=pb[:, 0:Wk + L], in_=sc[:, 0:Wk + L], func=AF.Exp, bias=mx[:, 0:1], accum_out=sm[:]),
                     reads=["na_sc", "na_scc", "na_mx"], writes=["na_pb", "na_sm"])
                P.op("dve", lambda e: e.reciprocal(out=sm[:], in_=sm[:]), reads=["na_sm"], writes=["na_sm"])
                tp = self.tp(0)
                for j in range(nj):
                    P.op("pe", lambda e, j=j: e.transpose(out=tp[:, j * 128:(j + 1) * 128], in_=pb[:, j * 128:(j + 1) * 128], identity=self.ident[:, :]),
                         reads=["na_pb", "ident"], writes=[("tp", 0)])
                P.op("act", lambda e: e.copy(out=pT[:, 0:nj, :], in_=tp[:, 0:nj * 128].rearrange("p (j x) -> p j x", j=nj)), reads=[("tp", 0)], writes=["na_pT"])
                ops_ = self.accb(2)
                for j in range(nj):
                    rhs = vwin[:, j, h * 128:(h + 1) * 128] if j < nkt else vctx[:, j - nkt, h * 128:(h + 1) * 128]
                    rk = ("na_vwin", j) if j < nkt else ("na_vctx", j - nkt)
                    P.op("pe", lambda e, j=j, rhs=rhs: e.matmul(out=ops_[:, 0:128], lhsT=pT[:, j, :], rhs=rhs, start=(j == 0), stop=(j == nj - 1)),
                         reads=["na_pT", rk], writes=[("acc", 2)])
                P.op("dve", lambda e, h=h: e.tensor_scalar(out=osb[:, h * 128:(h + 1) * 128], in0=ops_[:, 0:128], scalar1=sm[:, 0:1], scalar2=None, op0=ALU.mult),
                     reads=[("acc", 2), "na_sm"], writes=[("na_osb", h)])
            P.dma("sp", self.MIX[mt * 128:(mt + 1) * 128, 0:NAD], osb[:, 0:NAD], reads=[("na_osb", h) for h in range(NAH)], writes=[("MIX", mt, "na", 0)])

    def gmlp(self, l, last):
        c, P = self.c, self.P
        PB = self.PB
        GD, GW, RD = c["GD"], c["GW"], c["RD"]
        uo, vo = 2 * RD, 2 * RD + GD
        wsT = P.sb("gm_wsT", [128, 8, 128], BF16)
        wsf = P.sb("S0", [128, 2048], F32)
        P.dma("sp", wsf[:, 0:1024].rearrange("p (g x) -> p g x", g=8), self.small[f"gmlp_wsT{l}"].rearrange("g s p -> s g p"), writes=["S0"])
        P.op("dve", lambda e: e.tensor_copy(out=wsT[:], in_=wsf[:, 0:1024].rearrange("p (g x) -> p g x", g=8)), reads=["S0"], writes=["gm_wsT"])
        bsT = P.sb("gm_bsT", [128, 8], F32)
        P.dma("sp", bsT[:], self.small[f"gmlp_bsT{l}"], writes=["gm_bsT"])
        gng = P.sb("S1", [128, 2048], F32)
        P.dma("sp", gng[:, 0:GD], self.small[f"gmlp_norm{l}"].partition_broadcast(128), writes=["S1"])
        ntl = c["NT"] if last else c["NTT"]
        K2 = 2 * math.sqrt(2.0 / math.pi)
        for mt in range(ntl):
            pk = [("PB", mt, nb) for nb in range(c["EVC"] // 512)]
            r0 = mt * 128
            raw = P.sb("at_q", [128, 1024], BF16)
            raw2 = P.sb("at_k", [128, 1024], BF16)
            P.dma("sp", raw[:, 0:GD], PB[r0:r0 + 128, uo:uo + GD], reads=pk, writes=["at_q"])
            P.dma("sp", raw2[:, 0:GD], PB[r0:r0 + 128, vo:vo + GD], reads=pk, writes=["at_k"])
            gl = {}
            for (src, skey, dname) in ((raw, "at_q", "S2"), (raw2, "at_k", "S3")):
                dst = P.sb(dname, [128, 2048], F32)
                a, b = dst[:, 0:GD], dst[:, 1024:1024 + GD]
                eng = "dve" if dname == "S2" else "pool"
                P.op(eng, lambda e, a=a, src=src: e.tensor_tensor(out=a, in0=src[:, 0:GD], in1=src[:, 0:GD], op=ALU.mult), reads=[skey], writes=[dname])
                P.op(eng, lambda e, a=a: e.tensor_scalar(out=a, in0=a, scalar1=0.044715, scalar2=1.0, op0=ALU.mult, op1=ALU.add), reads=[dname], writes=[dname])
                P.op(eng, lambda e, a=a, src=src: e.tensor_tensor(out=a, in0=a, in1=src[:, 0:GD], op=ALU.mult), reads=[dname, skey], writes=[dname])
                P.op("act", lambda e, a=a, b=b: e.activation(out=b, in_=a, func=AF.Sigmoid, scale=K2), reads=[dname], writes=[dname + "b"])
                P.op(eng, lambda e, a=a, b=b, src=src: e.tensor_tensor(out=a, in0=b, in1=src[:, 0:GD], op=ALU.mult), reads=[dname + "b", skey], writes=[dname])
                gl[dname] = a
            ug, vg = gl["S2"], gl["S3"]
            ss = P.sb("nt_ss", [128, 1], F32)
            rs = P.sb("nt_rs", [128, 1], F32)
            junk = P.sb("nt_junk", [128, 2048], BF16)
            P.op("act", lambda e: e.activation(out=junk[:, 0:GD], in_=vg, func=AF.Square, accum_out=ss[:]), reads=["S3"], writes=["nt_junk", "nt_ss"])
            P.op("dve", lambda e: e.tensor_scalar(out=rs[:], in0=ss[:], scalar1=1.0 / GD, scalar2=1e-6, op0=ALU.mult, op1=ALU.add), reads=["nt_ss"], writes=["nt_rs"])
            P.op("act", lambda e: e.activation(out=rs[:], in_=rs[:], func=AF.Sqrt), reads=["nt_rs"], writes=["nt_rs"])
            P.op("dve", lambda e: e.reciprocal(out=rs[:], in_=rs[:]), reads=["nt_rs"], writes=["nt_rs"])
            vn = P.sb("at_v", [128, 1024], BF16)
            P.op("dve", lambda e: e.scalar_tensor_tensor(out=vn[:, 0:GD], in0=vg, scalar=rs[:, 0:1], in1=gng[:, 0:GD], op0=ALU.mult, op1=ALU.mult),
                 reads=["S3", "nt_rs", "S1"], writes=["at_v"])
            ob = P.sb("at_ob", [128, 1024], BF16)
            for g in range(8):
                bank = g * GW // 512
                acc = self.accb(bank)
                col = g * GW - bank * 512
                P.op("pe", lambda e, acc=acc, g=g, col=col: e.matmul(out=acc[:, col:col + GW], lhsT=wsT[:, g, :], rhs=vn[:, g * GW:(g + 1) * GW], start=True, stop=True),
                     reads=["gm_wsT", "at_v"], writes=[("acc", bank)])
                P.op("dve", lambda e, acc=acc, g=g, col=col: e.scalar_tensor_tensor(out=ob[:, g * GW:(g + 1) * GW], in0=acc[:, col:col + GW], scalar=bsT[:, g:g + 1],
                                                                                    in1=ug[:, g * GW:(g + 1) * GW], op0=ALU.add, op1=ALU.mult),
                     reads=[("acc", bank), "gm_bsT", "S2"], writes=["at_ob"])
            P.dma("sp", self.MIX[r0:r0 + 128, RD:RD + GD], ob[:, 0:GD], reads=["at_ob"], writes=[("MIX", mt, "gmlp", 0)])

    def phase_final(self):
        c, P = self.c, self.P
        D = c["D"]
        P.push()
        g = P.sb("S0", [128, 2048], F32)
        P.dma("sp", g[:, 0:D], self.small["norm_final"].partition_broadcast(128), writes=["S0"])
        for mt in range(c["NT"]):
            i = mt % 2
            ob = P.sb(f"S{1 + i}", [128, 2048], F32)
            xt = P.sb("S5", [128, 2048], F32)
            junk = P.sb("nt_junk", [128, 2048], BF16)
            ss = P.sb("nt_ss", [128, 1], F32)
            rs = P.sb("nt_rs", [128, 1], F32)
            P.dma("sp", xt[:, 0:D], self.XC[mt * 128:(mt + 1) * 128, :], reads=[("XC", mt)], writes=["S5"])
            P.op("act", lambda e: e.activation(out=junk[:, 0:D], in_=xt[:, 0:D], func=AF.Square, accum_out=ss[:]),
                 reads=["S5"], writes=["nt_junk", "nt_ss"])
            P.op("dve", lambda e: e.tensor_scalar(out=rs[:], in0=ss[:], scalar1=1.0 / D, scalar2=1e-6,
                                                  op0=ALU.mult, op1=ALU.add), reads=["nt_ss"], writes=["nt_rs"])
            P.op("act", lambda e: e.activation(out=rs[:], in_=rs[:], func=AF.Sqrt), reads=["nt_rs"], writes=["nt_rs"])
            P.op("dve", lambda e: e.reciprocal(out=rs[:], in_=rs[:]), reads=["nt_rs"], writes=["nt_rs"])
            P.op("dve", lambda e, ob=ob: e.scalar_tensor_tensor(out=ob[:, 0:D], in0=xt[:, 0:D], scalar=rs[:, 0:1], in1=g[:, 0:D],
                                                                op0=ALU.mult, op1=ALU.mult),
                 reads=["S5", "nt_rs", "S0"], writes=[f"S{1 + i}"])
            P.dma("sp", self.out[mt * 128:(mt + 1) * 128, :], ob[:, 0:D], reads=[f"S{1 + i}"], writes=[("out", mt)])
        P.pop()


def na_geometry(c):
    NT, rows = c["NT"], c["rows"]
    nkt = min(5, NT)
    Wk = nkt * 128
    wr = min(8, rows)
    uniq, cls_of, maps = {}, [], []
    for mt in range(NT):
        kt0 = min(max(mt - 2, 0), NT - nkt)
        m = np.full((128, Wk), -1, np.int64)
        for p in range(128):
            t = mt * 128 + p
            r, col = t // 64, t % 64
            rstart = min(max(r - wr // 2, 0), rows - wr)
            cstart = min(max(col - 8, 0), 64 - 16)
            for r2 in range(rstart, rstart + wr):
                kk0 = r2 * 64 - kt0 * 128
                dr = r2 - r + 7
                for c2 in range(cstart, cstart + 16):
                    kk = kk0 + c2
                    assert 0 <= kk < Wk
                    m[p, kk] = dr * 31 + (c2 - col + 15)
        key = m.tobytes()
        if key not in uniq:
            uniq[key] = len(maps)
            maps.append(m)
        cls_of.append(uniq[key])
    return cls_of, maps


def host_inputs(c, inputs, ncores):
    f32 = lambda a: np.ascontiguousarray(a, dtype=np.float32)
    E, D, F = c["E"], c["D"], c["F"]
    shared = {"norm_final": f32(inputs["norm_final"])[None, :]}
    for l in range(c["depth"]):
        li = l // 2
        shared[f"ada{l}"] = f32(inputs["ada_w"][l])
        shared[f"win{l}"] = f32(inputs["ev_w_in"][li] if l % 2 == 0 else inputs["od_w_in"][li])
        shared[f"wout{l}"] = f32(inputs["ev_w_out"][li] if l % 2 == 0 else inputs["od_w_out"][li])
        shared[f"wg{l}"] = f32(inputs["moe_w_gate"][l]).reshape(E * D, F)
        shared[f"wu{l}"] = f32(inputs["moe_w_up"][l]).reshape(E * D, F)
        shared[f"wd{l}"] = f32(inputs["moe_w_down"][l]).reshape(E * F, D)
        shared[f"ada_b{l}"] = f32(inputs["ada_b"][l])[None, :]
        shared[f"norm_mix{l}"] = f32(inputs["norm_mix"][l])[None, :]
        shared[f"norm_ffn{l}"] = f32(inputs["norm_ffn"][l])[None, :]
        shared[f"router{l}"] = f32(inputs["router_w"][l])
        if l % 2 == 0:
            shared[f"gamma{l}"] = f32(inputs["ret_gamma_logit"][li]).reshape(1, -1)
            shared[f"ret_norm{l}"] = f32(inputs["ret_norm"][li])[None, :]
            shared[f"gmlp_norm{l}"] = f32(inputs["gmlp_norm"][li])[None, :]
            shared[f"gmlp_wsT{l}"] = f32(np.transpose(inputs["gmlp_ws"][li], (0, 2, 1)))
            shared[f"gmlp_bsT{l}"] = f32(np.transpose(inputs["gmlp_bs"][li], (1, 0)))
        else:
            shared[f"gla_wup{l}"] = f32(inputs["gla_w_up"][li])
            shared[f"gla_bup{l}"] = f32(inputs["gla_b_up"][li])[None]
            shared[f"gla_norm{l}"] = f32(inputs["gla_norm"][li])[None, :]
            cls_of, maps = na_geometry(c)
            rpb = f32(inputs["na_rpb"][li]).reshape(c["NAH"], -1)
            tabs = np.empty((6, c["NAH"], 128, maps[0].shape[1]), np.float32)
            tabs[:] = NEG
            for ci, m in enumerate(maps):
                valid = m >= 0
                for h in range(c["NAH"]):
                    tabs[ci, h][valid] = rpb[h][m[valid]]
            shared[f"na_bias{l}"] = tabs
    T = c["T"]
    t = np.arange(T)
    freq = (10000.0 ** (-np.arange(32, dtype=np.float32) / 32)).astype(np.float32)
    angr = ((t // 64).astype(np.float32)[:, None] * freq).astype(np.float32)
    angc = ((t % 64).astype(np.float32)[:, None] * freq).astype(np.float32)
    shared["rot_cs"] = np.concatenate([np.cos(angr), np.cos(angr), np.cos(angc), np.cos(angc)], 1).astype(np.float32)
    shared["rot_sn"] = np.concatenate([-np.sin(angr), np.sin(angr), -np.sin(angc), np.sin(angc)], 1).astype(np.float32)
    maps = []
    for core in range(ncores):
        s = core % 4
        m = dict(shared)
        m.update({"x": f32(inputs["x"][s]), "ctx": f32(inputs["ctx"][s]),
                  "cc": np.stack([inputs["c"][s], inputs["c_ctx"]]).astype(np.float32)})
        maps.append(m)
    return maps


def run(c, inputs):
    kb = K(c)
    nc = kb.build()
    maps = host_inputs(c, {k: np.asarray(v) for k, v in inputs.items()}, NCORES)
    res = run_bass_kernel_spmd(nc, maps, core_ids=list(range(NCORES)))
    return kb, res


def kernel(**inputs):
    c = make_cfg()
    kb, res = run(c, inputs)
    return np.stack([res.results[s]["out"] for s in range(4)]).astype(np.float32)
```

```python
import math
import numpy as np
from contextlib import ExitStack
import concourse.bass as bass
import concourse.mybir as mybir
from concourse.bass_utils import run_bass_kernel_spmd

F32 = mybir.dt.float32
BF16 = mybir.dt.bfloat16
I32 = mybir.dt.int32
U32 = mybir.dt.uint32
ALU = mybir.AluOpType
AF = mybir.ActivationFunctionType
ENGS = ("pe", "act", "dve", "pool", "sp")
NCORES = 4
BIG = 60000.0
NEG = -30000.0


class Prog:
    def __init__(self):
        self.nc = bass.Bass("TRN2", target_bir_lowering=False)
        self.es = ExitStack()
        self.ops = []
        self.n_dma_sems = 12
        self.bufs = {}
        self.arena = None

    def dram(self, name, shape, dt, kind="Internal"):
        return self.nc.dram_tensor(name, list(shape), dt, kind=kind)

    ARENA_BYTES = 176 * 1024

    def sb(self, name, shape, dt):
        if name in self.bufs:
            return self.bufs[name]
        if self.arena is None:
            self.arena = self.es.enter_context(self.nc.sbuf_tensor("arena", [128, self.ARENA_BYTES // 4], F32))
            self.bump = 0
            self.scopes = []
        esz = mybir.dt.size(dt)
        n = 1
        for d in shape[1:]:
            n *= d
        nbytes = (n * esz + 31) // 32 * 32
        assert self.bump + nbytes <= self.ARENA_BYTES, (name, self.bump, nbytes)
        v = self.arena[0:shape[0], self.bump // 4:(self.bump + nbytes) // 4]
        if dt != F32:
            v = v.bitcast(dt)
        v = v[:, 0:n]
        if len(shape) == 3:
            v = v.rearrange("p (a b) -> p a b", a=shape[1])
        elif len(shape) == 4:
            v = v.rearrange("p (a b c) -> p a b c", a=shape[1], b=shape[2])
        self.bump += nbytes
        self.peak = max(getattr(self, "peak", 0), self.bump)
        self.bufs[name] = v
        if self.scopes:
            self.scopes[-1][1].append(name)
        return v

    def push(self):
        if self.arena is None:
            self.sb("_dummy", [128, 8], F32)
        self.scopes.append((self.bump, []))

    def pop(self):
        bump, names = self.scopes.pop()
        for n in names:
            del self.bufs[n]
        self.bump = bump
        self.ops.append(("*", None, (), (), "bar"))

    def ps(self, name, shape, dt=F32):
        if name not in self.bufs:
            self.bufs[name] = self.es.enter_context(self.nc.psum_tensor(name, list(shape), dt))
        return self.bufs[name]

    def op(self, eng, fn, reads=(), writes=(), dma=False):
        self.ops.append((eng, fn, tuple(reads), tuple(writes), dma))

    def dma(self, q, out, in_, reads=(), writes=(), **kw):
        self.op(q, lambda e: e.dma_start(out=out, in_=in_, **kw), reads, writes, dma=True)

    def emit(self):
        nc, es = self.nc, self.es
        sem_c = {e: es.enter_context(nc.semaphore("s_" + e)) for e in ENGS}
        sem_d = {e: [es.enter_context(nc.semaphore(f"d_{e}{i}")) for i in range(self.n_dma_sems)]
                 for e in ("sp", "act", "pool")}
        sem_cc = es.enter_context(nc.semaphore("s_cc"))
        cnt_cc = [0]
        cnt_c = {e: 0 for e in ENGS}
        cnt_d = {e: [0] * self.n_dma_sems for e in sem_d}
        rr = {e: 0 for e in sem_d}
        last_w, readers = {}, {}
        known = {e: {} for e in ENGS}
        streams = {e: [] for e in ENGS}
        for (eng, fn, reads, writes, is_dma) in self.ops:
            if is_dma == "bar":
                alltok = [(sem_c[e], cnt_c[e]) for e in ENGS if cnt_c[e]]
                alltok += [(sem_d[e][k], cnt_d[e][k] * 16) for e in sem_d for k in range(self.n_dma_sems) if cnt_d[e][k]]
                if cnt_cc[0]:
                    alltok.append((sem_cc, cnt_cc[0]))
                for e in ENGS:
                    need = [(s_, v_) for (s_, v_) in alltok if known[e].get(id(s_), 0) < v_]
                    for (s_, v_) in need:
                        known[e][id(s_)] = v_
                    if need:
                        streams[e].append((need, None, None, 0))
                last_w, readers = {}, {}
                continue
            toks = []
            for r in reads:
                if r in last_w:
                    toks.append(last_w[r])
            for w in writes:
                if w in last_w:
                    toks.append(last_w[w])
                toks.extend(readers.get(w, ()))
            if is_dma == "cc":
                sem = sem_cc
                cnt_cc[0] += 1
                tok = (sem, cnt_cc[0])
                inc = 1
            elif is_dma:
                k = rr[eng]
                rr[eng] = (k + 1) % self.n_dma_sems
                sem = sem_d[eng][k]
                if cnt_d[eng][k] > 0:
                    toks.append((sem, cnt_d[eng][k] * 16))
                cnt_d[eng][k] += 1
                tok = (sem, cnt_d[eng][k] * 16)
                inc = 16
            else:
                sem = sem_c[eng]
                cnt_c[eng] += 1
                tok = (sem, cnt_c[eng])
                inc = 1
            need = {}
            for (s, v) in toks:
                if known[eng].get(id(s), 0) >= v:
                    continue
                if need.get(id(s), (None, 0))[1] < v:
                    need[id(s)] = (s, v)
            for (s, v) in need.values():
                known[eng][id(s)] = v
            streams[eng].append((list(need.values()), fn, sem, inc))
            for r in reads:
                readers.setdefault(r, []).append(tok)
            for w in writes:
                last_w[w] = tok
                readers[w] = []
        fin = []
        for e in ENGS:
            if cnt_c[e]:
                fin.append((sem_c[e], cnt_c[e]))
        for e in sem_d:
            for k in range(self.n_dma_sems):
                if cnt_d[e][k]:
                    fin.append((sem_d[e][k], cnt_d[e][k] * 16))
        if cnt_cc[0]:
            fin.append((sem_cc, cnt_cc[0]))
        self.n_instr = {e: len(streams[e]) for e in ENGS}
        self.n_instr['peak_sbuf'] = getattr(self, 'peak', 0)
        with nc.Block() as block:
            def mk(e):
                def body(engine):
                    for (waits, fn, sem, inc) in streams[e]:
                        for (s, v) in waits:
                            engine.wait_ge(s, v)
                        if fn is not None:
                            fn(engine).then_inc(sem, inc)
                    if e == "sp":
                        for (s, v) in fin:
                            engine.wait_ge(s, v)
                return body
            block.tensor(mk("pe"))
            block.scalar(mk("act"))
            block.vector(mk("dve"))
            block.gpsimd(mk("pool"))
            block.sync(mk("sp"))
        self.es.close()
        return nc


def make_cfg(T=4096, L=256, D=2048, F=2048, E=16, depth=2, debug=()):
    c = dict(T=T, L=L, D=D, F=F, E=E, depth=depth, debug=tuple(debug))
    c["NT"], c["NCT"] = T // 128, L // 128
    c["NTT"] = c["NT"] + c["NCT"]
    c["M"] = T + L
    c["HD"] = 128
    c["RH"] = D // 2 // 128
    c["RD"] = c["RH"] * 128
    c["GD"] = D - c["RD"]
    c["GG"] = 8
    c["GW"] = c["GD"] // 8
    c["NAH"] = D // 2 // 128
    c["NAD"] = c["NAH"] * 128
    c["GH"] = 8
    c["GDV"] = (D - c["NAD"]) // 8
    c["GDK"] = c["GDV"] // 2
    c["GQK"] = 8 * c["GDK"]
    c["GV"] = 8 * c["GDV"]
    c["EVC"] = 4 * c["RD"] + 2 * c["GD"]
    c["ODQ"] = c["NAD"] + c["GQK"] + c["GV"]
    c["ODC"] = c["ODQ"] + 2 * c["NAD"] + c["GQK"] + c["GV"] + 32
    c["capL"] = 2 * T // E
    c["capC"] = 2 * L // E
    c["rows"] = T // 64
    return c


def big_layout(c):
    D, F, E = c["D"], c["F"], c["E"]
    items = []
    for l in range(c["depth"]):
        items.append((f"ada{l}", D, 6 * D))
        items.append((f"win{l}", D, c["EVC"] if l % 2 == 0 else c["ODC"]))
        items.append((f"wout{l}", D, D))
        for e in range(E):
            items.append((f"wg{l}_{e}", D, F))
            items.append((f"wu{l}_{e}", D, F))
            items.append((f"wd{l}_{e}", F, D))
    off, table = 0, {}
    for (n, K, N) in items:
        table[n] = (off, K, N)
        off += K * N
    CH = 8 * 2048 * 16
    tot = (off + CH - 1) // CH * CH
    return table, tot


class K:
    def __init__(self, c):
        self.c = c
        self.P = Prog()
        self.nc = self.P.nc
        self.dbg = {}

    def D_(self, name, shape, dt, kind=None):
        if kind is None:
            kind = "ExternalOutput" if name in self.c["debug"] else "Internal"
        t = self.P.dram(name, shape, dt, kind)
        if kind == "ExternalOutput":
            self.dbg[name] = t
        return t.ap()

    def bcreg(self, eng, val):
        if not hasattr(self, "_bcregs"):
            self._bcregs = {}
        if val not in self._bcregs:
            self._bcregs[val] = eng.to_reg(val)
        return self._bcregs[val]

    def nkt(self):
        return min(5, self.c["NT"])

    def accb(self, i):
        return self.pp[i // 2][:, (i % 2) * 512:(i % 2) * 512 + 512]

    def tp(self, i):
        return self.pp[2][:].bitcast(BF16)[:, i * 1024:(i + 1) * 1024]

    def linear(self, tag, tiles, Kd, N, producer, W, epilogue, G=4, wkey=()):
        P = self.P
        KC = Kd // 128
        ident = self.ident
        NB = (N + 511) // 512
        ngrp = (len(tiles) + G - 1) // G
        for g in range(ngrp):
            grp = tiles[g * G:(g + 1) * G]
            xT = P.sb("lin_xT", [128, 16, max(G, 5) * 128], BF16)
            for j, (mt, rows) in enumerate(grp):
                i = g * G + j
                xb = P.sb(f"lin_xbf{i % 2}", [128, 2048], BF16)
                xkey = ("xbf", i % 2)
                producer(i, mt, rows, xb, xkey)
                for kc in range(KC):
                    tpi = (i * KC + kc) % 2
                    tp = self.tp(tpi)
                    P.op("pe", lambda e, tp=tp, xb=xb, kc=kc, rows=rows: e.transpose(
                        out=tp[:, 0:rows], in_=xb[0:rows, kc * 128:(kc + 1) * 128], identity=ident[0:rows, 0:rows]),
                        reads=[xkey, "ident"], writes=[("tp", tpi)])
                    ce = "act" if kc % 2 == 0 else "dve"
                    if ce == "act":
                        P.op("act", lambda e, tp=tp, kc=kc, j=j, rows=rows: e.copy(
                            out=xT[:, kc, j * 128:j * 128 + rows], in_=tp[:, 0:rows]),
                            reads=[("tp", tpi)], writes=[("xT", kc, j)])
                    else:
                        P.op("dve", lambda e, tp=tp, kc=kc, j=j, rows=rows: e.tensor_copy(
                            out=xT[:, kc, j * 128:j * 128 + rows], in_=tp[:, 0:rows]),
                            reads=[("tp", tpi)], writes=[("xT", kc, j)])
            def load_block(nb):
                ncols = min(512, N - nb * 512)
                wi = self.wcnt % 2
                self.wcnt += 1
                wb = P.sb(f"lin_wb{wi}", [128, 16, 512], BF16)
                for hf in range((KC + 7) // 8):
                    k0, k1 = hf * 8, min(KC, hf * 8 + 8)
                    si = self.scnt % 2
                    self.scnt += 1
                    wst = P.sb(f"lin_wst{si}", [128, 8, 512], F32)
                    src = W[k0 * 128:k1 * 128, nb * 512:nb * 512 + ncols].rearrange("(c p) n -> p c n", p=128)
                    P.dma("sp", wst[:, 0:k1 - k0, 0:ncols], src, reads=list(wkey), writes=[("wst", si)])
                    ce = "pool"
                    if ce == "pool":
                        P.op("pool", lambda e, wb=wb, wst=wst, k0=k0, k1=k1, ncols=ncols: e.tensor_copy(
                            out=wb[:, k0:k1, 0:ncols], in_=wst[:, 0:k1 - k0, 0:ncols]),
                            reads=[("wst", si)], writes=[("wb", wi, hf)])
                    else:
                        P.op("act", lambda e, wb=wb, wst=wst, k0=k0, k1=k1, ncols=ncols: e.copy(
                            out=wb[:, k0:k1, 0:ncols], in_=wst[:, 0:k1 - k0, 0:ncols]),
                            reads=[("wst", si)], writes=[("wb", wi, hf)])
                return (wb, wi, ncols)
            cur = load_block(0)
            for nb in range(NB):
                nxt = load_block(nb + 1) if nb + 1 < NB else None
                wb, wi, ncols = cur
                cur = nxt
                for j, (mt, rows) in enumerate(grp):
                    i = g * G + j
                    ai = self.acnt % 4
                    self.acnt += 1
                    acc = self.accb(ai)
                    for kc in range(KC):
                        P.op("pe", lambda e, acc=acc, kc=kc, j=j, rows=rows, wb=wb, ncols=ncols: e.matmul(
                            out=acc[0:rows, 0:ncols], lhsT=xT[:, kc, j * 128:j * 128 + rows], rhs=wb[:, kc, 0:ncols],
                            start=(kc == 0), stop=(kc == KC - 1)),
                            reads=[("xT", kc, j), ("wb", wi, kc // 8)], writes=[("acc", ai)])
                    epilogue(i, mt, rows, nb, ncols, acc, ("acc", ai))

    def setup_consts(self):
        P = self.P
        self.wcnt = self.scnt = self.acnt = 0
        identf = P.sb("identf", [128, 128], F32)
        ident = P.sb("ident", [128, 128], BF16)
        P.op("pool", lambda e: e.memset(identf[:], 0.0), writes=["identf"])
        P.op("pool", lambda e: e.affine_select(out=identf[:], in_=identf[:], pattern=[[-1, 128]],
                                               compare_op=ALU.not_equal, fill=1.0, base=0, channel_multiplier=1),
             reads=["identf"], writes=["identf"])
        P.op("dve", lambda e: e.tensor_copy(out=ident[:], in_=identf[:]), reads=["identf"], writes=["ident"])
        self.ident, self.identf = ident, identf
        self.pp = [P.ps(f"pp{i}", [128, 1024], F32) for i in range(4)]
        self.consts_attn()

    def build(self):
        c, P, nc = self.c, self.P, self.nc
        D, M, T, L = c["D"], c["M"], c["T"], c["L"]
        table, tot = big_layout(c)
        self.table = table
        EI = lambda n, s, dt=F32: P.dram(n, s, dt, "ExternalInput").ap()
        self.x_in = EI("x", [T, D])
        self.ctx_in = EI("ctx", [L, D])
        self.cc_in = EI("cc", [2, D])
        self.small = {}
        for l in range(c["depth"]):
            self.small[f"ada_b{l}"] = EI(f"ada_b{l}", [1, 6 * D])
            self.small[f"norm_mix{l}"] = EI(f"norm_mix{l}", [1, D])
            self.small[f"norm_ffn{l}"] = EI(f"norm_ffn{l}", [1, D])
            self.small[f"router{l}"] = EI(f"router{l}", [D, c["E"]])
        self.small["norm_final"] = EI("norm_final", [1, D])
        for l in range(c["depth"]):
            if l % 2 == 0:
                self.small[f"gamma{l}"] = EI(f"gamma{l}", [1, 2 * c["RH"]])
                self.small[f"ret_norm{l}"] = EI(f"ret_norm{l}", [1, c["RD"]])
                self.small[f"gmlp_norm{l}"] = EI(f"gmlp_norm{l}", [1, c["GD"]])
                self.small[f"gmlp_wsT{l}"] = EI(f"gmlp_wsT{l}", [8, 128, 128])
                self.small[f"gmlp_bsT{l}"] = EI(f"gmlp_bsT{l}", [128, 8])
            else:
                self.small[f"gla_wup{l}"] = EI(f"gla_wup{l}", [2, 16, c["GQK"]])
                self.small[f"gla_bup{l}"] = EI(f"gla_bup{l}", [1, 2, c["GQK"]])
                self.small[f"gla_norm{l}"] = EI(f"gla_norm{l}", [1, c["GV"]])
                self.small[f"na_bias{l}"] = EI(f"na_bias{l}", [6, c["NAH"], 128, self.nkt() * 128])
        self.small["rot_cs"] = EI("rot_cs", [T, 128])
        self.small["rot_sn"] = EI("rot_sn", [T, 128])
        self.out = P.dram("out", [T, D], F32, "ExternalOutput").ap()
        self.setup_consts()
        E, F = c["E"], c["F"]
        self.W = {}
        for l in range(c["depth"]):
            self.W[f"ada{l}"] = EI(f"ada{l}", [D, 6 * D])
            self.W[f"win{l}"] = EI(f"win{l}", [D, c["EVC"] if l % 2 == 0 else c["ODC"]])
            self.W[f"wout{l}"] = EI(f"wout{l}", [D, D])
            wg = EI(f"wg{l}", [E * D, F]); wu = EI(f"wu{l}", [E * D, F]); wd = EI(f"wd{l}", [E * F, D])
            for e in range(E):
                self.W[f"wg{l}_{e}"] = wg[e * D:(e + 1) * D, :]
                self.W[f"wu{l}_{e}"] = wu[e * D:(e + 1) * D, :]
                self.W[f"wd{l}_{e}"] = wd[e * F:(e + 1) * F, :]
        self.Wm = lambda name: self.W[name]
        self.XC = self.D_("XC", [M, D], F32)
        self.X1 = self.D_("X1", [M, D], F32)
        for mt in range(c["NTT"]):
            src = self.x_in[mt * 128:(mt + 1) * 128, :] if mt < c["NT"] else \
                self.ctx_in[(mt - c["NT"]) * 128:(mt - c["NT"] + 1) * 128, :]
            P.dma("sp", self.XC[mt * 128:(mt + 1) * 128, :], src, writes=[("XC", mt)])
        self.phase_mod()
        for l in range(c["depth"]):
            self.layer(l)
            if f"stop_l{l}" in c["debug"]:
                break
        self.phase_final()
        return P.emit()

    def phase_mod(self):
        c, P = self.c, self.P
        D = c["D"]
        self.MOD = []
        P.push()
        ccs = P.sb("S5", [128, 2048], F32)
        P.op("pool", lambda e: e.memset(ccs[:], 0.0), writes=["S5"])
        P.dma("sp", ccs[0:2, 0:D], self.cc_in[:, :], reads=["S5"], writes=["S5"])
        for l in range(c["depth"]):
            MODl = self.D_(f"MOD{l}", [2, 6 * D], F32)
            self.MOD.append(MODl)
            bias = None

            def prod(i, mt, rows, xb, xkey):
                P.op("act", lambda e: e.activation(out=xb[:, 0:D], in_=ccs[:, 0:D], func=AF.Silu),
                     reads=["S5"], writes=[xkey])

            def epi(i, mt, rows, nb, ncols, acc, akey, l=l, MODl=MODl, bias=bias):
                st = P.sb("mod_st", [2, 512], F32)
                bst = P.sb("mod_bst", [2, 512], F32)
                bsrc = self.small[f"ada_b{l}"][0:1, nb * 512:nb * 512 + ncols]
                P.dma("sp", bst[0:1, 0:ncols], bsrc, writes=["mod_bst0"])
                P.dma("sp", bst[1:2, 0:ncols], bsrc, writes=["mod_bst1"])
                P.op("dve", lambda e: e.tensor_tensor(out=st[0:2, 0:ncols], in0=acc[0:2, 0:ncols],
                                                      in1=bst[0:2, 0:ncols], op=ALU.add),
                     reads=[akey, "mod_bst0", "mod_bst1"], writes=["mod_st"])
                P.dma("sp", MODl[:, nb * 512:nb * 512 + ncols], st[0:2, 0:ncols], reads=["mod_st"],
                      writes=[("MOD", l, nb)])
            self.linear(f"mod{l}", [(0, 128)], D, 6 * D, prod, self.Wm(f"ada{l}"), epi)
        P.pop()

    def load_modvec(self, name, l, row, seg, gain=None, plus1=False):
        c, P = self.c, self.P
        D = c["D"]
        t = P.sb(name, [128, 2048], F32)
        src = self.MOD[l][row:row + 1, seg * D:(seg + 1) * D]
        rk = [("MOD", l, nb) for nb in range(seg * D // 512, (seg + 1) * D // 512)]
        P.dma("sp", t[:, 0:D], src.partition_broadcast(128), reads=rk, writes=[name])
        if gain is not None:
            g = P.sb("S4", [128, 2048], F32)
            P.dma("sp", g[:, 0:D], self.small[gain].partition_broadcast(128), writes=["S4"])
            P.op("dve", lambda e: e.scalar_tensor_tensor(out=t[:, 0:D], in0=t[:, 0:D], scalar=1.0 if plus1 else 0.0, in1=g[:, 0:D],
                                                         op0=ALU.add, op1=ALU.mult),
                 reads=[name, "S4"], writes=[name])
        return t

    def norm_tile(self, src_ap, src_keys, A, Akey, Bv, Bkey, out_bf, out_key, xname="S5"):
        c, P = self.c, self.P
        D = c["D"]
        xt = P.sb(xname, [128, 2048], F32)
        junk = P.sb("nt_junk", [128, 2048], BF16)
        ss = P.sb("nt_ss", [128, 1], F32)
        rs = P.sb("nt_rs", [128, 1], F32)
        P.dma("sp", xt[:, 0:D], src_ap, reads=src_keys, writes=[xname])
        P.op("act", lambda e: e.activation(out=junk[:, 0:D], in_=xt[:, 0:D], func=AF.Square, accum_out=ss[:]),
             reads=[xname], writes=["nt_junk", "nt_ss"])
        P.op("dve", lambda e: e.tensor_scalar(out=rs[:], in0=ss[:], scalar1=1.0 / D, scalar2=1e-6,
                                              op0=ALU.mult, op1=ALU.add), reads=["nt_ss"], writes=["nt_rs"])
        P.op("act", lambda e: e.activation(out=rs[:], in_=rs[:], func=AF.Sqrt), reads=["nt_rs"], writes=["nt_rs"])
        P.op("dve", lambda e: e.reciprocal(out=rs[:], in_=rs[:]), reads=["nt_rs"], writes=["nt_rs"])
        P.op("dve", lambda e: e.scalar_tensor_tensor(out=xt[:, 0:D], in0=xt[:, 0:D], scalar=rs[:, 0:1], in1=A[:, 0:D],
                                                     op0=ALU.mult, op1=ALU.mult),
             reads=[xname, "nt_rs", Akey], writes=[xname])
        P.op("pool", lambda e: e.tensor_tensor(out=out_bf[:, 0:D], in0=xt[:, 0:D], in1=Bv[:, 0:D], op=ALU.add),
             reads=[xname, Bkey], writes=[out_key])

    def layer(self, l):
        c, P = self.c, self.P
        D, M = c["D"], c["M"]
        last = (l == c["depth"] - 1)
        even = (l % 2 == 0)
        NCOL = c["EVC"] if even else c["ODC"]
        PB = self.D_(f"PB{l}", [M, NCOL], BF16)
        self.PB = PB
        P.push()
        A1 = self.load_modvec("S0", l, 0, 1, gain=f"norm_mix{l}", plus1=True)
        B1 = self.load_modvec("S1", l, 0, 0)
        A1c = self.load_modvec("S2", l, 1, 1, gain=f"norm_mix{l}", plus1=True)
        B1c = self.load_modvec("S3", l, 1, 0)
        tiles = [(mt, 128) for mt in range(c["NTT"])]

        def prod(i, mt, rows, xb, xkey):
            isc = mt >= c["NT"]
            self.norm_tile(self.XC[mt * 128:(mt + 1) * 128, :], [("XC", mt)],
                           A1c if isc else A1, "S2" if isc else "S0",
                           B1c if isc else B1, "S3" if isc else "S1", xb, xkey)

        def epi(i, mt, rows, nb, ncols, acc, akey):
            oi = self.ocnt % 2
            self.ocnt += 1
            st = P.sb(f"p1_st{oi}", [128, 512], BF16)
            P.op("act", lambda e: e.copy(out=st[:, 0:ncols], in_=acc[:, 0:ncols]), reads=[akey],
                 writes=[("p1_st", oi)])
            P.dma("sp", PB[mt * 128:(mt + 1) * 128, nb * 512:nb * 512 + ncols], st[:, 0:ncols],
                  reads=[("p1_st", oi)], writes=[("PB", mt, nb)])
        self.ocnt = 0
        self.linear(f"win{l}", tiles, D, NCOL, prod, self.Wm(f"win{l}"), epi, G=8)
        P.pop()
        if "stop_p1" in c["debug"] or f"stop_p1_{l}" in c["debug"]:
            return
        self.MIX = self.D_(f"MIX{l}", [M, D], BF16)
        for fn in ((lambda: self.gmlp(l, last)), (lambda: self.decay_attn(l, "ret", last))) if even else \
                ((lambda: self.nattn(l)), (lambda: self.decay_attn(l, "gla", last))):
            if (not even) and (("nattn" in fn.__code__.co_names and "skip_na" in c["debug"]) or ("decay_attn" in fn.__code__.co_names and "skip_gla" in c["debug"])):
                continue
            P.push()
            fn()
            P.pop()
        if "stop_p2" in c["debug"] or f"stop_p2_{l}" in c["debug"]:
            return
        ntl = c["NT"] if last else c["NTT"]
        tiles = [(mt, 128) for mt in range(ntl)]
        P.push()
        G1 = self.load_modvec("S0", l, 0, 2)
        G1c = self.load_modvec("S1", l, 1, 2) if not last else None
        X1 = self.X1
        mixkeys = lambda mt: [("MIX", mt, k_, s_) for (k_, s_) in ((("gmlp", 0), ("ret", 0)) if even else (("na", 0), ("gla", 0)))]

        def prod3(i, mt, rows, xb, xkey):
            P.dma("sp", xb[:, 0:D], self.MIX[mt * 128:(mt + 1) * 128, :], reads=mixkeys(mt), writes=[xkey])

        def epi3(i, mt, rows, nb, ncols, acc, akey):
            oi = self.ocnt % 2
            self.ocnt += 1
            xt = P.sb(f"p3_x{oi}", [128, 512], F32)
            g = G1c if mt >= c["NT"] else G1
            gk = "S1" if mt >= c["NT"] else "S0"
            P.dma("sp", xt[:, 0:ncols], self.XC[mt * 128:(mt + 1) * 128, nb * 512:nb * 512 + ncols], reads=[("XC", mt)], writes=[("p3_x", oi)])
            st = P.sb(f"p3_st{oi}", [128, 512], F32)
            P.op("dve", lambda e: e.tensor_tensor(out=st[:, 0:ncols], in0=acc[:, 0:ncols], in1=g[:, nb * 512:nb * 512 + ncols], op=ALU.mult),
                 reads=[akey, gk], writes=[("p3_st", oi)])
            P.op("pool", lambda e: e.tensor_tensor(out=st[:, 0:ncols], in0=st[:, 0:ncols], in1=xt[:, 0:ncols], op=ALU.add),
                 reads=[("p3_st", oi), ("p3_x", oi)], writes=[("p3_st", oi)])
            P.dma("sp", X1[mt * 128:(mt + 1) * 128, nb * 512:nb * 512 + ncols], st[:, 0:ncols], reads=[("p3_st", oi)], writes=[("X1", mt, nb)])
        self.linear(f"wout{l}", tiles, D, D, prod3, self.Wm(f"wout{l}"), epi3, G=8)
        P.pop()
        if "stop_p3" in c["debug"] or f"stop_p3_{l}" in c["debug"]:
            return
        self.moe(l, last, tiles)

    def moe(self, l, last, tiles):
        c, P = self.c, self.P
        D, M, E, F, NT = c["D"], c["M"], c["E"], c["F"], c["NT"]
        self.consts_attn()
        capL, capC = c["capL"], (0 if last else c["capC"])
        SLOTS = capL + capC
        H2 = self.D_(f"H2_{l}", [M, D], BF16)
        AFF = self.D_(f"AFF{l}", [M, E], F32)
        P.push()
        affT = P.sb("moe_affT", [16, M], F32)
        idxT = P.sb("moe_idxT", [128, 34, 16], I32)
        wT = P.sb("moe_wT", [128, 34, 16], F32)
        P.push()
        A2 = self.load_modvec("S0", l, 0, 4, gain=f"norm_ffn{l}", plus1=True)
        B2 = self.load_modvec("S1", l, 0, 3)
        if not last:
            A2c = self.load_modvec("S2", l, 1, 4, gain=f"norm_ffn{l}", plus1=True)
            B2c = self.load_modvec("S3", l, 1, 3)
        x1keys = lambda mt: [("X1", mt, nb) for nb in range(D // 512)]

        def prod(i, mt, rows, xb, xkey):
            isc = mt >= NT
            self.norm_tile(self.X1[mt * 128:(mt + 1) * 128, :], x1keys(mt), A2c if isc else A2, "S2" if isc else "S0",
                           B2c if isc else B2, "S3" if isc else "S1", xb, xkey)
            P.dma("sp", H2[mt * 128:(mt + 1) * 128, :], xb[:, 0:D], reads=[xkey], writes=[("H2", mt)])

        def epi(i, mt, rows, nb, ncols, acc, akey):
            lg = P.sb("moe_lg", [128, 16], F32)
            mx = P.sb("moe_mx", [128, 1], F32)
            sm = P.sb("moe_sm", [128, 1], F32)
            P.op("dve", lambda e: e.tensor_reduce(out=mx[:], in_=acc[:, 0:E], axis=mybir.AxisListType.X, op=ALU.max), reads=[akey], writes=["moe_mx"])
            P.op("dve", lambda e: e.tensor_scalar(out=mx[:], in0=mx[:], scalar1=-1.0, scalar2=None, op0=ALU.mult), reads=["moe_mx"], writes=["moe_mx"])
            P.op("act", lambda e: e.activation(out=lg[:, 0:E], in_=acc[:, 0:E], func=AF.Exp, bias=mx[:, 0:1], accum_out=sm[:]),
                 reads=[akey, "moe_mx"], writes=["moe_lg", "moe_sm"])
            P.op("dve", lambda e: e.reciprocal(out=sm[:], in_=sm[:]), reads=["moe_sm"], writes=["moe_sm"])
            P.op("dve", lambda e: e.tensor_scalar(out=lg[:, 0:E], in0=lg[:, 0:E], scalar1=sm[:, 0:1], scalar2=None, op0=ALU.mult),
                 reads=["moe_lg", "moe_sm"], writes=["moe_lg"])
            P.dma("sp", AFF[mt * 128:(mt + 1) * 128, :], lg[:, 0:E], reads=["moe_lg"], writes=[("AFF", mt)])
            tpf = self.pp[2]
            P.op("pe", lambda e: e.transpose(out=tpf[0:E, 0:128], in_=lg[:, 0:E], identity=self.identf[:, :]), reads=["moe_lg", "identf"], writes=[("tp", 0)])
            P.op("act", lambda e: e.copy(out=affT[0:E, mt * 128:(mt + 1) * 128], in_=tpf[0:E, 0:128]), reads=[("tp", 0)], writes=[("affT", mt)])
        self.linear(f"router{l}", tiles, D, E, prod, self.small[f"router{l}"], epi)
        P.pop()
        P.push()
        ones16 = P.sb("mo_ones", [16, 2048], F32)
        P.op("pool", lambda e: e.memset(ones16[:], 1.0), writes=["mo_ones"])
        work = P.sb("moe_work", [16, M], F32)
        m8 = P.sb("moe_m8", [16, 8], F32)
        idxf = P.sb("moe_idxf", [16, M], F32)
        segs = [(0, c["T"], capL, 0)] + ([] if last else [(c["T"], c["L"], capC, capL)])
        ntl = len(tiles)
        for (o0, n, cap, slot0) in segs:
            rk = [("affT", mt) for mt in range(o0 // 128, (o0 + n) // 128)]
            P.op("dve", lambda e, o0=o0, n=n: e.tensor_copy(out=work[0:E, o0:o0 + n], in_=affT[0:E, o0:o0 + n]), reads=rk, writes=["moe_work"])
            for it in range(cap // 8):
                P.op("dve", lambda e, o0=o0, n=n: e.max(out=m8[0:E, :], in_=work[0:E, o0:o0 + n]), reads=["moe_work"], writes=["moe_m8"])
                if it < cap // 8 - 1:
                    P.op("dve", lambda e, o0=o0, n=n: e.match_replace(out=work[0:E, o0:o0 + n], in_to_replace=m8[0:E, :], in_values=work[0:E, o0:o0 + n],
                                                                      imm_value=-1.0), reads=["moe_work", "moe_m8"], writes=["moe_work"])
            sel = work
            P.op("dve", lambda e, o0=o0, n=n: e.tensor_scalar(out=sel[0:E, o0:o0 + n], in0=affT[0:E, o0:o0 + n], scalar1=m8[0:E, 7:8], scalar2=None,
                                                              op0=ALU.is_ge), reads=rk + ["moe_m8", "moe_work"], writes=["moe_work"])
            for q0 in range(0, n, 2048):
                qn = min(2048, n - q0)
                init = 0.0 if q0 == 0 else idxf[0:E, o0 + q0 - 1:o0 + q0]
                P.op("dve", lambda e, a=o0 + q0, qn=qn, init=init: e.tensor_tensor_scan(out=idxf[0:E, a:a + qn], data0=ones16[0:E, 0:qn], data1=sel[0:E, a:a + qn],
                                                                                        initial=init, op0=ALU.mult, op1=ALU.add), reads=["moe_work", "mo_ones", "moe_idxf"], writes=["moe_idxf"])
            P.op("dve", lambda e, o0=o0, n=n, cap=cap: e.scalar_tensor_tensor(out=sel[0:E, o0:o0 + n], in0=idxf[0:E, o0:o0 + n], scalar=float(cap) + 0.5,
                                                                              in1=sel[0:E, o0:o0 + n], op0=ALU.is_le, op1=ALU.mult),
                 reads=["moe_work", "moe_idxf"], writes=["moe_work"])
            P.op("dve", lambda e, o0=o0, n=n, slot0=slot0: e.tensor_scalar(out=idxf[0:E, o0:o0 + n], in0=idxf[0:E, o0:o0 + n], scalar1=float(slot0 - 1) - BIG,
                                                                           scalar2=None, op0=ALU.add), reads=["moe_idxf"], writes=["moe_idxf"])
            P.op("dve", lambda e, o0=o0, n=n: e.tensor_tensor(out=idxf[0:E, o0:o0 + n], in0=idxf[0:E, o0:o0 + n], in1=sel[0:E, o0:o0 + n], op=ALU.mult),
                 reads=["moe_idxf", "moe_work"], writes=["moe_idxf"])
            P.op("dve", lambda e, o0=o0, n=n: e.tensor_scalar(out=idxf[0:E, o0:o0 + n], in0=idxf[0:E, o0:o0 + n], scalar1=BIG, scalar2=None, op0=ALU.add),
                 reads=["moe_idxf"], writes=["moe_idxf"])
        tpf = self.pp[2]
        for (mt, rows) in tiles:
            P.op("pe", lambda e, mt=mt: e.transpose(out=tpf[:, 0:E], in_=idxf[0:E, mt * 128:(mt + 1) * 128], identity=self.identf[0:E, 0:E]),
                 reads=["moe_idxf", "identf"], writes=[("tp", 0)])
            P.op("pe", lambda e, mt=mt: e.transpose(out=tpf[:, 512:512 + E], in_=affT[0:E, mt * 128:(mt + 1) * 128], identity=self.identf[0:E, 0:E]),
                 reads=[("affT", mt), "identf"], writes=[("tp", 1)])
            P.op("dve", lambda e, mt=mt: e.tensor_copy(out=idxT[:, mt, :], in_=tpf[:, 0:E]), reads=[("tp", 0)], writes=[("idxT", mt)])
            P.op("dve", lambda e, mt=mt: e.tensor_scalar(out=wT[:, mt, :], in0=tpf[:, 0:E], scalar1=BIG / 2, scalar2=None, op0=ALU.is_lt),
                 reads=[("tp", 0)], writes=[("wT", mt)])
            P.op("dve", lambda e, mt=mt: e.tensor_tensor(out=wT[:, mt, :], in0=wT[:, mt, :], in1=tpf[:, 512:512 + E], op=ALU.mult),
                 reads=[("wT", mt), ("tp", 1)], writes=[("wT", mt)])
        P.pop()
        P.push()
        XSe = [self.D_(f"XS{l}_{ex}", [SLOTS, D], BF16) for ex in range(E)]
        YSe = [self.D_(f"YS{l}_{ex}", [SLOTS, D], F32) for ex in range(E)]
        for (mt, rows) in tiles:
            hb = P.sb(f"lin_xbf{mt % 2}", [128, 2048], BF16)
            hk = ("xbf", mt % 2)
            P.dma("sp", hb[:, 0:D], H2[mt * 128:(mt + 1) * 128, :], reads=[("H2", mt)], writes=[hk])
            for ex in range(E):
                P.op("pool", lambda e, mt=mt, ex=ex, hb=hb: e.indirect_dma_start(
                    out=XSe[ex][:, :], out_offset=bass.IndirectOffsetOnAxis(ap=idxT[:, mt, ex:ex + 1], axis=0),
                    in_=hb[:, 0:D], in_offset=None, bounds_check=self.bcreg(e, SLOTS - 1), oob_is_err=False),
                    reads=[hk, ("idxT", mt)], writes=[("XS", ex, mt)], dma=True)
        P.pop()
        P.push()
        stiles = [(j, min(128, SLOTS - j * 128)) for j in range((SLOTS + 127) // 128)]
        HS = self.D_(f"HS{l}", [SLOTS, F], BF16)
        GA = self.D_(f"GA{l}", [SLOTS, F], BF16)
        for ex in range(E):
            xsk = [("XS", ex, mt) for (mt, _) in tiles]

            def prodx(i, j, rows, xb, xkey, ex=ex):
                P.dma("sp", xb[0:rows, 0:D], XSe[ex][j * 128:j * 128 + rows, :], reads=xsk, writes=[xkey])

            def epig(i, j, rows, nb, ncols, acc, akey, ex=ex):
                oi = self.ocnt % 2
                self.ocnt += 1
                st = P.sb(f"p1_st{oi}", [128, 512], BF16)
                P.op("act", lambda e: e.activation(out=st[0:rows, 0:ncols], in_=acc[0:rows, 0:ncols], func=AF.Silu), reads=[akey], writes=[("p1_st", oi)])
                P.dma("sp", GA[j * 128:j * 128 + rows, nb * 512:nb * 512 + ncols], st[0:rows, 0:ncols], reads=[("p1_st", oi)], writes=[("GA", j, nb)])

            def epiu(i, j, rows, nb, ncols, acc, akey, ex=ex):
                oi = self.ocnt % 2
                self.ocnt += 1
                ga = P.sb(f"p1_st{oi}", [128, 512], BF16)
                st = P.sb(f"mo_hs{oi}", [128, 512], BF16)
                P.dma("sp", ga[0:rows, 0:ncols], GA[j * 128:j * 128 + rows, nb * 512:nb * 512 + ncols], reads=[("GA", j, nb)], writes=[("p1_st", oi)])
                P.op("dve", lambda e: e.tensor_tensor(out=st[0:rows, 0:ncols], in0=acc[0:rows, 0:ncols], in1=ga[0:rows, 0:ncols], op=ALU.mult),
                     reads=[akey, ("p1_st", oi)], writes=[("mo_hs", oi)])
                P.dma("sp", HS[j * 128:j * 128 + rows, nb * 512:nb * 512 + ncols], st[0:rows, 0:ncols], reads=[("mo_hs", oi)], writes=[("HS", j, nb)])

            def prodh(i, j, rows, xb, xkey):
                P.dma("sp", xb[0:rows, 0:F], HS[j * 128:j * 128 + rows, :], reads=[("HS", j, nb) for nb in range((F + 511) // 512)], writes=[xkey])

            def epid(i, j, rows, nb, ncols, acc, akey, ex=ex):
                oi = self.ocnt % 2
                self.ocnt += 1
                st = P.sb(f"p3_st{oi}", [128, 512], F32)
                P.op("act", lambda e: e.copy(out=st[0:rows, 0:ncols], in_=acc[0:rows, 0:ncols]), reads=[akey], writes=[("p3_st", oi)])
                P.dma("sp", YSe[ex][j * 128:j * 128 + rows, nb * 512:nb * 512 + ncols], st[0:rows, 0:ncols],
                      reads=[("p3_st", oi)], writes=[("YS", ex, j, nb)])
            self.linear(f"g{l}_{ex}", stiles, D, F, prodx, self.Wm(f"wg{l}_{ex}"), epig, G=len(stiles))
            self.linear(f"u{l}_{ex}", stiles, D, F, prodx, self.Wm(f"wu{l}_{ex}"), epiu, G=len(stiles))
            self.linear(f"d{l}_{ex}", stiles, F, D, prodh, self.Wm(f"wd{l}_{ex}"), epid, G=len(stiles))
        P.pop()
        P.push()
        G2 = self.load_modvec("S0", l, 0, 5)
        G2c = self.load_modvec("S1", l, 1, 5) if not last else None
        for (mt, rows) in tiles:
            isc = mt >= NT
            accs = P.sb("S2", [128, 2048], F32)
            x1t = P.sb("S3", [128, 2048], F32)
            P.dma("sp", x1t[:, 0:D], self.X1[mt * 128:(mt + 1) * 128, :], reads=x1keys(mt), writes=["S3"])
            P.op("pool", lambda e: e.memset(accs[:, 0:D], 0.0), writes=["S2"])
            for ex in range(E):
                bi = ex % 4
                buf = P.sb(f"S{4 + bi}", [128, 2048], F32)
                if mt == tiles[0][0] and ex < 4:
                    P.op("pool", lambda e, buf=buf: e.memset(buf[:, 0:D], 0.0), writes=[f"S{4 + bi}"])
                yk = [("YS", ex, j, nb) for (j, _) in stiles for nb in range(D // 512)]
                P.op("pool", lambda e, mt=mt, ex=ex, buf=buf: e.indirect_dma_start(
                    out=buf[:, 0:D], out_offset=None, in_=YSe[ex][:, :],
                    in_offset=bass.IndirectOffsetOnAxis(ap=idxT[:, mt, ex:ex + 1], axis=0), bounds_check=self.bcreg(e, SLOTS - 1), oob_is_err=False),
                    reads=yk + [("idxT", mt)], writes=[f"S{4 + bi}"], dma=True)
                P.op("dve", lambda e, mt=mt, ex=ex, buf=buf: e.scalar_tensor_tensor(out=accs[:, 0:D], in0=buf[:, 0:D], scalar=wT[:, mt, ex:ex + 1], in1=accs[:, 0:D],
                                                                                    op0=ALU.mult, op1=ALU.add), reads=[f"S{4 + bi}", ("wT", mt), "S2"], writes=["S2"])
            g = G2c if isc else G2
            P.op("pool", lambda e, g=g: e.tensor_tensor(out=accs[:, 0:D], in0=accs[:, 0:D], in1=g[:, 0:D], op=ALU.mult), reads=["S2", "S1" if isc else "S0"], writes=["S2"])
            P.op("dve", lambda e: e.tensor_tensor(out=accs[:, 0:D], in0=accs[:, 0:D], in1=x1t[:, 0:D], op=ALU.add), reads=["S2", "S3"], writes=["S2"])
            P.dma("sp", self.XC[mt * 128:(mt + 1) * 128, :], accs[:, 0:D], reads=["S2"], writes=[("XC", mt)])
        P.pop()
        P.pop()

    def consts_attn(self):
        P = self.P
        if hasattr(self, "maskF"):
            return
        ones = P.sb("c_ones", [128, 128], F32)
        self.maskF = P.sb("c_maskF", [128, 128], F32)
        self.maskB = P.sb("c_maskB", [128, 128], F32)
        P.op("pool", lambda e: e.memset(ones[:], 1.0), writes=["c_ones"])
        P.op("pool", lambda e: e.affine_select(out=self.maskF[:], in_=ones[:], pattern=[[1, 128]], compare_op=ALU.is_ge,
                                               fill=0.0, base=0, channel_multiplier=-1), reads=["c_ones"], writes=["c_maskF", "c_maskB"])
        P.op("pool", lambda e: e.affine_select(out=self.maskB[:], in_=ones[:], pattern=[[-1, 128]], compare_op=ALU.is_ge,
                                               fill=0.0, base=0, channel_multiplier=1), reads=["c_ones"], writes=["c_maskF", "c_maskB"])
        self.ones = ones
        pi = P.sb("c_pi", [128, 1], I32)
        pf = P.sb("c_pf", [128, 8], F32)
        P.op("pool", lambda e: e.iota(out=pi[:], pattern=[[0, 1]], base=0, channel_multiplier=1), writes=["c_pi"])
        P.op("dve", lambda e: e.tensor_copy(out=pf[:, 0:1], in_=pi[:]), reads=["c_pi"], writes=["c_pf"])
        for j, (m, a) in enumerate([(1.0, 1.0), (-1.0, -1.0), (1.0, -127.0), (-1.0, 128.0), (1.0, -128.0), (-1.0, 0.0)]):
            P.op("dve", lambda e, j=j, m=m, a=a: e.tensor_scalar(out=pf[:, j + 1:j + 2], in0=pf[:, 0:1], scalar1=m, scalar2=a,
                                                               op0=ALU.mult, op1=ALU.add), reads=["c_pf"], writes=["c_pf"])
        self.pf = pf

    def decay_attn(self, l, kind, last):
        c, P = self.c, self.P
        PB = self.PB
        NT, NTT = c["NT"], c["NTT"]
        C = 128
        if kind == "ret":
            H, dk, dv = c["RH"], 128, 128
            qo, go, ko, vo = 0, c["RD"], 2 * c["RD"] + 2 * c["GD"], 3 * c["RD"] + 2 * c["GD"]
            qscale = 128 ** -0.5
            mixo = 0
            gnorm_name = f"ret_norm{l}"
            NCOLS = c["EVC"]
        else:
            H, dk, dv = 8, c["GDK"], c["GDV"]
            qo = c["NAD"]
            go = c["NAD"] + c["GQK"]
            ko = c["ODQ"] + 2 * c["NAD"]
            vo = ko + c["GQK"]
            lro = vo + c["GV"]
            qscale = dk ** -0.5
            mixo = c["NAD"]
            gnorm_name = f"gla_norm{l}"
            NCOLS = c["ODC"]
        hp = 128 // dk
        npk = H // hp
        HK, HV = H * dk, H * dv
        lnq = math.log(qscale)
        OF = self.D_(f"OF{l}", [c["M"], HV], F32)
        lat_units = list(range(NT))
        ctx_units = list(range(NT, NTT))
        gn = P.sb("at_gn", [128, 1024], F32)
        P.dma("sp", gn[:, 0:HV], self.small[gnorm_name].partition_broadcast(128), writes=["at_gn"])
        S32 = P.sb("at_S32", [128, 1024], F32)
        Sbf = P.sb("at_Sbf", [128, 1024], BF16)
        qt = P.sb("at_q", [128, 1024], BF16)
        kt = P.sb("at_k", [128, 1024], BF16)
        vt = P.sb("at_v", [128, 1024], BF16)
        gt = P.sb("at_g", [128, 1024], BF16)
        qin = P.sb("at_qin", [128, 1024], BF16)
        kin = P.sb("at_kin", [128, 1024], BF16)
        kout = P.sb("at_kout", [128, 1024], BF16)
        qT = P.sb("at_qT", [128, 8, 128], BF16)
        kT = P.sb("at_kT", [128, 8, 128], BF16)
        attT = P.sb("at_attT", [128, 8, 128], BF16)
        osb = P.sb("S4", [128, 2048], F32)
        ofl = P.sb("S5", [128, 2048], F32)
        pf = self.pf
        if hp > 1:
            rowm = P.sb("at_rowm", [128, 4], F32)
            P.op("pool", lambda e: e.memset(rowm[:, :], 0.0), writes=["at_rowm"])
            for j in range(hp):
                P.op("pool", lambda e, j=j: e.memset(rowm[j * dk:(j + 1) * dk, j:j + 1], 1.0), reads=["at_rowm"], writes=["at_rowm"])
        if kind == "ret":
            gl = P.sb("at_gl", [128, 16], F32)
            P.dma("sp", gl[:, 0:2 * H], self.small[f"gamma{l}"].partition_broadcast(128), writes=["at_gl"])
            P.op("act", lambda e: e.activation(out=gl[:, 0:2 * H], in_=gl[:, 0:2 * H], func=AF.Exp, scale=-1.0),
                 reads=["at_gl"], writes=["at_gl"])
            P.op("act", lambda e: e.activation(out=gl[:, 0:2 * H], in_=gl[:, 0:2 * H], func=AF.Ln, bias=1.0),
                 reads=["at_gl"], writes=["at_gl"])
            tb = P.sb("at_tb", [128, 2, 4, 8], F32)
            for d in range(2):
                sp_d = gl[:, d * H:(d + 1) * H]
                cols = [(2, lnq), (1, 0.0), (3, 0.0)] if d == 0 else [(5, lnq), (4, 0.0), (6, 0.0)]
                for j, (pc, bias) in enumerate(cols):
                    P.op("act", lambda e, d=d, j=j, pc=pc, bias=bias, sp_d=sp_d: e.activation(
                        out=tb[:, d, j, 0:H], in_=sp_d, func=AF.Exp, scale=pf[:, pc:pc + 1], bias=bias),
                        reads=["at_gl", "c_pf"], writes=["at_tb"])
                P.op("act", lambda e, d=d, sp_d=sp_d: e.activation(out=tb[:, d, 3, 0:H], in_=sp_d, func=AF.Exp, scale=-128.0),
                     reads=["at_gl"], writes=["at_tb"])
            rcs = P.sb("at_rcs", [128, 128], F32)
            rsn = P.sb("at_rsn", [128, 128], F32)
        else:
            lrT = P.sb("at_lrT", [128, 128], BF16)
            wup = P.sb("at_wup", [128, 2, 512], F32)
            wuph = P.sb("at_wuph", [128, 2, 512], BF16)
            wupl = P.sb("at_wupl", [128, 2, 512], BF16)
            P.op("pool", lambda e: e.memset(lrT[:, :], 1.0), writes=["at_lrT"])
            P.op("pool", lambda e: e.memset(wup[:, :, :], 0.0), writes=["at_wup"])
            P.dma("sp", wup[0:16, :, 0:HK], self.small[f"gla_wup{l}"].rearrange("d r n -> r d n"), reads=["at_wup"], writes=["at_wup"])
            P.dma("sp", wup[16:17, :, 0:HK], self.small[f"gla_bup{l}"], reads=["at_wup"], writes=["at_wup"])
            P.op("dve", lambda e: e.tensor_copy(out=wuph[:, :, :], in_=wup[:, :, :]), reads=["at_wup"], writes=["at_wuph"])
            P.op("dve", lambda e: e.tensor_tensor(out=wupl[:, :, :], in0=wup[:, :, :], in1=wuph[:, :, :], op=ALU.subtract),
                 reads=["at_wup", "at_wuph"], writes=["at_wupl"])
            sph = P.sb("at_sph", [128, 512], BF16)
            spl = P.sb("at_spl", [128, 512], BF16)
            sp = P.sb("at_sp", [128, 512], F32)
            bsb = P.sb("at_bsb", [128, 512], F32)
            EQ = P.sb("at_EQ", [128, 512], F32)
            EKI = P.sb("at_EKI", [128, 512], F32)
            EKO = P.sb("at_EKO", [128, 512], F32)
            dec = P.sb("at_dec", [128, 4, 2], F32)
            LmF = P.sb("at_LmF", [128, 128], BF16)
            LmB = P.sb("at_LmB", [128, 128], BF16)
            Em = P.sb("at_Em", [128, 128], BF16)
            nsc = P.sb("at_nsc", [128, 2], BF16)
            P.op("dve", lambda e: e.tensor_scalar(out=LmF[:], in0=self.maskF[:, :], scalar1=-1.0 / 16, scalar2=None,
                                                  op0=ALU.mult), reads=["c_maskF"], writes=["at_LmF"])
            P.op("dve", lambda e: e.tensor_scalar(out=LmB[:], in0=self.maskB[:, :], scalar1=-1.0 / 16, scalar2=None,
                                                  op0=ALU.mult), reads=["c_maskB"], writes=["at_LmB"])
            P.op("pool", lambda e: e.memset(Em[:], -1.0 / 16), writes=["at_Em"])
            P.op("pool", lambda e: e.memset(nsc[:], -1.0 / 16), writes=["at_nsc"])

        if kind == "gla":
            SPD = self.D_(f"SPD{l}", [2 * c["M"], HK], F32)
            for mt in range(NTT):
                r0 = mt * 128
                pk = [("PB", mt, nb) for nb in range((NCOLS + 511) // 512)]
                P.dma("sp", gt[:, 0:32], PB[r0:r0 + C, lro:lro + 32], reads=pk, writes=["at_g"])
                for d in range(2):
                    tpb = self.tp(0)
                    P.op("pe", lambda e, d=d, tpb=tpb: e.transpose(out=tpb[0:16, 0:C], in_=gt[:, d * 16:(d + 1) * 16], identity=self.ident[:, :]),
                         reads=["at_g", "ident"], writes=[("tp", 0)])
                    P.op("act", lambda e, tpb=tpb: e.copy(out=lrT[0:16, :], in_=tpb[0:16, 0:C]), reads=[("tp", 0)], writes=["at_lrT"])
                    zps = self.accb(2)
                    P.op("pe", lambda e, d=d: e.matmul(out=zps[:, 0:HK], lhsT=lrT[:, :], rhs=wuph[:, d, 0:HK], start=True, stop=False),
                         reads=["at_lrT", "at_wuph"], writes=[("acc", 2)])
                    P.op("pe", lambda e, d=d: e.matmul(out=zps[:, 0:HK], lhsT=lrT[:, :], rhs=wupl[:, d, 0:HK], start=False, stop=True),
                         reads=["at_lrT", "at_wupl", ("acc", 2)], writes=[("acc", 2)])
                    P.op("act", lambda e: e.activation(out=sp[:, 0:HK], in_=zps[:, 0:HK], func=AF.Exp, scale=-1.0),
                         reads=[("acc", 2)], writes=["at_sp"])
                    P.op("act", lambda e: e.activation(out=sp[:, 0:HK], in_=sp[:, 0:HK], func=AF.Ln, bias=1.0),
                         reads=["at_sp"], writes=["at_sp"])
                    P.dma("sp", SPD[d * c["M"] + r0:d * c["M"] + r0 + C, :], sp[:, 0:HK], reads=["at_sp"], writes=[("SPD", d, mt)])
            P.ops.append(("*", None, (), (), "bar"))
            if c.get("gla_cut") == 1:
                return

        for d in range(2):
            order = (ctx_units + lat_units) if d == 0 else (ctx_units[::-1] + lat_units[::-1])
            mask = self.maskF if d == 0 else self.maskB
            mkey = "c_maskF" if d == 0 else "c_maskB"
            P.op("pool", lambda e: e.memset(S32[:], 0.0), writes=["at_S32"])
            P.op("pool", lambda e: e.memset(Sbf[:], 0.0), writes=["at_Sbf"])
            for mt in order:
                isc = mt >= NT
                r0 = mt * 128
                need_out = (not isc) or (not last)
                pk = [("PB", mt, nb) for nb in range((NCOLS + 511) // 512)]
                P.dma("sp", kt[:, 0:HK], PB[r0:r0 + C, ko:ko + HK], reads=pk, writes=["at_k"])
                P.dma("sp", vt[:, 0:HV], PB[r0:r0 + C, vo:vo + HV], reads=pk, writes=["at_v"])
                if need_out:
                    P.dma("sp", qt[:, 0:HK], PB[r0:r0 + C, qo:qo + HK], reads=pk, writes=["at_q"])
                qsrc, ksrc, qk_, kk_ = qt, kt, "at_q", "at_k"
                if kind == "ret":
                    if not isc:
                        P.dma("sp", rcs[:], self.small["rot_cs"][r0:r0 + 128, :], writes=["at_rcs"])
                        P.dma("sp", rsn[:], self.small["rot_sn"][r0:r0 + 128, :], writes=["at_rsn"])
                        for (src, skey, dstname) in ((qt, "at_q", "S0"), (kt, "at_k", "S1")):
                            dst = P.sb(dstname, [128, 2048], F32)
                            t1 = dst[:, 0:HK].rearrange("p (h x) -> p h x", h=H)
                            t2 = dst[:, 1024:1024 + HK].rearrange("p (h a b x) -> p h a b x", h=H, a=2, b=2)
                            sv = src[:, 0:HK].rearrange("p (h x) -> p h x", h=H)
                            sv5 = src[:, 0:HK].rearrange("p (h a b x) -> p h a b x", h=H, a=2, b=2)
                            csb = rcs[:].unsqueeze(1).broadcast_to([128, H, 128])
                            sn5 = rsn[:].rearrange("p (a b x) -> p a b x", a=2, b=2)
                            P.op("dve", lambda e, t1=t1, sv=sv, csb=csb: e.tensor_tensor(out=t1, in0=sv, in1=csb, op=ALU.mult),
                                 reads=[skey, "at_rcs"], writes=[dstname])
                            for b_ in range(2):
                                snb = sn5[:, :, b_, :].unsqueeze(1).broadcast_to([128, H, 2, 32])
                                P.op("pool", lambda e, t2=t2, sv5=sv5, snb=snb, b_=b_: e.tensor_tensor(
                                    out=t2[:, :, :, b_, :], in0=sv5[:, :, :, 1 - b_, :], in1=snb, op=ALU.mult),
                                    reads=[skey, "at_rsn"], writes=[dstname + "b"])
                            P.op("dve", lambda e, dst=dst: e.tensor_tensor(out=dst[:, 0:HK], in0=dst[:, 0:HK],
                                                                           in1=dst[:, 1024:1024 + HK], op=ALU.add),
                                 reads=[dstname, dstname + "b"], writes=[dstname])
                        qsrc, ksrc, qk_, kk_ = P.sb("S0", [128, 2048], F32), P.sb("S1", [128, 2048], F32), "S0", "S1"
                    bq = lambda j, d=d: tb[:, d, j, 0:H].unsqueeze(2).broadcast_to([128, H, dk])
                    tq, tki, tko = bq(0), bq(1), bq(2)
                    v3 = lambda t: t[:, 0:HK].rearrange("p (h x) -> p h x", h=H)
                    tkeys = ["at_tb"]
                    decap = tb[:, d, 3, 0:H]
                    dkeys = ["at_tb"]
                else:
                    P.dma("sp", sp[:, 0:HK], SPD[d * c["M"] + r0:d * c["M"] + r0 + C, :], reads=[("SPD", d, mt)], writes=["at_sp"])
                    if c.get("gla_cut") == 21:
                        continue
                    Lm = LmF if d == 0 else LmB
                    bps, eps_ = self.accb(2), self.accb(3)
                    P.op("dve", lambda e: e.tensor_copy(out=sph[:, 0:HK], in_=sp[:, 0:HK]), reads=["at_sp"], writes=["at_sph"])
                    P.op("dve", lambda e: e.tensor_tensor(out=spl[:, 0:HK], in0=sp[:, 0:HK], in1=sph[:, 0:HK], op=ALU.subtract),
                         reads=["at_sp", "at_sph"], writes=["at_spl"])
                    for (dst_, akey_, lhs_, lk_) in ((bps, ("acc", 2), Lm, ["at_LmF", "at_LmB"]), (eps_, ("acc", 3), Em, ["at_Em"])):
                        P.op("pe", lambda e, dst_=dst_, lhs_=lhs_: e.matmul(out=dst_[:, 0:HK], lhsT=lhs_[:, :], rhs=sph[:, 0:HK], start=True, stop=False),
                             reads=["at_sph"] + lk_, writes=[akey_])
                        P.op("pe", lambda e, dst_=dst_, lhs_=lhs_: e.matmul(out=dst_[:, 0:HK], lhsT=lhs_[:, :], rhs=spl[:, 0:HK], start=False, stop=True),
                             reads=["at_spl", akey_] + lk_, writes=[akey_])
                    if c.get("gla_cut") == 22:
                        continue
                    P.op("act", lambda e: e.activation(out=EQ[:, 0:HK], in_=bps[:, 0:HK], func=AF.Exp, bias=lnq),
                         reads=[("acc", 2)], writes=["at_EQ"])
                    P.op("act", lambda e: e.activation(out=EKI[:, 0:HK], in_=bps[:, 0:HK], func=AF.Exp, scale=-1.0),
                         reads=[("acc", 2)], writes=["at_EKI"])
                    if c.get("gla_cut") == 23:
                        continue
                    P.op("act", lambda e: e.activation(out=bsb[:, 0:HK], in_=eps_[:, 0:HK], func=AF.Exp), reads=[("acc", 3)], writes=["at_bsb"])
                    P.op("dve", lambda e: e.tensor_tensor(out=EKO[:, 0:HK], in0=bsb[:, 0:HK], in1=EKI[:, 0:HK], op=ALU.mult),
                         reads=["at_bsb", "at_EKI"], writes=["at_EKO"])
                    if c.get("gla_cut") == 2:
                        continue
                    dps = self.pp[2]
                    for p_ in range(npk):
                        P.op("pe", lambda e, p_=p_: e.matmul(out=dps[:, 512 + 2 * p_:512 + 2 * p_ + 2], lhsT=sph[:, p_ * 128:(p_ + 1) * 128], rhs=nsc[:, 0:2],
                                                             start=True, stop=False), reads=["at_sph", "at_nsc"], writes=[("tp", 1)])
                        P.op("pe", lambda e, p_=p_: e.matmul(out=dps[:, 512 + 2 * p_:512 + 2 * p_ + 2], lhsT=spl[:, p_ * 128:(p_ + 1) * 128], rhs=nsc[:, 0:2],
                                                             start=False, stop=True), reads=["at_spl", "at_nsc", ("tp", 1)], writes=[("tp", 1)])
                    P.op("act", lambda e: e.activation(out=dec[:, 0:npk, :], in_=dps[:, 512:512 + 2 * npk].rearrange("p (h x) -> p h x", h=npk), func=AF.Exp),
                         reads=[("tp", 1)], writes=["at_dec"])
                    if c.get("gla_cut") == 3:
                        continue
                    v3 = lambda t: t[:, 0:HK]
                    tq, tki, tko = EQ[:, 0:HK], EKI[:, 0:HK], EKO[:, 0:HK]
                    tkeys = ["at_EQ", "at_EKI", "at_EKO"]
                    decap = dec[:, 0:npk, 0]
                    dkeys = ["at_dec"]
                if need_out:
                    P.op("dve", lambda e, qsrc=qsrc, tq=tq, v3=v3: e.tensor_tensor(out=v3(qin), in0=v3(qsrc), in1=tq, op=ALU.mult),
                         reads=[qk_] + tkeys, writes=["at_qin"])
                    P.op("pool", lambda e, ksrc=ksrc, tki=tki, v3=v3: e.tensor_tensor(out=v3(kin), in0=v3(ksrc), in1=tki, op=ALU.mult),
                         reads=[kk_] + tkeys, writes=["at_kin"])
                P.op("dve", lambda e, ksrc=ksrc, tko=tko, v3=v3: e.tensor_tensor(out=v3(kout), in0=v3(ksrc), in1=tko, op=ALU.mult),
                     reads=[kk_] + tkeys, writes=["at_kout"])
                if c.get("gla_cut") == 4 and kind == "gla":
                    continue
                if need_out:
                    for (src, skey, dstT, dkey, tpi) in ((qin, "at_qin", qT, "at_qT", 0), (kin, "at_kin", kT, "at_kT", 1)):
                        tp = self.tp(tpi)
                        for p_ in range(npk):
                            P.op("pe", lambda e, tp=tp, src=src, p_=p_: e.transpose(out=tp[:, p_ * 128:(p_ + 1) * 128],
                                                                                   in_=src[:, p_ * 128:(p_ + 1) * 128], identity=self.ident[:, :]),
                                 reads=[skey, "ident"], writes=[("tp", tpi)])
                        tpv = tp[:, 0:npk * 128].rearrange("p (h x) -> p h x", h=npk)
                        if hp == 1:
                            P.op("act", lambda e, tpv=tpv, dstT=dstT: e.copy(out=dstT[:, 0:H, :], in_=tpv), reads=[("tp", tpi)], writes=[dkey])
                        else:
                            for j in range(hp):
                                P.op("act", lambda e, tpv=tpv, dstT=dstT, j=j: e.activation(out=dstT[:, j:H:hp, :], in_=tpv, func=AF.Copy, scale=rowm[:, j:j + 1]),
                                     reads=[("tp", tpi), "at_rowm"], writes=[dkey + str(j)])
                    if c.get("gla_cut") == 5 and kind == "gla":
                        continue
                    tkq = ["at_qT"] + [f"at_qT{j}" for j in range(hp)]
                    tkk = ["at_kT"] + [f"at_kT{j}" for j in range(hp)]
                    nbh = 4
                    for bk in range((H + nbh - 1) // nbh):
                        acc = self.accb(bk)
                        hs = list(range(bk * nbh, min(H, (bk + 1) * nbh)))
                        for h in hs:
                            P.op("pe", lambda e, acc=acc, h=h, bk=bk: e.matmul(out=acc[:, (h - bk * nbh) * C:(h - bk * nbh + 1) * C],
                                                                               lhsT=kT[:, h, :], rhs=qT[:, h, :], start=True, stop=True),
                                 reads=tkq + tkk, writes=[("acc", bk)])
                        P.op("dve", lambda e, acc=acc, hs=hs, mask=mask: e.tensor_tensor(
                            out=attT[:, hs[0]:hs[-1] + 1, :], in0=acc[:, 0:len(hs) * C].rearrange("p (h x) -> p h x", h=len(hs)),
                            in1=mask[:, :].unsqueeze(1).broadcast_to([C, len(hs), C]), op=ALU.mult),
                            reads=[("acc", bk), mkey], writes=["at_attT"])
                    if c.get("gla_cut") == 6 and kind == "gla":
                        continue
                    ops_ = self.pp[3]
                    for h in range(H):
                        P.op("pe", lambda e, h=h: e.matmul(out=ops_[:, h * dv:(h + 1) * dv], lhsT=attT[:, h, :],
                                                           rhs=vt[:, h * dv:(h + 1) * dv], start=True, stop=False),
                             reads=["at_attT", "at_v"], writes=["pp3"])
                        P.op("pe", lambda e, h=h: e.matmul(out=ops_[:, h * dv:(h + 1) * dv], lhsT=qT[:, h, :],
                                                           rhs=Sbf[:, (h // hp) * dv:(h // hp + 1) * dv], start=False, stop=True),
                             reads=tkq + ["at_Sbf", "pp3"], writes=["pp3"])
                if c.get("gla_cut") == 7 and kind == "gla":
                    continue
                kvp = self.pp[1]
                for h in range(H):
                    P.op("pe", lambda e, h=h: e.matmul(out=kvp[:, h * dv:(h + 1) * dv], lhsT=kout[:, (h // hp) * 128:(h // hp + 1) * 128],
                                                       rhs=vt[:, h * dv:(h + 1) * dv], start=True, stop=True),
                         reads=["at_kout", "at_v"], writes=[("acc", 2), ("acc", 3)])
                for j in range(hp):
                    rr = slice(j * dk, (j + 1) * dk)
                    s3 = S32[rr, 0:npk * dv].rearrange("p (h x) -> p h x", h=npk)
                    k3 = kvp[rr, 0:HV].rearrange("p (a b x) -> p a b x", a=npk, b=hp)[:, :, j, :]
                    P.op("dve", lambda e, s3=s3, decap=decap, rr=rr: e.tensor_tensor(out=s3, in0=s3, in1=decap[rr, :].unsqueeze(2).broadcast_to([dk, npk, dv]),
                                                                                    op=ALU.mult), reads=["at_S32"] + dkeys, writes=["at_S32"])
                    P.op("dve", lambda e, s3=s3, k3=k3: e.tensor_tensor(out=s3, in0=k3, in1=s3, op=ALU.add),
                         reads=["at_S32", ("acc", 2), ("acc", 3)], writes=["at_S32"])
                P.op("act", lambda e: e.copy(out=Sbf[:, 0:npk * dv], in_=S32[:, 0:npk * dv]), reads=["at_S32", "pp3"], writes=["at_Sbf"])
                if not need_out:
                    continue
                okey = ("OF", mt)
                if d == 0:
                    P.op("act", lambda e: e.copy(out=osb[:, 0:HV], in_=self.pp[3][:, 0:HV]), reads=["pp3"], writes=["S4"])
                    P.dma("sp", OF[r0:r0 + C, :], osb[:, 0:HV], reads=["S4"], writes=[okey])
                    continue
                P.dma("sp", ofl[:, 0:HV], OF[r0:r0 + C, :], reads=[okey], writes=["S5"])
                P.dma("sp", gt[:, 0:HV], PB[r0:r0 + C, go:go + HV], reads=pk, writes=["at_g"])
                P.op("dve", lambda e: e.tensor_tensor(out=osb[:, 0:HV], in0=self.pp[3][:, 0:HV], in1=ofl[:, 0:HV], op=ALU.add),
                     reads=["pp3", "S5"], writes=["S4"])
                if f"OFB{l}" in c["debug"]:
                    if not hasattr(self, "OFB"):
                        self.OFB = self.D_(f"OFB{l}", [c["M"], HV], F32)
                    P.dma("sp", self.OFB[r0:r0 + C, :], osb[:, 0:HV], reads=["S4"], writes=[("OFB", mt)])
                sq = ofl
                ss = P.sb("at_ss", [128, 8], F32)
                P.op("pool", lambda e: e.tensor_tensor(out=sq[:, 0:HV], in0=osb[:, 0:HV], in1=osb[:, 0:HV], op=ALU.mult),
                     reads=["S4"], writes=["S5"])
                P.op("dve", lambda e: e.tensor_reduce(out=ss[:, 0:H], in_=sq[:, 0:HV].rearrange("p (h x) -> p h x", h=H),
                                                      axis=mybir.AxisListType.X, op=ALU.add), reads=["S5"], writes=["at_ss"])
                P.op("dve", lambda e: e.tensor_scalar(out=ss[:, 0:H], in0=ss[:, 0:H], scalar1=1.0 / dv, scalar2=1e-6,
                                                      op0=ALU.mult, op1=ALU.add), reads=["at_ss"], writes=["at_ss"])
                P.op("act", lambda e: e.activation(out=ss[:, 0:H], in_=ss[:, 0:H], func=AF.Sqrt), reads=["at_ss"], writes=["at_ss"])
                P.op("dve", lambda e: e.reciprocal(out=ss[:, 0:H], in_=ss[:, 0:H]), reads=["at_ss"], writes=["at_ss"])
                o3 = osb[:, 0:HV].rearrange("p (h x) -> p h x", h=H)
                P.op("dve", lambda e, o3=o3: e.tensor_tensor(out=o3, in0=o3, in1=ss[:, 0:H].unsqueeze(2).broadcast_to([C, H, dv]), op=ALU.mult),
                     reads=["S4", "at_ss"], writes=["S4"])
                P.op("pool", lambda e: e.tensor_tensor(out=osb[:, 0:HV], in0=osb[:, 0:HV], in1=gn[:, 0:HV], op=ALU.mult),
                     reads=["S4", "at_gn"], writes=["S4"])
                P.op("act", lambda e: e.activation(out=sq[:, 0:HV], in_=gt[:, 0:HV], func=AF.Silu), reads=["at_g"], writes=["S5"])
                ob = P.sb("at_ob", [128, 1024], BF16)
                P.op("dve", lambda e, ob=ob: e.tensor_tensor(out=ob[:, 0:HV], in0=osb[:, 0:HV], in1=sq[:, 0:HV], op=ALU.mult),
                     reads=["S4", "S5"], writes=["at_ob"])
                P.dma("sp", self.MIX[r0:r0 + C, mixo:mixo + HV], ob[:, 0:HV], reads=["at_ob"], writes=[("MIX", mt, kind, 0)])

    def nattn(self, l):
        c, P = self.c, self.P
        PB = self.PB
        NT, NTT, T, L = c["NT"], c["NTT"], c["T"], c["L"]
        NAH, NAD = c["NAH"], c["NAD"]
        nkt = self.nkt()
        Wk = nkt * 128
        qo, ko = 0, c["ODQ"]
        vo = ko + NAD
        pkeys = lambda mt: [("PB", mt, nb) for nb in range((c["ODC"] + 511) // 512)]
        QT = self.D_(f"QT{l}", [NAH, 128, T], BF16)
        KT = self.D_(f"KT{l}", [NAH, 128, c["M"]], BF16)
        cls_of, _ = na_geometry(c)
        for mt in range(NTT):
            for (which, off, dst, scale) in (("q", qo, QT, 128 ** -0.5), ("k", ko, KT, 1.0)):
                if which == "q" and mt >= NT:
                    continue
                src = P.sb(f"na_src{which}", [128, 1024], BF16)
                stg = P.sb(f"na_stg{which}", [128, 8, 128], BF16)
                tpi = 0 if which == "q" else 1
                tp = self.tp(tpi)
                P.dma("sp", src[:, 0:NAD], PB[mt * 128:(mt + 1) * 128, off:off + NAD], reads=pkeys(mt), writes=[f"na_src{which}"])
                for h in range(NAH):
                    P.op("pe", lambda e, tp=tp, src=src, h=h: e.transpose(out=tp[:, h * 128:(h + 1) * 128], in_=src[:, h * 128:(h + 1) * 128], identity=self.ident[:, :]),
                         reads=[f"na_src{which}", "ident"], writes=[("tp", tpi)])
                P.op("act", lambda e, tp=tp, stg=stg, scale=scale: e.activation(out=stg[:, 0:NAH, :], in_=tp[:, 0:NAH * 128].rearrange("p (h x) -> p h x", h=NAH),
                                                                              func=AF.Copy, scale=scale), reads=[("tp", tpi)], writes=[f"na_stg{which}"])
                P.dma("sp", dst[:, :, mt * 128:(mt + 1) * 128].rearrange("h d t -> d h t"), stg[:, 0:NAH, :], reads=[f"na_stg{which}"], writes=[(which + "T", mt)])
        vctx = P.sb("na_vctx", [128, c["NCT"], 1024], BF16)
        kctx = P.sb("na_kctx", [128, 8, L], BF16)
        for j in range(c["NCT"]):
            P.dma("sp", vctx[:, j, 0:NAD], PB[(NT + j) * 128:(NT + j + 1) * 128, vo:vo + NAD], reads=pkeys(NT + j), writes=[("na_vctx", j)])
        P.dma("sp", kctx[:, 0:NAH, :], KT[:, :, T:T + L].rearrange("h d t -> d h t"), reads=[("kT", NT + j) for j in range(c["NCT"])], writes=["na_kctx"])
        vwin = P.sb("na_vwin", [128, 5, 1024], BF16)
        kTh = P.sb("na_kTh", [128, 640], BF16)
        qTh = P.sb("na_qTh", [128, 128], BF16)
        bias = P.sb("na_bias", [128, 640], F32)
        sc = P.sb("na_sc", [128, 896], F32)
        pb = P.sb("na_pb", [128, 896], BF16)
        pT = P.sb("na_pT", [128, 7, 128], BF16)
        osb = P.sb("na_osb", [128, 1024], BF16)
        mx = P.sb("na_mx", [128, 1], F32)
        sm = P.sb("na_sm", [128, 1], F32)
        nj = nkt + c["NCT"]
        for mt in range(NT):
            kt0 = min(max(mt - 2, 0), NT - nkt)
            for j in range(nkt):
                P.dma("sp", vwin[:, j, 0:NAD], PB[(kt0 + j) * 128:(kt0 + j + 1) * 128, vo:vo + NAD], reads=pkeys(kt0 + j), writes=[("na_vwin", j)])
            for h in range(NAH):
                P.dma("sp", qTh[:, :], QT[h, :, mt * 128:(mt + 1) * 128], reads=[("qT", mt)], writes=["na_qTh"])
                P.dma("sp", kTh[:, 0:Wk], KT[h, :, kt0 * 128:kt0 * 128 + Wk], reads=[("kT", kt0 + j) for j in range(nkt)], writes=["na_kTh"])
                P.dma("sp", bias[:, 0:Wk], self.small[f"na_bias{l}"][cls_of[mt], h, :, :], writes=["na_bias"])
                sps = self.pp[0]
                for c0 in range(0, Wk, 512):
                    cn = min(512, Wk - c0)
                    P.op("pe", lambda e, c0=c0, cn=cn: e.matmul(out=sps[:, c0:c0 + cn], lhsT=qTh[:, :], rhs=kTh[:, c0:c0 + cn], start=True, stop=True),
                         reads=["na_qTh", "na_kTh"], writes=[("acc", c0 // 512)])
                P.op("pe", lambda e, h=h: e.matmul(out=sps[:, Wk:Wk + L], lhsT=qTh[:, :], rhs=kctx[:, h, :], start=True, stop=True),
                     reads=["na_qTh", "na_kctx", ("acc", 1)], writes=[("acc", 1)])
                P.op("dve", lambda e: e.tensor_tensor(out=sc[:, 0:Wk], in0=sps[:, 0:Wk], in1=bias[:, 0:Wk], op=ALU.add),
                     reads=[("acc", 0), ("acc", 1), "na_bias"], writes=["na_sc"])
                P.op("act", lambda e: e.copy(out=sc[:, Wk:Wk + L], in_=sps[:, Wk:Wk + L]), reads=[("acc", 1)], writes=["na_scc"])
                P.op("dve", lambda e: e.tensor_reduce(out=mx[:], in_=sc[:, 0:Wk + L], axis=mybir.AxisListType.X, op=ALU.max), reads=["na_sc", "na_scc"], writes=["na_mx"])
                P.op("dve", lambda e: e.tensor_scalar(out=mx[:], in0=mx[:], scalar1=-1.0, scalar2=None, op0=ALU.mult), reads=["na_mx"], writes=["na_mx"])
                P.op("act", lambda e: e.activation(out=pb[:, 0:Wk + L], in_=sc[:, 0:Wk + L], func=AF.Exp, bias=mx[:, 0:1], accum_out=sm[:]),
                     reads=["na_sc", "na_scc", "na_mx"], writes=["na_pb", "na_sm"])
                P.op("dve", lambda e: e.reciprocal(out=sm[:], in_=sm[:]), reads=["na_sm"], writes=["na_sm"])
                tp = self.tp(0)
                for j in range(nj):
                    P.op("pe", lambda e, j=j: e.transpose(out=tp[:, j * 128:(j + 1) * 128], in_=pb[:, j * 128:(j + 1) * 128], identity=self.ident[:, :]),
                         reads=["na_pb", "ident"], writes=[("tp", 0)])
                P.op("act", lambda e: e.copy(out=pT[:, 0:nj, :], in_=tp[:, 0:nj * 128].rearrange("p (j x) -> p j x", j=nj)), reads=[("tp", 0)], writes=["na_pT"])
                ops_ = self.accb(2)
                for j in range(nj):
                    rhs = vwin[:, j, h * 128:(h + 1) * 128] if j < nkt else vctx[:, j - nkt, h * 128:(h + 1) * 128]
                    rk = ("na_vwin", j) if j < nkt else ("na_vctx", j - nkt)
                    P.op("pe", lambda e, j=j, rhs=rhs: e.matmul(out=ops_[:, 0:128], lhsT=pT[:, j, :], rhs=rhs, start=(j == 0), stop=(j == nj - 1)),
                         reads=["na_pT", rk], writes=[("acc", 2)])
                P.op("dve", lambda e, h=h: e.tensor_scalar(out=osb[:, h * 128:(h + 1) * 128], in0=ops_[:, 0:128], scalar1=sm[:, 0:1], scalar2=None, op0=ALU.mult),
                     reads=[("acc", 2), "na_sm"], writes=[("na_osb", h)])
            P.dma("sp", self.MIX[mt * 128:(mt + 1) * 128, 0:NAD], osb[:, 0:NAD], reads=[("na_osb", h) for h in range(NAH)], writes=[("MIX", mt, "na", 0)])

    def gmlp(self, l, last):
        c, P = self.c, self.P
        PB = self.PB
        GD, GW, RD = c["GD"], c["GW"], c["RD"]
        uo, vo = 2 * RD, 2 * RD + GD
        wsT = P.sb("gm_wsT", [128, 8, 128], BF16)
        wsf = P.sb("S0", [128, 2048], F32)
        P.dma("sp", wsf[:, 0:1024].rearrange("p (g x) -> p g x", g=8), self.small[f"gmlp_wsT{l}"].rearrange("g s p -> s g p"), writes=["S0"])
        P.op("dve", lambda e: e.tensor_copy(out=wsT[:], in_=wsf[:, 0:1024].rearrange("p (g x) -> p g x", g=8)), reads=["S0"], writes=["gm_wsT"])
        bsT = P.sb("gm_bsT", [128, 8], F32)
        P.dma("sp", bsT[:], self.small[f"gmlp_bsT{l}"], writes=["gm_bsT"])
        gng = P.sb("S1", [128, 2048], F32)
        P.dma("sp", gng[:, 0:GD], self.small[f"gmlp_norm{l}"].partition_broadcast(128), writes=["S1"])
        ntl = c["NT"] if last else c["NTT"]
        K2 = 2 * math.sqrt(2.0 / math.pi)
        for mt in range(ntl):
            pk = [("PB", mt, nb) for nb in range(c["EVC"] // 512)]
            r0 = mt * 128
            raw = P.sb("at_q", [128, 1024], BF16)
            raw2 = P.sb("at_k", [128, 1024], BF16)
            P.dma("sp", raw[:, 0:GD], PB[r0:r0 + 128, uo:uo + GD], reads=pk, writes=["at_q"])
            P.dma("sp", raw2[:, 0:GD], PB[r0:r0 + 128, vo:vo + GD], reads=pk, writes=["at_k"])
            gl = {}
            for (src, skey, dname) in ((raw, "at_q", "S2"), (raw2, "at_k", "S3")):
                dst = P.sb(dname, [128, 2048], F32)
                a, b = dst[:, 0:GD], dst[:, 1024:1024 + GD]
                eng = "dve" if dname == "S2" else "pool"
                P.op(eng, lambda e, a=a, src=src: e.tensor_tensor(out=a, in0=src[:, 0:GD], in1=src[:, 0:GD], op=ALU.mult), reads=[skey], writes=[dname])
                P.op(eng, lambda e, a=a: e.tensor_scalar(out=a, in0=a, scalar1=0.044715, scalar2=1.0, op0=ALU.mult, op1=ALU.add), reads=[dname], writes=[dname])
                P.op(eng, lambda e, a=a, src=src: e.tensor_tensor(out=a, in0=a, in1=src[:, 0:GD], op=ALU.mult), reads=[dname, skey], writes=[dname])
                P.op("act", lambda e, a=a, b=b: e.activation(out=b, in_=a, func=AF.Sigmoid, scale=K2), reads=[dname], writes=[dname + "b"])
                P.op(eng, lambda e, a=a, b=b, src=src: e.tensor_tensor(out=a, in0=b, in1=src[:, 0:GD], op=ALU.mult), reads=[dname + "b", skey], writes=[dname])
                gl[dname] = a
            ug, vg = gl["S2"], gl["S3"]
            ss = P.sb("nt_ss", [128, 1], F32)
            rs = P.sb("nt_rs", [128, 1], F32)
            junk = P.sb("nt_junk", [128, 2048], BF16)
            P.op("act", lambda e: e.activation(out=junk[:, 0:GD], in_=vg, func=AF.Square, accum_out=ss[:]), reads=["S3"], writes=["nt_junk", "nt_ss"])
            P.op("dve", lambda e: e.tensor_scalar(out=rs[:], in0=ss[:], scalar1=1.0 / GD, scalar2=1e-6, op0=ALU.mult, op1=ALU.add), reads=["nt_ss"], writes=["nt_rs"])
            P.op("act", lambda e: e.activation(out=rs[:], in_=rs[:], func=AF.Sqrt), reads=["nt_rs"], writes=["nt_rs"])
            P.op("dve", lambda e: e.reciprocal(out=rs[:], in_=rs[:]), reads=["nt_rs"], writes=["nt_rs"])
            vn = P.sb("at_v", [128, 1024], BF16)
            P.op("dve", lambda e: e.scalar_tensor_tensor(out=vn[:, 0:GD], in0=vg, scalar=rs[:, 0:1], in1=gng[:, 0:GD], op0=ALU.mult, op1=ALU.mult),
                 reads=["S3", "nt_rs", "S1"], writes=["at_v"])
            ob = P.sb("at_ob", [128, 1024], BF16)
            for g in range(8):
                bank = g * GW // 512
                acc = self.accb(bank)
                col = g * GW - bank * 512
                P.op("pe", lambda e, acc=acc, g=g, col=col: e.matmul(out=acc[:, col:col + GW], lhsT=wsT[:, g, :], rhs=vn[:, g * GW:(g + 1) * GW], start=True, stop=True),
                     reads=["gm_wsT", "at_v"], writes=[("acc", bank)])
                P.op("dve", lambda e, acc=acc, g=g, col=col: e.scalar_tensor_tensor(out=ob[:, g * GW:(g + 1) * GW], in0=acc[:, col:col + GW], scalar=bsT[:, g:g + 1],
                                                                                    in1=ug[:, g * GW:(g + 1) * GW], op0=ALU.add, op1=ALU.mult),
                     reads=[("acc", bank), "gm_bsT", "S2"], writes=["at_ob"])
            P.dma("sp", self.MIX[r0:r0 + 128, RD:RD + GD], ob[:, 0:GD], reads=["at_ob"], writes=[("MIX", mt, "gmlp", 0)])

    def phase_final(self):
        c, P = self.c, self.P
        D = c["D"]
        P.push()
        g = P.sb("S0", [128, 2048], F32)
        P.dma("sp", g[:, 0:D], self.small["norm_final"].partition_broadcast(128), writes=["S0"])
        for mt in range(c["NT"]):
            i = mt % 2
            ob = P.sb(f"S{1 + i}", [128, 2048], F32)
            xt = P.sb("S5", [128, 2048], F32)
            junk = P.sb("nt_junk", [128, 2048], BF16)
            ss = P.sb("nt_ss", [128, 1], F32)
            rs = P.sb("nt_rs", [128, 1], F32)
            P.dma("sp", xt[:, 0:D], self.XC[mt * 128:(mt + 1) * 128, :], reads=[("XC", mt)], writes=["S5"])
            P.op("act", lambda e: e.activation(out=junk[:, 0:D], in_=xt[:, 0:D], func=AF.Square, accum_out=ss[:]),
                 reads=["S5"], writes=["nt_junk", "nt_ss"])
            P.op("dve", lambda e: e.tensor_scalar(out=rs[:], in0=ss[:], scalar1=1.0 / D, scalar2=1e-6,
                                                  op0=ALU.mult, op1=ALU.add), reads=["nt_ss"], writes=["nt_rs"])
            P.op("act", lambda e: e.activation(out=rs[:], in_=rs[:], func=AF.Sqrt), reads=["nt_rs"], writes=["nt_rs"])
            P.op("dve", lambda e: e.reciprocal(out=rs[:], in_=rs[:]), reads=["nt_rs"], writes=["nt_rs"])
            P.op("dve", lambda e, ob=ob: e.scalar_tensor_tensor(out=ob[:, 0:D], in0=xt[:, 0:D], scalar=rs[:, 0:1], in1=g[:, 0:D],
                                                                op0=ALU.mult, op1=ALU.mult),
                 reads=["S5", "nt_rs", "S0"], writes=[f"S{1 + i}"])
            P.dma("sp", self.out[mt * 128:(mt + 1) * 128, :], ob[:, 0:D], reads=[f"S{1 + i}"], writes=[("out", mt)])
        P.pop()


def na_geometry(c):
    NT, rows = c["NT"], c["rows"]
    nkt = min(5, NT)
    Wk = nkt * 128
    wr = min(8, rows)
    uniq, cls_of, maps = {}, [], []
    for mt in range(NT):
        kt0 = min(max(mt - 2, 0), NT - nkt)
        m = np.full((128, Wk), -1, np.int64)
        for p in range(128):
            t = mt * 128 + p
            r, col = t // 64, t % 64
            rstart = min(max(r - wr // 2, 0), rows - wr)
            cstart = min(max(col - 8, 0), 64 - 16)
            for r2 in range(rstart, rstart + wr):
                kk0 = r2 * 64 - kt0 * 128
                dr = r2 - r + 7
                for c2 in range(cstart, cstart + 16):
                    kk = kk0 + c2
                    assert 0 <= kk < Wk
                    m[p, kk] = dr * 31 + (c2 - col + 15)
        key = m.tobytes()
        if key not in uniq:
            uniq[key] = len(maps)
            maps.append(m)
        cls_of.append(uniq[key])
    return cls_of, maps


def host_inputs(c, inputs, ncores):
    f32 = lambda a: np.ascontiguousarray(a, dtype=np.float32)
    E, D, F = c["E"], c["D"], c["F"]
    shared = {"norm_final": f32(inputs["norm_final"])[None, :]}
    for l in range(c["depth"]):
        li = l // 2
        shared[f"ada{l}"] = f32(inputs["ada_w"][l])
        shared[f"win{l}"] = f32(inputs["ev_w_in"][li] if l % 2 == 0 else inputs["od_w_in"][li])
        shared[f"wout{l}"] = f32(inputs["ev_w_out"][li] if l % 2 == 0 else inputs["od_w_out"][li])
        shared[f"wg{l}"] = f32(inputs["moe_w_gate"][l]).reshape(E * D, F)
        shared[f"wu{l}"] = f32(inputs["moe_w_up"][l]).reshape(E * D, F)
        shared[f"wd{l}"] = f32(inputs["moe_w_down"][l]).reshape(E * F, D)
        shared[f"ada_b{l}"] = f32(inputs["ada_b"][l])[None, :]
        shared[f"norm_mix{l}"] = f32(inputs["norm_mix"][l])[None, :]
        shared[f"norm_ffn{l}"] = f32(inputs["norm_ffn"][l])[None, :]
        shared[f"router{l}"] = f32(inputs["router_w"][l])
        if l % 2 == 0:
            shared[f"gamma{l}"] = f32(inputs["ret_gamma_logit"][li]).reshape(1, -1)
            shared[f"ret_norm{l}"] = f32(inputs["ret_norm"][li])[None, :]
            shared[f"gmlp_norm{l}"] = f32(inputs["gmlp_norm"][li])[None, :]
            shared[f"gmlp_wsT{l}"] = f32(np.transpose(inputs["gmlp_ws"][li], (0, 2, 1)))
            shared[f"gmlp_bsT{l}"] = f32(np.transpose(inputs["gmlp_bs"][li], (1, 0)))
        else:
            shared[f"gla_wup{l}"] = f32(inputs["gla_w_up"][li])
            shared[f"gla_bup{l}"] = f32(inputs["gla_b_up"][li])[None]
            shared[f"gla_norm{l}"] = f32(inputs["gla_norm"][li])[None, :]
            cls_of, maps = na_geometry(c)
            rpb = f32(inputs["na_rpb"][li]).reshape(c["NAH"], -1)
            tabs = np.empty((6, c["NAH"], 128, maps[0].shape[1]), np.float32)
            tabs[:] = NEG
            for ci, m in enumerate(maps):
                valid = m >= 0
                for h in range(c["NAH"]):
                    tabs[ci, h][valid] = rpb[h][m[valid]]
            shared[f"na_bias{l}"] = tabs
    T = c["T"]
    t = np.arange(T)
    freq = (10000.0 ** (-np.arange(32, dtype=np.float32) / 32)).astype(np.float32)
    angr = ((t // 64).astype(np.float32)[:, None] * freq).astype(np.float32)
    angc = ((t % 64).astype(np.float32)[:, None] * freq).astype(np.float32)
    shared["rot_cs"] = np.concatenate([np.cos(angr), np.cos(angr), np.cos(angc), np.cos(angc)], 1).astype(np.float32)
    shared["rot_sn"] = np.concatenate([-np.sin(angr), np.sin(angr), -np.sin(angc), np.sin(angc)], 1).astype(np.float32)
    maps = []
    for core in range(ncores):
        s = core % 4
        m = dict(shared)
        m.update({"x": f32(inputs["x"][s]), "ctx": f32(inputs["ctx"][s]),
                  "cc": np.stack([inputs["c"][s], inputs["c_ctx"]]).astype(np.float32)})
        maps.append(m)
    return maps


def run(c, inputs):
    kb = K(c)
    nc = kb.build()
    maps = host_inputs(c, {k: np.asarray(v) for k, v in inputs.items()}, NCORES)
    res = run_bass_kernel_spmd(nc, maps, core_ids=list(range(NCORES)))
    return kb, res


def kernel(**inputs):
    c = make_cfg()
    kb, res = run(c, inputs)
    return np.stack([res.results[s]["out"] for s in range(4)]).astype(np.float32)
```
